# Optimizing a Trainium2 kernel written in Bass

```python
import math
import jax
import jax.numpy as jnp
from jax import lax
import numpy as np

D_MODEL = 1024
BATCH = 16
SEQ = 2048
DEPTH = 2

CTX_LEN = 256
GRID_W = 64
HEAD_DIM = 64
A_HEADS = D_MODEL // (2 * HEAD_DIM)
A_KV_HEADS = A_HEADS // 4
A_GROUP = A_HEADS // A_KV_HEADS
A_Q_BLOCK = 128
B_HEADS = D_MODEL // (4 * HEAD_DIM)
WIN_R = 8
WIN_C = 16
C_WIDTH = D_MODEL // 4
CONV_W = 3
A_WIDTH = A_HEADS * HEAD_DIM
A_KV_WIDTH = A_KV_HEADS * HEAD_DIM
B_WIDTH = B_HEADS * HEAD_DIM
MIX_WIDTH = A_WIDTH + B_WIDTH + C_WIDTH
IN_SIZES = (A_WIDTH, B_WIDTH, C_WIDTH, C_WIDTH, C_WIDTH, A_KV_WIDTH, A_KV_WIDTH, B_WIDTH, B_WIDTH)
KV_START = A_WIDTH + B_WIDTH + 3 * C_WIDTH
KV_SIZES = (A_KV_WIDTH, A_KV_WIDTH, B_WIDTH, B_WIDTH)
IN_WIDTH = KV_START + 2 * A_KV_WIDTH + 2 * B_WIDTH
D_FF = 256 * ((8 * D_MODEL // 3 + 255) // 256)
N_EXPERTS = 8
TOP_K = 2
MOE_BLOCK = 512
ROPE_THETA = 10000.0
NORM_EPS = 1e-6
NEG_INF = -1e30

kernel_name = 'hybrid_parallel_group_dit_block'


def rms_norm(x, gain=None):
    xf = x.astype(jnp.float32)
    y = (xf * lax.rsqrt(jnp.mean(xf * xf, axis=-1, keepdims=True) + NORM_EPS)).astype(x.dtype)
    return y if gain is None else y * gain


def modulate(x, shift, scale):
    return rms_norm(x) * (1 + scale) + shift


def split_cols(p, sizes):
    return jnp.split(p, np.cumsum(sizes)[:-1].tolist(), axis=-1)


def axial_rope_tables(n_tokens):
    t = jnp.arange(n_tokens)
    row = (t // GRID_W).astype(jnp.float32)
    col = (t % GRID_W).astype(jnp.float32)
    half = HEAD_DIM // 2
    inv_freq = ROPE_THETA ** (-jnp.arange(0, half, 2, dtype=jnp.float32) / half)
    ang_r = row[:, None] * inv_freq
    ang_c = col[:, None] * inv_freq
    return (jnp.cos(ang_r), jnp.sin(ang_r), jnp.cos(ang_c), jnp.sin(ang_c))


def axial_rope(x, tabs):
    cos_r, sin_r, cos_c, sin_c = tabs
    extra = x.ndim - 3

    def rot(u, cos, sin):
        cos = cos.reshape(cos.shape[0], *([1] * extra), cos.shape[1]).astype(u.dtype)
        sin = sin.reshape(sin.shape[0], *([1] * extra), sin.shape[1]).astype(u.dtype)
        u1, u2 = jnp.split(u, 2, axis=-1)
        return jnp.concatenate([u1 * cos - u2 * sin, u2 * cos + u1 * sin], axis=-1)

    xr, xc = jnp.split(x, 2, axis=-1)
    return jnp.concatenate([rot(xr, cos_r, sin_r), rot(xc, cos_c, sin_c)], axis=-1)


def gqa_attend(q, k, v):
    s = jnp.einsum('bqkgd,bskd->bkgqs', q, k, preferred_element_type=jnp.float32) * (HEAD_DIM ** -0.5)
    p = jax.nn.softmax(s, axis=-1).astype(v.dtype)
    return jnp.einsum('bkgqs,bskd->bqkgd', p, v)


def global_gqa(q, k, v, k_ctx, v_ctx):
    b, s = q.shape[:2]
    nb = s // A_Q_BLOCK
    k_all = jnp.concatenate([k, k_ctx], axis=1)
    v_all = jnp.concatenate([v, v_ctx], axis=1)
    qb = q.reshape(b, nb, A_Q_BLOCK, A_KV_HEADS, A_GROUP, HEAD_DIM).transpose(1, 0, 2, 3, 4, 5)
    o = lax.map(lambda q_blk: gqa_attend(q_blk, k_all, v_all), qb)
    return o.transpose(1, 0, 2, 3, 4, 5).reshape(b, s, A_WIDTH)


def neighbourhood_attn(q, k, v, k_ctx, v_ctx, rpb):
    b, s, n_heads, hd = q.shape
    rows = s // GRID_W
    wr = min(WIN_R, rows)
    r = np.arange(rows)
    rs = np.clip(r - wr // 2, 0, rows - wr)
    cc = np.arange(GRID_W)
    cs = np.clip(cc - WIN_C // 2, 0, GRID_W - WIN_C)
    dr_idx = rs[:, None] + np.arange(wr)[None, :] - r[:, None] + WIN_R - 1
    dc_idx = np.clip(cc[None, :] - cc[:, None] + WIN_C - 1, 0, 2 * WIN_C - 2)
    in_win = (cc[None, :] >= cs[:, None]) & (cc[None, :] < cs[:, None] + WIN_C)
    bias = rpb[:, dr_idx[:, :, None, None], dc_idx[None, None, :, :]].astype(jnp.float32)
    bias = jnp.where(in_win[None, None, None], bias, NEG_INF)
    bias = bias.transpose(1, 0, 3, 2, 4).reshape(rows, n_heads, GRID_W, wr * GRID_W)
    k_grid = k.reshape(b, rows, GRID_W, n_heads, hd)
    v_grid = v.reshape(b, rows, GRID_W, n_heads, hd)
    q_rows = q.reshape(b, rows, GRID_W, n_heads, hd).transpose(1, 0, 2, 3, 4)
    scale = hd ** -0.5

    def row_block(args):
        q_r, r0, bias_r = args
        k_band = lax.dynamic_slice_in_dim(k_grid, r0, wr, axis=1).reshape(b, wr * GRID_W, n_heads, hd)
        v_band = lax.dynamic_slice_in_dim(v_grid, r0, wr, axis=1).reshape(b, wr * GRID_W, n_heads, hd)
        s_lat = jnp.einsum('bqhd,bkhd->bhqk', q_r, k_band, preferred_element_type=jnp.float32) * scale + bias_r[None]
        s_ctx = jnp.einsum('bqhd,bkhd->bhqk', q_r, k_ctx, preferred_element_type=jnp.float32) * scale
        p = jax.nn.softmax(jnp.concatenate([s_lat, s_ctx], axis=-1), axis=-1).astype(v.dtype)
        v_all = jnp.concatenate([v_band, v_ctx], axis=1)
        return jnp.einsum('bhqk,bkhd->bqhd', p, v_all)

    o = lax.map(row_block, (q_rows, jnp.asarray(rs, jnp.int32), bias))
    return o.transpose(1, 0, 2, 3, 4).reshape(b, s, n_heads * hd)


def short_conv_mixer(u_post, u_pre, u_val, conv_w, conv_b):
    z = u_pre * u_val
    y = lax.conv_general_dilated(z, conv_w[:, None, :], window_strides=(1,),
                                 padding=((CONV_W // 2, CONV_W // 2),),
                                 dimension_numbers=('NWC', 'WIO', 'NWC'),
                                 feature_group_count=C_WIDTH) + conv_b
    return u_post * y


def merge_groups(oa, ob, oc, g_out, w_out):
    ga, gb, gc = split_cols(g_out, (A_WIDTH, B_WIDTH, C_WIDTH))
    cat = jnp.concatenate([rms_norm(oa, ga), rms_norm(ob, gb), rms_norm(oc, gc)], axis=-1)
    return cat @ w_out


def swiglu(h, w1, w3, w2):
    return (jax.nn.silu(h @ w1) * (h @ w3)) @ w2


def moe_swiglu(h, w_router, b_router, w1, w3, w2):
    shp = h.shape
    xf = h.reshape(-1, shp[-1])
    n = xf.shape[0]
    logits = (xf @ w_router).astype(jnp.float32) + b_router.astype(jnp.float32)
    top_v, top_e = lax.top_k(logits, TOP_K)
    gates = jax.nn.softmax(top_v, axis=-1)
    e_flat = top_e.reshape(-1)
    g_flat = gates.reshape(-1)
    tok = jnp.repeat(jnp.arange(n, dtype=jnp.int32), TOP_K)
    n_assign = n * TOP_K
    counts = jnp.zeros((N_EXPERTS,), jnp.int32).at[e_flat].add(1)
    padded = (counts + MOE_BLOCK - 1) // MOE_BLOCK * MOE_BLOCK
    pad_end = jnp.cumsum(padded)
    pad_start = pad_end - padded
    start = jnp.cumsum(counts) - counts
    order = jnp.argsort(e_flat)
    e_sorted = e_flat[order]
    dest = pad_start[e_sorted] + jnp.arange(n_assign, dtype=jnp.int32) - start[e_sorted]
    n_blocks = -(-n_assign // MOE_BLOCK) + N_EXPERTS
    cap = n_blocks * MOE_BLOCK
    buf_tok = jnp.zeros((cap,), jnp.int32).at[dest].set(tok[order])
    buf_gate = jnp.zeros((cap,), jnp.float32).at[dest].set(g_flat[order])
    blk_e = jnp.minimum(jnp.searchsorted(pad_end, jnp.arange(n_blocks, dtype=jnp.int32) * MOE_BLOCK, side='right'),
                        N_EXPERTS - 1).astype(jnp.int32)
    xb = xf[buf_tok].reshape(n_blocks, MOE_BLOCK, shp[-1])

    def expert_block(args):
        x_blk, e = args
        return swiglu(x_blk, w1[e], w3[e], w2[e])

    yb = lax.map(expert_block, (xb, blk_e)).reshape(cap, shp[-1])
    y = jax.ops.segment_sum(yb * buf_gate[:, None].astype(yb.dtype), buf_tok, num_segments=n)
    return y.reshape(shp)


def setup_inputs(seed: int = 0) -> dict:
    key = jax.random.key(seed)
    ks = iter(jax.random.split(key, 32))
    n_dense = (DEPTH + 1) // 2
    n_moe = DEPTH // 2

    def nrm(shape, scale):
        return jax.random.normal(next(ks), shape, jnp.float32) * scale

    return {
        'x': nrm((BATCH, SEQ, D_MODEL), 1.0),
        'c': nrm((BATCH, D_MODEL), 1.0),
        'ctx': nrm((BATCH, CTX_LEN, D_MODEL), 1.0),
        'c_ctx': nrm((D_MODEL,), 1.0),
        'w_mod': nrm((DEPTH, D_MODEL, 6 * D_MODEL), 0.5 * D_MODEL ** -0.5),
        'b_mod': nrm((DEPTH, 6 * D_MODEL), 0.02),
        'w_in': nrm((DEPTH, D_MODEL, IN_WIDTH), D_MODEL ** -0.5),
        'gq_a': 1.0 + nrm((DEPTH, HEAD_DIM), 0.02),
        'gk_a': 1.0 + nrm((DEPTH, HEAD_DIM), 0.02),
        'gq_b': 1.0 + nrm((DEPTH, HEAD_DIM), 0.02),
        'gk_b': 1.0 + nrm((DEPTH, HEAD_DIM), 0.02),
        'rpb': nrm((DEPTH, B_HEADS, 2 * WIN_R - 1, 2 * WIN_C - 1), 0.1),
        'conv_w': nrm((DEPTH, CONV_W, C_WIDTH), CONV_W ** -0.5),
        'conv_b': nrm((DEPTH, C_WIDTH), 0.02),
        'g_out': 1.0 + nrm((DEPTH, MIX_WIDTH), 0.02),
        'w_out': nrm((DEPTH, MIX_WIDTH, D_MODEL), MIX_WIDTH ** -0.5),
        'ffn_w1': nrm((n_dense, D_MODEL, D_FF), D_MODEL ** -0.5),
        'ffn_w3': nrm((n_dense, D_MODEL, D_FF), D_MODEL ** -0.5),
        'ffn_w2': nrm((n_dense, D_FF, D_MODEL), D_FF ** -0.5),
        'moe_router': nrm((n_moe, D_MODEL, N_EXPERTS), D_MODEL ** -0.5),
        'moe_router_b': nrm((n_moe, N_EXPERTS), 0.01),
        'moe_w1': nrm((n_moe, N_EXPERTS, D_MODEL, D_FF), D_MODEL ** -0.5),
        'moe_w3': nrm((n_moe, N_EXPERTS, D_MODEL, D_FF), D_MODEL ** -0.5),
        'moe_w2': nrm((n_moe, N_EXPERTS, D_FF, D_MODEL), D_FF ** -0.5),
    }


def reference(x, c, ctx, c_ctx, w_mod, b_mod, w_in, gq_a, gk_a, gq_b, gk_b, rpb, conv_w, conv_b,
              g_out, w_out, ffn_w1, ffn_w3, ffn_w2, moe_router, moe_router_b, moe_w1, moe_w3, moe_w2):
    b, s, _ = x.shape
    n_ctx = ctx.shape[1]
    tabs = axial_rope_tables(s)
    silu_c = jax.nn.silu(c)
    silu_cc = jax.nn.silu(c_ctx)
    for l in range(DEPTH):
        last = l == DEPTH - 1
        mx = (silu_c @ w_mod[l] + b_mod[l])[:, None, :]
        mc = silu_cc @ w_mod[l] + b_mod[l]
        sh_a, sc_a, g_a, sh_f, sc_f, g_f = jnp.split(mx, 6, axis=-1)
        csh_a, csc_a, cg_a, csh_f, csc_f, cg_f = jnp.split(mc, 6, axis=-1)

        hx = modulate(x, sh_a, sc_a)
        hc = modulate(ctx, csh_a, csc_a)
        aq, bq, cpost, cpre, cval, ak, av, bk, bv = split_cols(hx @ w_in[l], IN_SIZES)
        if last:
            akc, avc, bkc, bvc = split_cols(hc @ w_in[l][:, KV_START:], KV_SIZES)
        else:
            aqc, bqc, cpostc, cprec, cvalc, akc, avc, bkc, bvc = split_cols(hc @ w_in[l], IN_SIZES)

        qa = axial_rope(rms_norm(aq.reshape(b, s, A_KV_HEADS, A_GROUP, HEAD_DIM), gq_a[l]), tabs)
        ka = axial_rope(rms_norm(ak.reshape(b, s, A_KV_HEADS, HEAD_DIM), gk_a[l]), tabs)
        va = av.reshape(b, s, A_KV_HEADS, HEAD_DIM)
        kac = rms_norm(akc.reshape(b, n_ctx, A_KV_HEADS, HEAD_DIM), gk_a[l])
        vac = avc.reshape(b, n_ctx, A_KV_HEADS, HEAD_DIM)
        oa = global_gqa(qa, ka, va, kac, vac)

        qb_ = rms_norm(bq.reshape(b, s, B_HEADS, HEAD_DIM), gq_b[l])
        kb_ = rms_norm(bk.reshape(b, s, B_HEADS, HEAD_DIM), gk_b[l])
        vb_ = bv.reshape(b, s, B_HEADS, HEAD_DIM)
        kbc = rms_norm(bkc.reshape(b, n_ctx, B_HEADS, HEAD_DIM), gk_b[l])
        vbc = bvc.reshape(b, n_ctx, B_HEADS, HEAD_DIM)
        ob = neighbourhood_attn(qb_, kb_, vb_, kbc, vbc, rpb[l])

        oc = short_conv_mixer(cpost, cpre, cval, conv_w[l], conv_b[l])

        x = x + g_a * merge_groups(oa, ob, oc, g_out[l], w_out[l])

        if not last:
            qac = rms_norm(aqc.reshape(b, n_ctx, A_KV_HEADS, A_GROUP, HEAD_DIM), gq_a[l])
            oac = gqa_attend(qac, kac, vac).reshape(b, n_ctx, A_WIDTH)
            qbc = rms_norm(bqc.reshape(b, n_ctx, B_HEADS, 1, HEAD_DIM), gq_b[l])
            obc = gqa_attend(qbc, kbc, vbc).reshape(b, n_ctx, B_WIDTH)
            occ = short_conv_mixer(cpostc, cprec, cvalc, conv_w[l], conv_b[l])
            ctx = ctx + cg_a * merge_groups(oac, obc, occ, g_out[l], w_out[l])

        hx = modulate(x, sh_f, sc_f)
        if l % 2 == 0:
            i = l // 2
            x = x + g_f * swiglu(hx, ffn_w1[i], ffn_w3[i], ffn_w2[i])
            if not last:
                hc2 = modulate(ctx, csh_f, csc_f)
                ctx = ctx + cg_f * swiglu(hc2, ffn_w1[i], ffn_w3[i], ffn_w2[i])
        else:
            i = l // 2
            x = x + g_f * moe_swiglu(hx, moe_router[i], moe_router_b[i], moe_w1[i], moe_w3[i], moe_w2[i])
            if not last:
                hc2 = modulate(ctx, csh_f, csc_f)
                ctx = ctx + cg_f * moe_swiglu(hc2, moe_router[i], moe_router_b[i], moe_w1[i], moe_w3[i], moe_w2[i])
    return x
```

```python
from contextlib import ExitStack

import numpy as np
import concourse.bass as bass
import concourse.mybir as mybir
from concourse.bass_utils import run_bass_kernel_spmd

F32 = mybir.dt.float32
BF16 = mybir.dt.bfloat16
AF = mybir.ActivationFunctionType
ALU = mybir.AluOpType
AX = mybir.AxisListType

D = 1024
SEQ = 2048
NCTX = 256
NB = 2
DEPTH = 2
GW = 64
HD = 64
INW = 2304
DFF = 2816
NF = DFF // 128
NE = 8
EPS = 1e-6
NEG = -30000.0
LT = SEQ // 128
CT = NCTX // 128
NT = LT + CT
NKEY = SEQ + NCTX

ENGS = ("pe", "act", "dve", "pool", "sp")
SKIP = set()
NROT = 8


class Res:
    __slots__ = ("w", "r")

    def __init__(self):
        self.w = None
        self.r = []


class Op:
    __slots__ = ("eng", "fn", "deps", "dma", "needs_inc", "tok")

    def __init__(self, eng, fn, dma):
        self.eng = eng
        self.fn = fn
        self.dma = dma
        self.deps = []
        self.needs_inc = False
        self.tok = None


class Sched:
    def __init__(self, sems, dsems):
        self.sems = sems
        self.dsems = dsems
        self.cnt = {e: 0 for e in ENGS}
        self.ndma = {e: 0 for e in ENGS}
        self.waited = {e: {} for e in ENGS}
        self.total = 0
        self.reset()

    def reset(self):
        self.ops = {e: [] for e in ENGS}
        self.dma_hist = {e: [] for e in ENGS}

    def op(self, eng, fn, reads=(), writes=(), dma=False):
        o = Op(eng, fn, dma)
        deps = {}
        for r in reads:
            if r.w is not None:
                deps[id(r.w)] = (r.w, True)
        for w in writes:
            if w.w is not None and id(w.w) not in deps:
                deps[id(w.w)] = (w.w, False)
            for rd in w.r:
                if id(rd) not in deps:
                    deps[id(rd)] = (rd, False)
        for p, raw in deps.values():
            if p is o:
                continue
            if p.eng == eng and not p.dma and not dma:
                if eng == "pe" or not raw:
                    continue
            o.deps.append(p)
            p.needs_inc = True
        if dma:
            h = self.dma_hist[eng]
            if len(h) >= NROT:
                o.deps.append(h[-NROT])
            h.append(o)
            o.needs_inc = True
        for r in reads:
            r.r.append(o)
        for w in writes:
            w.w = o
            w.r = []
        self.ops[eng].append(o)
        self.total += 1
        return o

    def finish_block(self):
        tail = []
        for e in ENGS:
            tail += self.dma_hist[e][-NROT:]
        o = Op("sp", lambda e: e.nop(), False)
        o.deps = tail
        self.ops["sp"].append(o)

    def emit(self, block):
        for e in ENGS:
            for o in self.ops[e]:
                if o.dma:
                    nd = self.ndma[e]
                    o.tok = (self.dsems[e][nd % NROT], 16 * (nd // NROT + 1))
                    self.ndma[e] = nd + 1
                elif o.needs_inc:
                    self.cnt[e] += 1
                    o.tok = (self.sems[e], self.cnt[e])

        def run(e, engine):
            waited = self.waited[e]
            for o in self.ops[e]:
                need = {}
                for p in o.deps:
                    s, v = p.tok
                    k = s.num
                    if waited.get(k, 0) >= v:
                        continue
                    if k not in need or need[k][1] < v:
                        need[k] = (s, v)
                for k, (s, v) in need.items():
                    engine.wait_ge(s, v)
                    waited[k] = v
                ins = o.fn(engine)
                if o.dma:
                    ins.then_inc(o.tok[0], 16)
                elif o.needs_inc:
                    ins.then_inc(o.tok[0], 1)

        block.tensor(lambda pe: run("pe", pe))
        block.scalar(lambda act: run("act", act))
        block.vector(lambda dve: run("dve", dve))
        block.gpsimd(lambda pool: run("pool", pool))
        block.sync(lambda sp: run("sp", sp))
        self.reset()


class Ring:
    def __init__(self, K, name, shape, dt, n, psum=False):
        self.t = [(K.ps if psum else K.sb)("%s%d" % (name, i), shape, dt) for i in range(n)]
        self.r = [Res() for _ in range(n)]
        self.i = 0

    def next(self):
        j = self.i % len(self.t)
        self.i += 1
        return self.t[j], self.r[j]


class Kern:
    pass


def build(phases=("mod", "attn0", "ffn0", "attn1", "ffn1"), dbg=False):
    nc = bass.Bass("TRN2", target_bir_lowering=False)
    K = Kern()
    K.nc = nc

    def din(name, shape):
        return nc.dram_tensor(name, list(shape), F32, kind="ExternalInput").ap()

    K.x2 = din("x2", [NB, SEQ, D])
    K.ctx2 = din("ctx2", [NB, NCTX, D])
    K.cvec = din("cvec", [NB + 1, D])
    K.w_mod = din("w_mod", [DEPTH, D, 6 * D])
    K.b_mod = din("b_mod", [DEPTH, 6 * D])
    K.w_in = din("w_in", [DEPTH, D, INW])
    K.gains = din("gains", [DEPTH, 4, HD])
    K.rpbT = din("rpbT", [DEPTH, 15, 64, 4, 64])
    K.conv_w = din("conv_w", [DEPTH, 3, 256])
    K.conv_b = din("conv_b", [DEPTH, 256])
    K.g_out = din("g_out", [DEPTH, D])
    K.w_out = din("w_out", [DEPTH, D, D])
    K.ffn_w1 = din("ffn_w1", [1, D, DFF])
    K.ffn_w3 = din("ffn_w3", [1, D, DFF])
    K.ffn_w2 = din("ffn_w2", [1, DFF, D])
    K.moe_router = din("moe_router", [1, D, NE])
    K.moe_router_b = din("moe_router_b", [1, NE])
    K.moe_w1 = din("moe_w1", [1, NE, D, DFF])
    K.moe_w3 = din("moe_w3", [1, NE, D, DFF])
    K.moe_w2 = din("moe_w2", [1, NE, DFF, D])
    K.ident = din("ident", [128, 128])
    K.rope = din("rope", [NKEY, 128])
    K.y = nc.dram_tensor("y", [NB, SEQ, D], F32, kind="ExternalOutput").ap()
    K.xcur = nc.dram_tensor("xcur", [NB, NKEY, D], F32, kind="Internal").ap()
    K.modv = nc.dram_tensor("modv", [DEPTH, NB + 1, 6 * D], F32, kind="Internal").ap()
    K.czT = nc.dram_tensor("czT", [2, 256, 2308], F32, kind="Internal").ap()
    K.dbg = {}
    if dbg:
        K.dbg["modv"] = nc.dram_tensor("dbg_modv", [DEPTH, NB + 1, 6 * D], F32, kind="ExternalOutput").ap()
        K.dbg["xcur"] = nc.dram_tensor("dbg_xcur", [NB, NKEY, D], F32, kind="ExternalOutput").ap()
        K.dbg["oT"] = nc.dram_tensor("dbg_oT", [NB, D, NKEY], F32, kind="ExternalOutput").ap()

    with ExitStack() as gst:
        sems = {e: gst.enter_context(nc.semaphore("s_" + e)) for e in ENGS}
        dsems = {}
        for e in ("sp", "pool", "act"):
            dsems[e] = [gst.enter_context(nc.semaphore("d_%s%d" % (e, i))) for i in range(NROT)]
        dsems["pe"] = dsems["sp"]
        dsems["dve"] = dsems["sp"]
        K.S = Sched(sems, dsems)
        K.outs = []

        for ph in phases:
            with ExitStack() as st:
                K.st = st
                K.sb = lambda name, shape, dt, st=st, ph=ph: st.enter_context(nc.sbuf_tensor(ph + "_" + name, list(shape), dt))
                K.ps = lambda name, shape, dt, st=st, ph=ph: st.enter_context(nc.psum_tensor(ph + "_" + name, list(shape), dt))
                if ph == "mod":
                    phase_mod(K)
                elif ph.startswith("attn"):
                    phase_attn(K, int(ph[4:]))
                elif ph.startswith("ffn"):
                    phase_ffn(K, int(ph[3:]))
                elif ph == "dbgcopy":
                    phase_dbgcopy(K)
                elif ph == "initx":
                    phase_initx(K)
                K.S.finish_block()
                with nc.Block() as block:
                    K.S.emit(block)
    return nc


def xsrc(K, l, b, t):
    if l == 0:
        if t < LT:
            return K.x2[b, t * 128:(t + 1) * 128, :]
        return K.ctx2[b, (t - LT) * 128:(t - LT + 1) * 128, :]
    return K.xcur[b, t * 128:(t + 1) * 128, :]


def phase_mod(K):
    nc, S = K.nc, K.S
    NV = NB + 1
    cT = K.sb("cT", [128, 8, NV], F32)
    r_cT = Res()
    scT = K.sb("scT", [128, 8, NV], BF16)
    r_scT = Res()
    for v in range(NV):
        S.op("sp", lambda e, v=v: e.dma_start(out=cT[:, :, v], in_=K.cvec[v].rearrange("(k p) -> p k", p=128),
                                              allow_slow_non_contiguous=True), writes=[r_cT], dma=True)
    S.op("act", lambda e: e.activation(out=scT[:], in_=cT[:], func=AF.Silu), reads=[r_cT], writes=[r_scT])
    wring = Ring(K, "wm", [128, 8, 512], BF16, 3)
    bring = Ring(K, "bm", [NV, 512], F32, 2)
    oring = Ring(K, "om", [NV, 512], F32, 2)
    pring = Ring(K, "pm", [NV, 512], F32, 2, psum=True)
    for l in range(DEPTH):
        for cb in range(12):
            wt, wr = wring.next()
            S.op("pool", lambda e, wt=wt, l=l, cb=cb: e.dma_start(
                out=wt[:], in_=K.w_mod[l, :, cb * 512:(cb + 1) * 512].rearrange("(k p) c -> p k c", p=128)),
                writes=[wr], dma=True)
            bt, br = bring.next()
            S.op("sp", lambda e, bt=bt, l=l, cb=cb: e.dma_start(
                out=bt[:], in_=K.b_mod[l, cb * 512:(cb + 1) * 512].partition_broadcast(NV)),
                writes=[br], dma=True)
            pt, pr = pring.next()
            for k in range(8):
                S.op("pe", lambda e, pt=pt, wt=wt, k=k: e.matmul(pt[:], lhsT=scT[:, k, :], rhs=wt[:, k, :],
                                                                 start=(k == 0), stop=(k == 7)),
                     reads=[r_scT, wr], writes=[pr])
            ot, orr = oring.next()
            S.op("dve", lambda e, ot=ot, pt=pt, bt=bt: e.tensor_tensor(out=ot[:], in0=pt[:], in1=bt[:], op=ALU.add),
                 reads=[pr, br], writes=[orr])
            S.op("sp", lambda e, ot=ot, l=l, cb=cb: e.dma_start(out=K.modv[l, :, cb * 512:(cb + 1) * 512], in_=ot[:]),
                 reads=[orr], writes=[Res()], dma=True)
            if "modv" in K.dbg:
                S.op("sp", lambda e, ot=ot, l=l, cb=cb: e.dma_start(
                    out=K.dbg["modv"][l, :, cb * 512:(cb + 1) * 512], in_=ot[:]),
                    reads=[orr], writes=[Res()], dma=True)


def load_modcols(K, l, v, idx, name, plus1):
    S = K.S
    t = K.sb(name, [128, 8], F32)
    r = Res()
    S.op("sp", lambda e: e.dma_start(out=t[:], in_=K.modv[l, v, idx * D:(idx + 1) * D].rearrange("(k p) -> p k", p=128),
                                     allow_slow_non_contiguous=True), writes=[r], dma=True)
    if plus1:
        S.op("dve", lambda e: e.tensor_scalar_add(out=t[:], in0=t[:], scalar1=1.0), reads=[r], writes=[r])
    return t, r


def load_bcast(K, src_row, n, name, dt=F32, eng="sp"):
    S = K.S
    t = K.sb(name, [128, n], dt)
    r = Res()
    S.op(eng, lambda e: e.dma_start(out=t[:], in_=src_row.partition_broadcast(128)), writes=[r], dma=True)
    return t, r


def phase_ffn(K, l):
    nc, S = K.nc, K.S
    moe = (l % 2 == 1)
    last = (l == DEPTH - 1)
    E = NE if moe else 1
    if moe:
        W1, W3, W2 = K.moe_w1[0], K.moe_w3[0], K.moe_w2[0]
    else:
        W1, W3, W2 = K.ffn_w1, K.ffn_w3, K.ffn_w2

    ident = K.sb("identf", [128, 128], F32)
    r_ident = Res()
    S.op("sp", lambda e: e.dma_start(out=ident[:], in_=K.ident), writes=[r_ident], dma=True)

    mods = {}
    for v in range(NB + 1):
        if v == NB and last:
            continue
        sh, r_sh = load_modcols(K, l, v, 3, "shf%d" % v, False)
        sc, r_sc = load_modcols(K, l, v, 4, "scf%d" % v, True)
        gf, r_gf = load_bcast(K, K.modv[l, v, 5 * D:6 * D], D, "gf%d" % v)
        mods[v] = (sh, r_sh, sc, r_sc, gf, r_gf)
    if moe:
        wr_t = K.sb("wrt", [128, 8, NE], F32)
        r_wr = Res()
        S.op("sp", lambda e: e.dma_start(out=wr_t[:], in_=K.moe_router[0].rearrange("(k p) e -> p k e", p=128)),
             writes=[r_wr], dma=True)
        br_t, r_br = load_bcast(K, K.moe_router_b[0], NE, "brt")

    TB = 8
    hT = K.sb("hT", [128, 8, TB * 128], BF16)
    r_hT = [Res() for _ in range(TB)]
    uT = K.sb("uT", [128, NF, TB * 128], BF16)
    r_uT = [[Res() for _ in range(2)] for _ in range(NF)]
    acc = K.sb("acc", [128, TB, D], F32)
    r_acc = [[Res(), Res()] for _ in range(TB)]
    G = K.sb("G", [128, TB, NE], F32)
    r_G = [Res() for _ in range(TB)]
    w2b = K.sb("w2b", [128, NF, D], BF16)
    r_w2 = [Res() for _ in range(NF)]
    xring = Ring(K, "xt", [128, D], F32, 2)
    nring = Ring(K, "xn", [128, D], F32, 2)
    jring = Ring(K, "jk", [128, D], BF16, 1)
    sring = Ring(K, "st", [128, 4], F32, 3)
    hfring = Ring(K, "hf", [128, 8, 128], F32, 2)
    w13ring = Ring(K, "w13", [128, 2, 8, 128], BF16, 3)
    sgring = Ring(K, "sg", [128, 512], F32, 2)
    tring = Ring(K, "tp", [128, 4, 128], F32, 1, psum=True)
    lgring = Ring(K, "lg", [128, NE], F32, 1, psum=True)
    gvring = Ring(K, "gv", [128, 2, 512], F32, 2, psum=True)
    oring = Ring(K, "op", [128, 512], F32, 2, psum=True)
    smring = Ring(K, "sm", [128, 4 * NE + 8], F32, 2)
    yring = Ring(K, "yt", [128, D], F32, 2)

    blocks = []
    for b in range(NB):
        blocks.append((b, b, 0, 8))
        blocks.append((b, b, 8, 8))
        if not last:
            blocks.append((b, NB, LT, CT))

    def router(ti, lg, lr):
        sm, mr = smring.next()
        L0, E1, L2, E2, M = sm[:, 0:8], sm[:, 8:16], sm[:, 16:24], sm[:, 24:32], sm[:, 32:40]
        seq = [
            lambda e: e.tensor_tensor(out=L0, in0=lg[:], in1=br_t[:], op=ALU.add),
            lambda e: e.reduce_max(out=M[:, 0:1], in_=L0, axis=AX.X),
            lambda e: e.tensor_scalar(out=E1, in0=L0, scalar1=M[:, 0:1], scalar2=None, op0=ALU.is_equal),
            lambda e: e.scalar_tensor_tensor(out=L2, in0=E1, scalar=-1e30, in1=L0, op0=ALU.mult, op1=ALU.add),
            lambda e: e.reduce_max(out=M[:, 1:2], in_=L2, axis=AX.X),
            lambda e: e.tensor_scalar(out=E2, in0=L2, scalar1=M[:, 1:2], scalar2=None, op0=ALU.is_equal),
            lambda e: e.tensor_tensor(out=M[:, 2:3], in0=M[:, 1:2], in1=M[:, 0:1], op=ALU.subtract),
        ]
        for i, fn in enumerate(seq):
            S.op("dve", (lambda fn: (lambda e: fn(e)))(fn), reads=[mr, lr, r_br] if i == 0 else [mr], writes=[mr])
        S.op("act", lambda e, M=M: e.activation(out=M[:, 3:4], in_=M[:, 2:3], func=AF.Exp), reads=[mr], writes=[mr])
        seq2 = [
            lambda e: e.tensor_scalar_add(out=M[:, 4:5], in0=M[:, 3:4], scalar1=1.0),
            lambda e: e.reciprocal(out=M[:, 5:6], in_=M[:, 4:5]),
            lambda e: e.tensor_tensor(out=M[:, 6:7], in0=M[:, 3:4], in1=M[:, 5:6], op=ALU.mult),
            lambda e: e.tensor_scalar(out=E1, in0=E1, scalar1=M[:, 5:6], scalar2=None, op0=ALU.mult),
        ]
        for fn in seq2:
            S.op("dve", (lambda fn: (lambda e: fn(e)))(fn), reads=[mr], writes=[mr])
        S.op("dve", lambda e, E1=E1, E2=E2, M=M, ti=ti: e.scalar_tensor_tensor(
            out=G[:, ti, :], in0=E2, scalar=M[:, 6:7], in1=E1, op0=ALU.mult, op1=ALU.add),
            reads=[mr], writes=[r_G[ti]])


    def ffn_block(b, v, t0, ntl):
        sh, r_sh, sc, r_sc, gf, r_gf = mods[v]
        for ti in range(ntl):
            t = t0 + ti
            xt, xr = xring.next()
            S.op("sp", lambda e, xt=xt, t=t: e.dma_start(out=xt[:], in_=xsrc(K, 1, b, t)), writes=[xr], dma=True)
            jk, jr = jring.next()
            stt, sr = sring.next()
            S.op("act", lambda e, jk=jk, xt=xt, stt=stt: e.activation(out=jk[:], in_=xt[:], func=AF.Square,
                                                                     scale=float(D ** -0.5), accum_out=stt[:, 0:1]),
                 reads=[xr], writes=[jr, sr])
            S.op("act", lambda e, stt=stt: e.activation(out=stt[:, 1:2], in_=stt[:, 0:1], func=AF.Sqrt, bias=EPS, scale=1.0),
                 reads=[sr], writes=[sr])
            S.op("dve", lambda e, stt=stt: e.reciprocal(out=stt[:, 2:3], in_=stt[:, 1:2]), reads=[sr], writes=[sr])
            xn, nr = nring.next()
            S.op("dve", lambda e, xn=xn, xt=xt, stt=stt: e.tensor_scalar(out=xn[:], in0=xt[:], scalar1=stt[:, 2:3],
                                                                       scalar2=None, op0=ALU.mult),
                 reads=[xr, sr], writes=[nr])
            hf, hr = hfring.next()
            for hh in range(2):
                tp, tr = tring.next()
                for kk in range(4):
                    k = hh * 4 + kk
                    S.op("pe", lambda e, tp=tp, xn=xn, k=k, kk=kk: e.transpose(out=tp[:, kk, :], in_=xn[:, k * 128:(k + 1) * 128],
                                                                             identity=ident[:]),
                         reads=[nr, r_ident], writes=[tr])
                for kk in range(4):
                    k = hh * 4 + kk
                    if moe:
                        S.op("dve", lambda e, hf=hf, tp=tp, k=k, kk=kk: e.tensor_scalar(
                            out=hf[:, k, :], in0=tp[:, kk, :], scalar1=sc[:, k:k + 1], scalar2=sh[:, k:k + 1],
                            op0=ALU.mult, op1=ALU.add), reads=[tr, r_sc, r_sh], writes=[hr])
                    else:
                        S.op("dve", lambda e, tp=tp, k=k, kk=kk, ti=ti: e.tensor_scalar(
                            out=hT[:, k, ti * 128:(ti + 1) * 128], in0=tp[:, kk, :], scalar1=sc[:, k:k + 1],
                            scalar2=sh[:, k:k + 1], op0=ALU.mult, op1=ALU.add),
                            reads=[tr, r_sc, r_sh], writes=[r_hT[ti]])
            if moe:
                S.op("pool", lambda e, hf=hf, ti=ti: e.tensor_copy(out=hT[:, :, ti * 128:(ti + 1) * 128], in_=hf[:]),
                     reads=[hr], writes=[r_hT[ti]])
                lg, lr = lgring.next()
                for k in range(8):
                    S.op("pe", lambda e, lg=lg, hf=hf, k=k: e.matmul(lg[:], lhsT=hf[:, k, :], rhs=wr_t[:, k, :],
                                                                     start=(k == 0), stop=(k == 7)),
                         reads=[hr, r_wr], writes=[lr])
                router(ti, lg, lr)
        NTOK = ntl * 128
        halves = [(h0, min(512, NTOK - h0)) for h0 in range(0, NTOK, 512)]
        for ex in range(E):
            w1e = W1[ex] if moe else W1[0]
            w3e = W3[ex] if moe else W3[0]
            w2e = W2[ex] if moe else W2[0]
            for f in range(NF):
                wt, wr = w13ring.next()
                S.op("pool", lambda e, wt=wt, w1e=w1e, f=f: e.dma_start(
                    out=wt[:, 0, :, :], in_=w1e[:, f * 128:(f + 1) * 128].rearrange("(k p) c -> p k c", p=128)),
                    writes=[wr], dma=True)
                r2 = Res()
                S.op("pool", lambda e, wt=wt, w3e=w3e, f=f: e.dma_start(
                    out=wt[:, 1, :, :], in_=w3e[:, f * 128:(f + 1) * 128].rearrange("(k p) c -> p k c", p=128)),
                    writes=[r2], dma=True)
                S.op("pool", lambda e, w2e=w2e, f=f: e.dma_start(out=w2b[:, f, :], in_=w2e[f * 128:(f + 1) * 128, :]),
                     writes=[r_w2[f]], dma=True)
                for hi, (h0, hn) in enumerate(halves):
                    gv, gr = gvring.next()
                    tiles_in = list(range(h0 // 128, (h0 + hn) // 128))
                    for j in range(2):
                        for k in range(8):
                            S.op("pe", lambda e, gv=gv, wt=wt, j=j, k=k, h0=h0, hn=hn: e.matmul(
                                gv[:, j, 0:hn], lhsT=wt[:, j, k, :], rhs=hT[:, k, h0:h0 + hn], start=(k == 0), stop=(k == 7)),
                                reads=[wr, r2] + [r_hT[i] for i in tiles_in], writes=[gr])
                    sg, sr2 = sgring.next()
                    S.op("act", lambda e, sg=sg, gv=gv, hn=hn: e.activation(out=sg[:, 0:hn], in_=gv[:, 0, 0:hn], func=AF.Silu),
                         reads=[gr], writes=[sr2])
                    S.op("dve", lambda e, sg=sg, gv=gv, f=f, h0=h0, hn=hn: e.tensor_tensor(
                        out=uT[:, f, h0:h0 + hn], in0=gv[:, 1, 0:hn], in1=sg[:, 0:hn], op=ALU.mult),
                        reads=[gr, sr2], writes=[r_uT[f][hi]])
            for ti in range(ntl):
                for cb in range(2):
                    ot, orr = oring.next()
                    for f in range(NF):
                        S.op("pe", lambda e, ot=ot, f=f, ti=ti, cb=cb: e.matmul(
                            ot[:], lhsT=uT[:, f, ti * 128:(ti + 1) * 128], rhs=w2b[:, f, cb * 512:(cb + 1) * 512],
                            start=(f == 0), stop=(f == NF - 1)),
                            reads=[r_uT[f][ti // 4], r_w2[f]], writes=[orr])
                    asl = acc[:, ti, cb * 512:(cb + 1) * 512]
                    if not moe:
                        S.op("dve", lambda e, asl=asl, ot=ot: e.tensor_copy(out=asl, in_=ot[:]),
                             reads=[orr], writes=[r_acc[ti][cb]])
                    elif ex == 0:
                        S.op("dve", lambda e, asl=asl, ot=ot, ti=ti, ex=ex: e.tensor_scalar(
                            out=asl, in0=ot[:], scalar1=G[:, ti, ex:ex + 1], scalar2=None, op0=ALU.mult),
                            reads=[orr, r_G[ti]], writes=[r_acc[ti][cb]])
                    else:
                        S.op("dve", lambda e, asl=asl, ot=ot, ti=ti, ex=ex: e.scalar_tensor_tensor(
                            out=asl, in0=ot[:], scalar=G[:, ti, ex:ex + 1], in1=asl, op0=ALU.mult, op1=ALU.add),
                            reads=[orr, r_G[ti], r_acc[ti][cb]], writes=[r_acc[ti][cb]])
        for ti in range(ntl):
            t = t0 + ti
            xt, xr = xring.next()
            S.op("sp", lambda e, xt=xt, t=t: e.dma_start(out=xt[:], in_=xsrc(K, 1, b, t)), writes=[xr], dma=True)
            yt, yr = yring.next()
            S.op("pool", lambda e, yt=yt, ti=ti: e.tensor_tensor(out=yt[:], in0=acc[:, ti, :], in1=gf[:], op=ALU.mult),
                 reads=[r_acc[ti][0], r_acc[ti][1], r_gf], writes=[yr])
            S.op("dve", lambda e, yt=yt, xt=xt: e.tensor_tensor(out=yt[:], in0=yt[:], in1=xt[:], op=ALU.add),
                 reads=[yr, xr], writes=[yr])
            if last:
                dst = K.y[b, t * 128:(t + 1) * 128, :]
            else:
                dst = K.xcur[b, t * 128:(t + 1) * 128, :]
            S.op("sp", lambda e, yt=yt, dst=dst: e.dma_start(out=dst, in_=yt[:]), reads=[yr], writes=[Res()], dma=True)

    for blk in blocks:
        ffn_block(*blk)


def phase_dbgcopy(K):
    S = K.S
    ring = Ring(K, "dc", [128, D], F32, 2)
    for b in range(NB):
        for t in range(NT):
            tt, tr = ring.next()
            S.op("sp", lambda e, tt=tt, b=b, t=t: e.dma_start(out=tt[:], in_=K.xcur[b, t * 128:(t + 1) * 128, :]),
                 writes=[tr], dma=True)
            S.op("sp", lambda e, tt=tt, b=b, t=t: e.dma_start(out=K.dbg["xcur"][b, t * 128:(t + 1) * 128, :], in_=tt[:]),
                 reads=[tr], writes=[Res()], dma=True)


def phase_initx(K):
    S = K.S
    ring = Ring(K, "ix", [128, D], F32, 2)
    for b in range(NB):
        for t in range(NT):
            tt, tr = ring.next()
            S.op("sp", lambda e, tt=tt, b=b, t=t: e.dma_start(out=tt[:], in_=xsrc(K, 0, b, t)), writes=[tr], dma=True)
            S.op("sp", lambda e, tt=tt, b=b, t=t: e.dma_start(out=K.xcur[b, t * 128:(t + 1) * 128, :], in_=tt[:]),
                 reads=[tr], writes=[Res()], dma=True)


def phase_attn(K, l):
    nc, S = K.nc, K.S
    last = (l == DEPTH - 1)
    op = S.op
    ZW = 2308

    identb = K.sb("identb", [128, 128], BF16)
    r_id = Res()
    op("pool", lambda e: e.dma_start(out=identb[:], in_=K.ident), writes=[r_id], dma=True)
    Wt = K.sb("Wt", [128, 8 * INW], BF16)
    w_in = Wt[:, :].rearrange("p (k c) -> p k c", k=8)
    woA = Wt[0:64, 0:8 * D].rearrange("p (h c) -> p h c", h=8)
    woB = Wt[0:64, 8 * D:12 * D].rearrange("p (h c) -> p h c", h=4)
    woC = Wt[:, 12 * D:14 * D].rearrange("p (h c) -> p h c", h=2)
    r_win = [Res() for _ in range(8)]
    gcol = K.sb("gcol", [128, 16], F32)
    r_gc = Res()
    op("sp", lambda e: e.dma_start(out=gcol[0:64, 0:8], in_=K.g_out[l, 0:512].rearrange("(h d) -> d h", d=64),
                                   allow_slow_non_contiguous=True), writes=[r_gc], dma=True)
    op("sp", lambda e: e.dma_start(out=gcol[0:64, 8:12], in_=K.g_out[l, 512:768].rearrange("(h d) -> d h", d=64),
                                   allow_slow_non_contiguous=True), writes=[r_gc], dma=True)
    op("sp", lambda e: e.dma_start(out=gcol[:, 12:14], in_=K.g_out[l, 768:1024].rearrange("(k p) -> p k", p=128),
                                   allow_slow_non_contiguous=True), writes=[r_gc], dma=True)
    stg = Ring(K, "wstg", [128, D], F32, 2)

    def load_win():
        for k in range(8):
            op("pool", lambda e, k=k: e.dma_start(out=w_in[:, k, :], in_=K.w_in[l, k * 128:(k + 1) * 128, :]),
               writes=[r_win[k]], dma=True)

    def load_wout():
        pieces = [(woA, h, 64, h * 64, h) for h in range(8)] + [(woB, h, 64, 512 + h * 64, 8 + h) for h in range(4)] + \
                 [(woC, k, 128, 768 + k * 128, 12 + k) for k in range(2)]
        for (dst, idx, np_, row0, gc) in pieces:
            st_, sr_ = stg.next()
            op("sp", lambda e, st_=st_, np_=np_, row0=row0: e.dma_start(out=st_[0:np_, :], in_=K.w_out[l, row0:row0 + np_, :]),
               writes=[sr_], dma=True)
            op("dve", lambda e, st_=st_, dst=dst, idx=idx, np_=np_, gc=gc: e.tensor_scalar(
                out=dst[0:np_, idx, :], in0=st_[0:np_, :], scalar1=gcol[0:np_, gc:gc + 1], scalar2=None, op0=ALU.mult),
                reads=[sr_, r_gc], writes=r_win)

    gn = K.sb("gn", [128, 4, 64], F32)
    gsw = K.sb("gsw", [128, 2, 64], F32)
    r_gn = Res()
    for i in range(4):
        op("sp", lambda e, i=i: e.dma_start(out=gn[:, i, :], in_=K.gains[l, i].partition_broadcast(128)), writes=[r_gn], dma=True)
    for i in range(2):
        for (d0, s0) in ((0, 16), (16, 0), (32, 48), (48, 32)):
            op("sp", lambda e, i=i, d0=d0, s0=s0: e.dma_start(out=gsw[:, i, d0:d0 + 16],
                                                              in_=K.gains[l, i, s0:s0 + 16].partition_broadcast(128)),
               writes=[r_gn], dma=True)
    tbl = K.sb("tbl", [128, 16, 4, 64], BF16)
    r_tbl = Res()
    for dr in range(15):
        for s_ in range(2):
            op("pool", lambda e, dr=dr, s_=s_: e.dma_start(out=tbl[s_ * 64:(s_ + 1) * 64, dr, :, :], in_=K.rpbT[l, dr]),
               writes=[r_tbl], dma=True)
    op("pool", lambda e: e.memset(tbl[:, 15, :, :], NEG), writes=[r_tbl])
    cwc = K.sb("cwc", [128, 2, 4], F32)
    r_cw = Res()
    for w in range(3):
        op("sp", lambda e, w=w: e.dma_start(out=cwc[:, :, w], in_=K.conv_w[l, w].rearrange("(k p) -> p k", p=128),
                                            allow_slow_non_contiguous=True), writes=[r_cw], dma=True)
    op("sp", lambda e: e.dma_start(out=cwc[:, :, 3], in_=K.conv_b[l].rearrange("(k p) -> p k", p=128),
                                   allow_slow_non_contiguous=True), writes=[r_cw], dma=True)
    ones_b = K.sb("ones_b", [128, 2], BF16)
    ones_f = K.sb("ones_f", [128, 64], F32)
    zt = K.sb("zt", [128, 2, 4], F32)
    r_one = Res()
    op("pool", lambda e: e.memset(ones_b[:], 1.0), writes=[r_one])
    op("pool", lambda e: e.memset(ones_f[:], 1.0), writes=[r_one])
    op("pool", lambda e: e.memset(zt[:], 0.0), writes=[r_one])
    r_cz = Res()
    for a in range(2):
        for (c0, n) in ((0, 1), (2049, 2), (2307, 1)):
            op("sp", lambda e, a=a, c0=c0, n=n: e.dma_start(
                out=K.czT[a, :, c0:c0 + n].rearrange("(k p) c -> p k c", p=128), in_=zt[:, :, 0:n], allow_slow_non_contiguous=True),
                reads=[r_one], writes=[r_cz], dma=True)
    mods = {}
    for v in range(NB + 1):
        sh, r_sh = load_modcols(K, l, v, 0, "sha%d" % v, False)
        sc, r_sc = load_modcols(K, l, v, 1, "sca%d" % v, True)
        mods[v] = (sh, r_sh, sc, r_sc)
    garing = Ring(K, "ga", [128, D], F32, 1)

    qTa = K.sb("qTa", [128, 4, NKEY], BF16)
    kTa = K.sb("kTa", [128, NKEY], BF16)
    Va = K.sb("Va", [128, NT, 2, 65], BF16)
    qTb = K.sb("qTb", [128, 2, NKEY], BF16)
    kTb = K.sb("kTb", [128, 2, NKEY], BF16)
    Vb = K.sb("Vb", [128, NT, 4, 65], BF16)
    r_q = [Res() for _ in range(NT)]
    op("pool", lambda e: e.memset(Va[:, :, :, 64:65], 1.0), writes=r_q)
    op("pool", lambda e: e.memset(Vb[:, :, :, 64:65], 1.0), writes=r_q)

    bk = [K.ps("bk%d" % i, [128, 512], F32) for i in range(8)]
    r_bk = [Res() for _ in range(8)]
    tpA = bk[6][:, :].bitcast(BF16).rearrange("p (k c) -> p k c", k=8)
    tpQ = bk[7][:, :].bitcast(BF16).rearrange("p (k c) -> p k c", k=8)
    r_tpA, r_tpQ = r_bk[6], r_bk[7]
    ST = [(bk[0], r_bk[0]), (bk[1], r_bk[1])]
    ST4 = [(bk[0], r_bk[0]), (bk[1], r_bk[1]), (bk[6], r_bk[6]), (bk[7], r_bk[7])]
    OB = [(bk[2], r_bk[2]), (bk[3], r_bk[3])]
    OP_, r_OP = bk[4], r_bk[4]
    OPR = [(bk[4], r_bk[4]), (bk[6], r_bk[6]), (bk[7], r_bk[7])]
    BC, r_BC = bk[5], r_bk[5]
    r_SS = Res()

    xring = Ring(K, "xa", [128, D], F32, 2)
    sring = Ring(K, "sa", [128, 4], F32, 3)
    nring = Ring(K, "na", [128, D], BF16, 1)
    hT = K.sb("hTa", [128, 8, 512], BF16)
    r_hT = [Res() for _ in range(4)]
    rpring = Ring(K, "rp", [128, 128], F32, 2)
    tabring = Ring(K, "tb", [128, 4, 64], F32, 2)
    q32ring = Ring(K, "q32", [128, 512], F32, 2)
    sqring = Ring(K, "sq32", [128, 512], F32, 1)
    t1ring = Ring(K, "t1", [128, 512], F32, 1)
    t2ring = Ring(K, "t2", [128, 512], F32, 1)
    smring = Ring(K, "sma", [128, 32], F32, 3)
    qbring = Ring(K, "qb", [128, 512], BF16, 2)
    czring = Ring(K, "cz", [128, 512], F32, 3)
    ptring = Ring(K, "pt", [128, 512], BF16, 4)
    sbring = Ring(K, "sbb", [128, 256], F32, 2)
    ptbring = Ring(K, "ptb", [128, 256], BF16, 16)
    rdring = Ring(K, "rd", [128, 512], F32, 1)
    bcring = Ring(K, "bcs", [64, 512], F32, 1)
    sqbring = Ring(K, "sqb", [128, 512], BF16, 2)
    sqcring = Ring(K, "sqc", [128, 512], BF16, 1)
    for i_ in range(2):
        op("pool", lambda e, i_=i_: e.memset(sqbring.t[i_][:], 0.0), writes=[sqbring.r[i_]])
    OaT = K.sb("OaT", [64, 8, 512], BF16)
    ObT = K.sb("ObT", [64, 4, 512], BF16)
    ocT = K.sb("ocT", [128, 2, 512], BF16)
    r_Oa = [Res() for _ in range(8)]
    r_Ob = [Res() for _ in range(4)]
    r_Oc = [Res() for _ in range(2)]
    zring = Ring(K, "zz", [128, 516], F32, 1)
    cpring = Ring(K, "cp", [128, 512], F32, 1)
    yring = Ring(K, "ya", [128, 512], F32, 1)
    accring = Ring(K, "aca", [128, D], F32, 1)
    rsring = Ring(K, "rsa", [128, 16], F32, 2)

    mctr = [0]

    def zcol(t):
        return 1 + t * 128 if t < LT else 2051 + (t - LT) * 128

    def qk_norm(src, r_src, nh, gi, rope_tab, dst, r_dst, perm):
        n = nh * 64
        sq, sqr = sqring.next()
        sm, smr = smring.next()
        op("act", lambda e: e.activation(out=sq[:, 0:n], in_=src, func=AF.Square), reads=[r_src], writes=[sqr])
        op("dve", lambda e: e.tensor_reduce(out=sm[:, 0:nh], in_=sq[:, 0:n].rearrange("p (h d) -> p h d", d=64),
                                            axis=AX.X, op=ALU.add), reads=[sqr], writes=[smr])
        op("act", lambda e: e.activation(out=sm[:, 8:8 + nh], in_=sm[:, 0:nh], func=AF.Sqrt, bias=EPS, scale=1.0 / 64),
           reads=[smr], writes=[smr])
        op("dve", lambda e: e.reciprocal(out=sm[:, 16:16 + nh], in_=sm[:, 8:8 + nh]), reads=[smr], writes=[smr])
        rq = sm[:, 16:16 + nh].unsqueeze(2).to_broadcast([128, nh, 64])
        s3 = src.rearrange("p (h d) -> p h d", d=64)
        t1, t1r = t1ring.next()
        t13 = t1[:, 0:n].rearrange("p (h d) -> p h d", d=64)
        if rope_tab is not None:
            tab, tabr, ci = rope_tab
            cosb = tab[:, ci, :].unsqueeze(1).to_broadcast([128, nh, 64])
            op("pool", lambda e: e.tensor_tensor(out=t13, in0=s3, in1=cosb, op=ALU.mult), reads=[r_src, tabr], writes=[t1r])
            t2, t2r = t2ring.next()
            s5 = src.rearrange("p (h a s d) -> p h a s d", a=2, s=2, d=16)
            t25 = t2[:, 0:n].rearrange("p (h a s d) -> p h a s d", a=2, s=2, d=16)
            sn5 = tab[:, ci + 1, :].rearrange("p (a s d) -> p a s d", a=2, s=2, d=16)
            for s_ in range(2):
                for a in range(2):
                    snb = sn5[:, a, s_, :].unsqueeze(1).to_broadcast([128, nh, 16])
                    op("dve", lambda e, s_=s_, a=a, snb=snb: e.tensor_tensor(
                        out=t25[:, :, a, s_, :], in0=s5[:, :, a, 1 - s_, :], in1=snb, op=ALU.mult),
                        reads=[r_src, tabr], writes=[t2r])
            op("pool", lambda e: e.tensor_tensor(out=t1[:, 0:n], in0=t1[:, 0:n], in1=t2[:, 0:n], op=ALU.add),
               reads=[t1r, t2r], writes=[t1r])
        else:
            gb = gn[:, gi, :].unsqueeze(1).to_broadcast([128, nh, 64])
            op("pool", lambda e: e.tensor_tensor(out=t13, in0=s3, in1=gb, op=ALU.mult), reads=[r_src, r_gn], writes=[t1r])
        if perm:
            dv = dst.rearrange("p (j g d) -> p g j d", j=4, g=2)
            iv = t1[:, 0:n].rearrange("p (g j d) -> p g j d", j=4, g=2)
            rv = sm[:, 16:16 + nh].rearrange("p (g j) -> p g j", g=2).unsqueeze(3).to_broadcast([128, 2, 4, 64])
            op("dve", lambda e: e.tensor_tensor(out=dv, in0=iv, in1=rv, op=ALU.mult), reads=[t1r, smr], writes=[r_dst])
        else:
            op("dve", lambda e: e.tensor_tensor(out=dst.rearrange("p (h d) -> p h d", d=64), in0=t13, in1=rq, op=ALU.mult),
               reads=[t1r, smr], writes=[r_dst])

    def pre_tile(b, t, ti):
        v = b if t < LT else NB
        sh, r_sh, sc, r_sc = mods[v]
        need_q = not (last and t >= LT)
        xt, xr = xring.next()
        op("sp", lambda e: e.dma_start(out=xt[:], in_=xsrc(K, l, b, t)), writes=[xr], dma=True)
        xn, nr = nring.next()
        stt, sr = sring.next()
        op("act", lambda e: e.activation(out=xn[:], in_=xt[:], func=AF.Square, scale=float(D ** -0.5), accum_out=stt[:, 0:1]),
           reads=[xr], writes=[nr, sr])
        op("act", lambda e: e.activation(out=stt[:, 1:2], in_=stt[:, 0:1], func=AF.Sqrt, bias=EPS, scale=1.0), reads=[sr], writes=[sr])
        op("dve", lambda e: e.reciprocal(out=stt[:, 2:3], in_=stt[:, 1:2]), reads=[sr], writes=[sr])
        op("dve", lambda e: e.tensor_scalar(out=xn[:], in0=xt[:], scalar1=stt[:, 2:3], scalar2=None, op0=ALU.mult),
           reads=[xr, sr], writes=[nr])
        for k in range(8):
            op("pe", lambda e, k=k: e.transpose(out=tpA[:, k, :], in_=xn[:, k * 128:(k + 1) * 128], identity=identb[:]),
               reads=[nr, r_id], writes=[r_tpA])
        for k in range(8):
            if k % 2 == 0:
                op("act", lambda e, k=k: e.activation(out=hT[:, k, ti * 128:(ti + 1) * 128], in_=tpA[:, k, :], func=AF.Identity,
                                                      bias=sh[:, k:k + 1], scale=sc[:, k:k + 1]),
                   reads=[r_tpA, r_sh, r_sc], writes=[r_hT[ti]])
            else:
                op("dve", lambda e, k=k: e.tensor_scalar(out=hT[:, k, ti * 128:(ti + 1) * 128], in0=tpA[:, k, :],
                                                         scalar1=sc[:, k:k + 1], scalar2=sh[:, k:k + 1], op0=ALU.mult, op1=ALU.add),
                   reads=[r_tpA, r_sh, r_sc], writes=[r_hT[ti]])
        rp, rpr = rpring.next()
        op("sp", lambda e: e.dma_start(out=rp[:], in_=K.rope[t * 128:(t + 1) * 128, :]), writes=[rpr], dma=True)
        tab, tabr = tabring.next()
        for i in range(2):
            op("pool", lambda e, i=i: e.tensor_tensor(out=tab[:, 2 * i, :], in0=rp[:, 0:64], in1=gn[:, i, :], op=ALU.mult),
               reads=[rpr, r_gn], writes=[tabr])
            op("pool", lambda e, i=i: e.tensor_tensor(out=tab[:, 2 * i + 1, :], in0=rp[:, 64:128], in1=gsw[:, i, :], op=ALU.mult),
               reads=[rpr, r_gn], writes=[tabr])
        tsl = slice(t * 128, (t + 1) * 128)
        if "noproj" in SKIP:
            return

        def proj(c0, n, pb, prr):
            for k in range(8):
                op("pe", lambda e, k=k: e.matmul(pb[:, 0:n], lhsT=hT[:, k, ti * 128:(ti + 1) * 128], rhs=w_in[:, k, c0:c0 + n],
                                                 start=(k == 0), stop=(k == 7)),
                   reads=[r_hT[ti], r_win[k]], writes=[prr])
            q32, q32r = q32ring.next()
            op("act", lambda e: e.activation(out=q32[:, 0:n], in_=pb[:, 0:n], func=AF.Copy), reads=[prr], writes=[q32r])
            return q32, q32r

        if need_q:
            pb, prr = OB[0]
            q32, q32r = proj(0, 512, pb, prr)
            qb_, qbr = qbring.next()
            qk_norm(q32[:, 0:512], q32r, 8, 0, (tab, tabr, 0), qb_[:, 0:512], qbr, True)
            for j in range(4):
                op("pe", lambda e, j=j, qb_=qb_: e.transpose(out=tpQ[:, j, :], in_=qb_[:, j * 128:(j + 1) * 128], identity=identb[:]),
                   reads=[qbr, r_id], writes=[r_tpQ])
            op("dve", lambda e: e.tensor_copy(out=qTa[:, :, tsl], in_=tpQ[:, 0:4, :]), reads=[r_tpQ], writes=[r_q[t]])
            pb, prr = OB[1]
            q32, q32r = proj(512, 256, pb, prr)
            qb_, qbr = qbring.next()
            qk_norm(q32[:, 0:256], q32r, 4, 2, None, qb_[:, 0:256], qbr, False)
            for j in range(2):
                op("pe", lambda e, j=j, qb_=qb_: e.transpose(out=tpQ[:, 4 + j, :], in_=qb_[:, j * 128:(j + 1) * 128], identity=identb[:]),
                   reads=[qbr, r_id], writes=[r_tpQ])
            op("dve", lambda e: e.tensor_copy(out=qTb[:, :, tsl], in_=tpQ[:, 4:6, :]), reads=[r_tpQ], writes=[r_q[t]])
        pb, prr = OB[0]
        k32, k32r = proj(1536, 512, pb, prr)
        qb_, qbr = qbring.next()
        qk_norm(k32[:, 0:128], k32r, 2, 1, (tab, tabr, 2), qb_[:, 0:128], qbr, False)
        op("pe", lambda e, qb_=qb_: e.transpose(out=tpQ[:, 6, :], in_=qb_[:, 0:128], identity=identb[:]), reads=[qbr, r_id], writes=[r_tpQ])
        op("dve", lambda e: e.tensor_copy(out=kTa[:, tsl], in_=tpQ[:, 6, :]), reads=[r_tpQ], writes=[r_q[t]])
        op("pool", lambda e: e.tensor_copy(out=Va[:, t, :, 0:64], in_=k32[:, 128:256].rearrange("p (g d) -> p g d", d=64)),
           reads=[k32r], writes=[r_q[t]])
        qb2, qbr2 = qbring.next()
        qk_norm(k32[:, 256:512], k32r, 4, 3, None, qb2[:, 0:256], qbr2, False)
        for j in range(2):
            op("pe", lambda e, j=j: e.transpose(out=tpQ[:, j, :], in_=qb2[:, j * 128:(j + 1) * 128], identity=identb[:]),
               reads=[qbr2, r_id], writes=[r_tpQ])
        op("dve", lambda e: e.tensor_copy(out=kTb[:, :, tsl], in_=tpQ[:, 0:2, :]), reads=[r_tpQ], writes=[r_q[t]])
        pb, prr = OB[1]
        v32, v32r = proj(2048, 256, pb, prr)
        op("pool", lambda e: e.tensor_copy(out=Vb[:, t, :, 0:64], in_=v32[:, 0:256].rearrange("p (g d) -> p g d", d=64)),
           reads=[v32r], writes=[r_q[t]])

    def pre_cgroup(b, t0, ntl):
        ntok = ntl * 128
        z0 = zcol(t0)
        for j in range(2):
            pre_cgroup_j(t0, ntl, ntok, z0, j)

    def pre_cgroup_j(t0, ntl, ntok, z0, j):
        if True:
            pbs = []
            for ci, chunk in enumerate((2 + j, 4 + j, j)):
                pb, prr = ST[ci % 2] if ci < 2 else (OP_, r_OP)
                for k in range(8):
                    c0 = 768 + chunk * 128
                    op("pe", lambda e, k=k, pb=pb, c0=c0: e.matmul(pb[:, 0:ntok], lhsT=w_in[:, k, c0:c0 + 128], rhs=hT[:, k, 0:ntok],
                                                                  start=(k == 0), stop=(k == 7)),
                       reads=[r_win[k]] + r_hT[0:ntl], writes=[prr])
                pbs.append((pb, prr))
            c1, c1r = czring.next()
            op("act", lambda e: e.activation(out=c1[:, 0:ntok], in_=pbs[0][0][:, 0:ntok], func=AF.Copy), reads=[pbs[0][1]], writes=[c1r])
            c2, c2r = czring.next()
            op("dve", lambda e: e.tensor_tensor(out=c2[:, 0:ntok], in0=pbs[1][0][:, 0:ntok], in1=c1[:, 0:ntok], op=ALU.mult),
               reads=[pbs[1][1], c1r], writes=[c2r])
            op("sp", lambda e: e.dma_start(out=K.czT[0, j * 128:(j + 1) * 128, z0:z0 + ntok], in_=c2[:, 0:ntok]),
               reads=[c2r], writes=[r_cz], dma=True)
            c3, c3r = czring.next()
            op("act", lambda e: e.activation(out=c3[:, 0:ntok], in_=pbs[2][0][:, 0:ntok], func=AF.Copy), reads=[pbs[2][1]], writes=[c3r])
            op("sp", lambda e: e.dma_start(out=K.czT[1, j * 128:(j + 1) * 128, z0:z0 + ntok], in_=c3[:, 0:ntok]),
               reads=[c3r], writes=[r_cz], dma=True)

    def normalise(ob, obr, n, dst_fn, dst_res):
        rd, rdr = rdring.next()
        op("dve", lambda e: e.reciprocal(out=rd[64:65, 0:n], in_=ob[64:65, 0:n]), reads=[obr], writes=[rdr])
        bcs, bcr = bcring.next()
        for h0 in range(0, n, 256):
            hn = min(256, n - h0)
            op("pe", lambda e, h0=h0, hn=hn: e.matmul(BC[0:64, 0:hn], lhsT=ones_f[64:65, 0:64], rhs=rd[64:65, h0:h0 + hn],
                                                      start=True, stop=True), reads=[rdr, r_one], writes=[r_BC])
            op("dve", lambda e, h0=h0, hn=hn: e.tensor_copy(out=bcs[:, h0:h0 + hn], in_=BC[0:64, 0:hn]), reads=[r_BC], writes=[bcr])
        dst_fn(ob, bcs, obr, bcr)

    def ss_cols(srcT, r_src, np_, n, col0, stride):
        if "noss" in SKIP:
            return
        sqb, sqr = (sqbring if np_ == 64 else sqcring).next()
        op("dve", lambda e: e.tensor_tensor(out=sqb[0:np_, 0:n], in0=srcT, in1=srcT, op=ALU.mult), reads=[r_src], writes=[sqr])
        for ti in range(n // 128):
            c = 256 + 2 * (col0 + ti * stride)
            op("pe", lambda e, ti=ti, c=c: e.matmul(BC[:, c:c + 2], lhsT=sqb[:, ti * 128:(ti + 1) * 128], rhs=ones_b[:, 0:2],
                                                    start=True, stop=True), reads=[sqr, r_one], writes=[r_SS])

    def attn_block(b, q0, n, ctxq):
        ntl = n // 128
        t_first = q0 // 128
        qres = [r_q[t_first + i] for i in range(ntl)]
        kcs = list(range(LT, NT)) if ctxq else list(range(NT))
        items = [(j, kc) for j in range(4) for kc in kcs]

        def a_st(i):
            j, kc = items[i]
            outs = []
            for g in range(2):
                stb, str_ = ST4[(2 * i + g) % 4]
                op("pe", lambda e, g=g, stb=stb: e.matmul(stb[:, 0:n], lhsT=kTa[64 * g:64 * g + 64, kc * 128:(kc + 1) * 128],
                                                         rhs=qTa[64 * g:64 * g + 64, j, q0:q0 + n], start=True, stop=True),
                   reads=[r_q[kc]] + qres, writes=[str_])
                outs.append((stb, str_))
            res_ = []
            for g in range(2):
                stb, str_ = outs[g]
                pt, ptr = ptring.next()
                op("act", lambda e, stb=stb, pt=pt: e.activation(out=pt[:, 0:n], in_=stb[:, 0:n], func=AF.Exp, scale=0.125),
                   reads=[str_], writes=[ptr])
                res_.append((pt, ptr))
            return res_

        def a_pv(i, pts):
            j, kc = items[i]
            for g in range(2):
                h = 4 * g + j
                pt, ptr = pts[g]
                ob, obr = OB[g]
                if "nopv" not in SKIP:
                    op("pe", lambda e, g=g, pt=pt, ob=ob: e.matmul(ob[0:65, 0:n], lhsT=Va[:, kc, g, :], rhs=pt[:, 0:n],
                                                                  start=(kc == kcs[0]), stop=(kc == kcs[-1])),
                       reads=[ptr, r_q[kc]], writes=[obr])
            if kc == kcs[-1] and "nonorm" not in SKIP:
                for g in range(2):
                    h = 4 * g + j
                    ob, obr = OB[g]

                    def fin(ob_, bcs, obr_, bcr, h=h):
                        op("dve", lambda e: e.tensor_tensor(out=OaT[:, h, 0:n], in0=ob_[0:64, 0:n], in1=bcs[:, 0:n], op=ALU.mult),
                           reads=[obr_, bcr], writes=[r_Oa[h]])
                    normalise(ob, obr, n, fin, None)
                    ss_cols(OaT[:, h, 0:n], r_Oa[h], 64, n, h, 8)

        if "noA" in SKIP:
            items = []
        cur = a_st(0) if items else None
        for i in range(len(items)):
            nxt = a_st(i + 1) if i + 1 < len(items) else None
            a_pv(i, cur)
            cur = nxt

        nrow = n // 64
        bitems = []
        for rr in range(nrow):
            if ctxq:
                chunks = [(LT, None, None), (LT + 1, None, None)]
            else:
                r = q0 // 64 + rr
                rs = min(max(r - 4, 0), 24)
                kt0 = rs // 2
                nch = 5 if rs % 2 else 4
                chunks = []
                for c in range(nch):
                    drs = []
                    for s_ in range(2):
                        kr = 2 * (kt0 + c) + s_
                        drs.append(kr - r + 7 if rs <= kr <= rs + 7 else 15)
                    chunks.append((kt0 + c, drs[0], drs[1]))
                chunks += [(LT, None, None), (LT + 1, None, None)]
            for ci, ch in enumerate(chunks):
                bitems.append((rr, ch, ci == 0, ci == len(chunks) - 1))

        def b_st(i):
            rr, (kt, d0, d1), first, lastc = bitems[i]
            qa = q0 + rr * 64
            for h in (0, 2, 1, 3):
                pr, hf = h // 2, h % 2
                stb, str_ = ST4[2 * (i % 2) + hf]
                op("pe", lambda e, h=h, pr=pr, hf=hf, stb=stb: e.matmul(stb[:, pr * 64:(pr + 1) * 64], lhsT=kTb[64 * hf:64 * hf + 64, pr, kt * 128:(kt + 1) * 128],
                                                                        rhs=qTb[64 * hf:64 * hf + 64, pr, qa:qa + 64], start=True, stop=True),
                   reads=[r_q[kt], r_q[qa // 128]], writes=[str_])
            pt, ptr = ptbring.next()
            ptv = pt[:, 0:256].rearrange("p (pr hf c) -> p hf pr c", pr=2, hf=2)
            for hf in range(2):
                stb, str_ = ST4[2 * (i % 2) + hf]
                sv = stb[:, 0:128].rearrange("p (pr c) -> p pr c", pr=2)
                if d0 is None:
                    op("act", lambda e, hf=hf, sv=sv: e.activation(out=ptv[:, hf], in_=sv, func=AF.Exp, scale=0.125), reads=[str_], writes=[ptr])
                else:
                    sbb, sbr = sbring.next()
                    bv_ = sbb[:, 0:128].rearrange("p (pr c) -> p pr c", pr=2)
                    for s_, dd in ((0, d0), (1, d1)):
                        ps_ = slice(64 * s_, 64 * s_ + 64)
                        tv = tbl[ps_, dd, :, :].rearrange("p (pr hf) c -> p hf pr c", hf=2)[:, hf]
                        op("dve", lambda e, ps_=ps_, tv=tv, sv=sv, bv_=bv_: e.scalar_tensor_tensor(
                            out=bv_[ps_], in0=sv[ps_], scalar=0.125, in1=tv, op0=ALU.mult, op1=ALU.add), reads=[str_, r_tbl], writes=[sbr])
                    op("act", lambda e, hf=hf, bv_=bv_: e.activation(out=ptv[:, hf], in_=bv_, func=AF.Exp), reads=[sbr], writes=[ptr])
            return pt, ptr

        def b_pv_row(rr, pts):
            ob, obr = OB[(rr // 2) % 2]
            cbase = (rr % 2) * 256
            nchk = len(pts)
            for h in range(4):
                for ci, (kt, pt, ptr) in enumerate(pts):
                    op("pe", lambda e, h=h, kt=kt, pt=pt, ci=ci: e.matmul(ob[0:65, cbase + h * 64:cbase + (h + 1) * 64], lhsT=Vb[:, kt, h, :],
                                                                        rhs=pt[:, h * 64:(h + 1) * 64], start=(ci == 0), stop=(ci == nchk - 1)),
                       reads=[ptr, r_q[kt]], writes=[obr])
            if rr % 2 == 1 and "nobnorm" not in SKIP:
                r0 = rr - 1

                def fin(ob_, bcs, obr_, bcr):
                    for h in range(4):
                        ov = ObT[:, h, r0 * 64:r0 * 64 + 128].rearrange("p (r c) -> p r c", r=2)
                        iv = ob_[0:64, :].rearrange("p (r h c) -> p r h c", r=2, h=4)[:, :, h, :]
                        bv = bcs[:, :].rearrange("p (r h c) -> p r h c", r=2, h=4)[:, :, h, :]
                        op("dve", lambda e, ov=ov, iv=iv, bv=bv: e.tensor_tensor(out=ov, in0=iv, in1=bv, op=ALU.mult),
                           reads=[obr_, bcr], writes=[r_Ob[h]])
                normalise(ob, obr, 512, fin, None)

        def b_st_row(rr):
            idxs = [i for i in range(len(bitems)) if bitems[i][0] == rr]
            out = []
            for i in idxs:
                pt, ptr = b_st(i)
                out.append((bitems[i][1][0], pt, ptr))
            return out

        if "noB" in SKIP:
            bitems = []
        nrows_b = nrow if bitems else 0
        curp = b_st_row(0) if nrows_b else None
        for rr in range(nrows_b):
            nxtp = b_st_row(rr + 1) if rr + 1 < nrows_b else None
            b_pv_row(rr, curp)
            curp = nxtp
        for h in range(4):
            if "noB" not in SKIP:
                ss_cols(ObT[:, h, 0:n], r_Ob[h], 64, n, 32 + h, 4)

        z0 = zcol(t_first)
        for j in range(2 if "noC" not in SKIP else 0):
            zz, zr = zring.next()
            op("sp", lambda e, j=j, zz=zz: e.dma_start(out=zz[:, 0:n + 2], in_=K.czT[0, j * 128:(j + 1) * 128, z0 - 1:z0 + n + 1]),
               reads=[r_cz], writes=[zr], dma=True)
            cp, cpr = cpring.next()
            op("sp", lambda e, j=j, cp=cp: e.dma_start(out=cp[:, 0:n], in_=K.czT[1, j * 128:(j + 1) * 128, z0:z0 + n]),
               reads=[r_cz], writes=[cpr], dma=True)
            yy, yr = yring.next()
            op("dve", lambda e, j=j, zz=zz, yy=yy: e.tensor_scalar(out=yy[:, 0:n], in0=zz[:, 0:n], scalar1=cwc[:, j, 0:1], scalar2=None, op0=ALU.mult),
               reads=[zr, r_cw], writes=[yr])
            for w in (1, 2):
                op("dve", lambda e, j=j, zz=zz, yy=yy, w=w: e.scalar_tensor_tensor(out=yy[:, 0:n], in0=zz[:, w:w + n], scalar=cwc[:, j, w:w + 1],
                                                                                   in1=yy[:, 0:n], op0=ALU.mult, op1=ALU.add),
                   reads=[zr, r_cw, yr], writes=[yr])
            op("dve", lambda e, j=j, cp=cp, yy=yy: e.scalar_tensor_tensor(out=ocT[:, j, 0:n], in0=yy[:, 0:n], scalar=cwc[:, j, 3:4], in1=cp[:, 0:n],
                                                                          op0=ALU.add, op1=ALU.mult), reads=[yr, cpr, r_cw], writes=[r_Oc[j]])
            ss_cols(ocT[:, j, 0:n], r_Oc[j], 128, n, 48 + j, 2)

        if "oT" in K.dbg:
            for h in range(8):
                op("pool", lambda e, h=h: e.dma_start(out=K.dbg["oT"][b, h * 64:(h + 1) * 64, q0:q0 + n], in_=OaT[:, h, 0:n]),
                   reads=[r_Oa[h]], writes=[Res()], dma=True)
            for h in range(4):
                op("pool", lambda e, h=h: e.dma_start(out=K.dbg["oT"][b, 512 + h * 64:512 + (h + 1) * 64, q0:q0 + n], in_=ObT[:, h, 0:n]),
                   reads=[r_Ob[h]], writes=[Res()], dma=True)
            for j in range(2):
                op("pool", lambda e, j=j: e.dma_start(out=K.dbg["oT"][b, 768 + j * 128:768 + (j + 1) * 128, q0:q0 + n], in_=ocT[:, j, 0:n]),
                   reads=[r_Oc[j]], writes=[Res()], dma=True)
        if "nomerge" in SKIP:
            return
        v = NB if ctxq else b
        gat, gar = garing.next()
        op("sp", lambda e: e.dma_start(out=gat[:], in_=K.modv[l, v, 2 * D:3 * D].partition_broadcast(128)), writes=[gar], dma=True)
        rs_, rsr = rsring.next()
        for gi_, (c0, nh, width) in enumerate(((0, 8, 512.0), (32, 4, 256.0), (48, 2, 256.0))):
            op("dve", lambda e, gi_=gi_, c0=c0, nh=nh: e.tensor_reduce(
                out=rs_[:, gi_ * 4:gi_ * 4 + ntl], in_=BC[:, 256 + 2 * c0:256 + 2 * (c0 + ntl * nh)].rearrange("p (t h two) -> p t h two", h=nh, two=2)[:, :, :, 0],
                axis=AX.X, op=ALU.add), reads=[r_SS], writes=[rsr])
            op("act", lambda e, gi_=gi_, width=width: e.activation(out=rs_[:, gi_ * 4:gi_ * 4 + ntl], in_=rs_[:, gi_ * 4:gi_ * 4 + ntl],
                                                                  func=AF.Sqrt, bias=EPS, scale=1.0 / width), reads=[rsr], writes=[rsr])
        op("dve", lambda e: e.reciprocal(out=rs_[:, 0:12], in_=rs_[:, 0:12]), reads=[rsr], writes=[rsr])
        for ti in range(ntl):
            t = t_first + ti
            tk = slice(ti * 128, (ti + 1) * 128)
            ac, acr = accring.next()
            xt, xr = xring.next()
            op("sp", lambda e, xt=xt, t=t: e.dma_start(out=xt[:], in_=xsrc(K, l, b, t)), writes=[xr], dma=True)
            for cb in range(2):
                cs = slice(cb * 512, (cb + 1) * 512)
                groups = (([(OaT[:, h, tk], woA[:, h, cs], r_Oa[h]) for h in range(8)], 0),
                          ([(ObT[:, h, tk], woB[:, h, cs], r_Ob[h]) for h in range(4)], 1),
                          ([(ocT[:, j, tk], woC[:, j, cs], r_Oc[j]) for j in range(2)], 2))
                for mm, gi_ in groups:
                    opb, opr = OPR[mctr[0] % 3]
                    mctr[0] += 1
                    for i, (lt, rh, rr_) in enumerate(mm):
                        op("pe", lambda e, lt=lt, rh=rh, i=i, nmm=len(mm), opb=opb: e.matmul(opb[:, :], lhsT=lt, rhs=rh, start=(i == 0), stop=(i == nmm - 1)),
                           reads=[rr_] + r_win, writes=[opr])
                    sc1 = rs_[:, gi_ * 4 + ti:gi_ * 4 + ti + 1]
                    if gi_ == 0:
                        op("dve", lambda e, ac=ac, cs=cs, sc1=sc1, opb=opb: e.tensor_scalar(out=ac[:, cs], in0=opb[:, :], scalar1=sc1, scalar2=None, op0=ALU.mult),
                           reads=[opr, rsr], writes=[acr])
                    else:
                        op("dve", lambda e, ac=ac, cs=cs, sc1=sc1, opb=opb: e.scalar_tensor_tensor(out=ac[:, cs], in0=opb[:, :], scalar=sc1, in1=ac[:, cs],
                                                                                          op0=ALU.mult, op1=ALU.add),
                           reads=[opr, rsr, acr], writes=[acr])
            op("pool", lambda e, ac=ac: e.tensor_tensor(out=ac[:], in0=ac[:], in1=gat[:], op=ALU.mult), reads=[acr, gar], writes=[acr])
            op("dve", lambda e, ac=ac, xt=xt: e.tensor_tensor(out=ac[:], in0=ac[:], in1=xt[:], op=ALU.add), reads=[acr, xr], writes=[acr])
            op("sp", lambda e, ac=ac, t=t: e.dma_start(out=K.xcur[b, t * 128:(t + 1) * 128, :], in_=ac[:]), reads=[acr], writes=[Res()], dma=True)

    for b in range(NB):
        load_win()
        for t0 in range(0, NT, 4):
            ntl = min(4, NT - t0)
            if "nopre" in SKIP:
                continue
            for ti in range(ntl):
                pre_tile(b, t0 + ti, ti)
            if "nocg" not in SKIP:
                pre_cgroup(b, t0, ntl)
        load_wout()
        if "noattn" in SKIP:
            continue
        for qb in range(4):
            attn_block(b, qb * 512, 512, False)
        if not last:
            attn_block(b, SEQ, NCTX, True)


def _rope_table():
    t = np.arange(SEQ)
    row = (t // GW).astype(np.float32)
    col = (t % GW).astype(np.float32)
    half = HD // 2
    inv = (np.float32(10000.0) ** (-np.arange(0, half, 2, dtype=np.float32) / np.float32(half))).astype(np.float32)
    ar = row[:, None] * inv
    ac = col[:, None] * inv
    cr, sr, cc, sc = np.cos(ar), np.sin(ar), np.cos(ac), np.sin(ac)
    tab = np.zeros((NKEY, 128), np.float32)
    tab[:SEQ, 0:64] = np.concatenate([cr, cr, cc, cc], 1)
    tab[:SEQ, 64:128] = np.concatenate([-sr, sr, -sc, sc], 1)
    tab[SEQ:, 0:64] = 1.0
    return tab


def _rpb_layout(rpb):
    cc = np.arange(GW)
    cs = np.clip(cc - 8, 0, GW - 16)
    dc_idx = np.clip(cc[None, :] - cc[:, None] + 15, 0, 30)
    in_win = (cc[None, :] >= cs[:, None]) & (cc[None, :] < cs[:, None] + 16)
    g = rpb[:, :, :, dc_idx]
    g = np.where(in_win[None, None, None], g, np.float32(NEG))
    return np.ascontiguousarray(g.transpose(0, 2, 4, 1, 3)).astype(np.float32)


def make_in_maps(inp, cores=range(8)):
    f = lambda a: np.ascontiguousarray(np.asarray(a, dtype=np.float32))
    shared = {
        "w_mod": f(inp["w_mod"]), "b_mod": f(inp["b_mod"]), "w_in": f(inp["w_in"]),
        "gains": f(np.stack([inp["gq_a"], inp["gk_a"], inp["gq_b"], inp["gk_b"]], 1)),
        "rpbT": _rpb_layout(np.asarray(inp["rpb"], np.float32)),
        "conv_w": f(inp["conv_w"]), "conv_b": f(inp["conv_b"]), "g_out": f(inp["g_out"]), "w_out": f(inp["w_out"]),
        "ffn_w1": f(inp["ffn_w1"]), "ffn_w3": f(inp["ffn_w3"]), "ffn_w2": f(inp["ffn_w2"]),
        "moe_router": f(inp["moe_router"]), "moe_router_b": f(inp["moe_router_b"]),
        "moe_w1": f(inp["moe_w1"]), "moe_w3": f(inp["moe_w3"]), "moe_w2": f(inp["moe_w2"]),
        "ident": np.eye(128, dtype=np.float32), "rope": _rope_table(),
    }
    maps = []
    for i in cores:
        m = dict(shared)
        m["x2"] = f(inp["x"][NB * i:NB * i + NB])
        m["ctx2"] = f(inp["ctx"][NB * i:NB * i + NB])
        m["cvec"] = f(np.concatenate([inp["c"][NB * i:NB * i + NB], np.asarray(inp["c_ctx"])[None]], 0))
        maps.append(m)
    return maps


_NC_CACHE = {}


def kernel(**inputs):
    if "nc" not in _NC_CACHE:
        _NC_CACHE["nc"] = build()
    nc = _NC_CACHE["nc"]
    maps = make_in_maps(inputs)
    res = run_bass_kernel_spmd(nc, maps, core_ids=list(range(8)))
    return np.concatenate([np.asarray(r["y"], dtype=np.float32) for r in res.results], axis=0)
```

```python
from contextlib import ExitStack

import numpy as np
import concourse.bass as bass
import concourse.mybir as mybir
from concourse.bass_utils import run_bass_kernel_spmd

F32 = mybir.dt.float32
BF16 = mybir.dt.bfloat16
AF = mybir.ActivationFunctionType
ALU = mybir.AluOpType
AX = mybir.AxisListType

D = 1024
SEQ = 2048
NCTX = 256
NB = 2
DEPTH = 2
GW = 64
HD = 64
INW = 2304
DFF = 2816
NF = DFF // 128
NE = 8
EPS = 1e-6
NEG = -30000.0
LT = SEQ // 128
CT = NCTX // 128
NT = LT + CT
NKEY = SEQ + NCTX

ENGS = ("pe", "act", "dve", "pool", "sp")
SKIP = set()
NROT = 8


class Res:
    __slots__ = ("w", "r")
    ALL = []

    def __init__(self):
        self.w = None
        self.r = []
        Res.ALL.append(self)


class Op:
    __slots__ = ("eng", "fn", "deps", "dma", "needs_inc", "tok")

    def __init__(self, eng, fn, dma):
        self.eng = eng
        self.fn = fn
        self.dma = dma
        self.deps = []
        self.needs_inc = False
        self.tok = None


class Sched:
    def __init__(self, sems, dsems):
        self.sems = sems
        self.dsems = dsems
        self.cnt = {e: 0 for e in ENGS}
        self.ndma = {e: 0 for e in ENGS}
        self.waited = {e: {} for e in ENGS}
        self.total = 0
        self.reset()

    def reset(self):
        self.ops = {e: [] for e in ENGS}
        self.dma_hist = {e: [] for e in ENGS}

    def op(self, eng, fn, reads=(), writes=(), dma=False):
        o = Op(eng, fn, dma)
        deps = {}
        for r in reads:
            if r.w is not None:
                deps[id(r.w)] = (r.w, True)
        for w in writes:
            if w.w is not None and id(w.w) not in deps:
                deps[id(w.w)] = (w.w, False)
            for rd in w.r:
                if id(rd) not in deps:
                    deps[id(rd)] = (rd, False)
        for p, raw in deps.values():
            if p is o:
                continue
            if p.eng == eng and not p.dma and not dma:
                if eng == "pe" or not raw:
                    continue
            o.deps.append(p)
            p.needs_inc = True
        if dma:
            h = self.dma_hist[eng]
            if len(h) >= NROT:
                o.deps.append(h[-NROT])
            h.append(o)
            o.needs_inc = True
        for r in reads:
            r.r.append(o)
        for w in writes:
            w.w = o
            w.r = []
        self.ops[eng].append(o)
        self.total += 1
        return o

    def finish_block(self):
        tail = []
        for e in ENGS:
            tail += self.dma_hist[e][-NROT:]
        o = Op("sp", lambda e: e.nop(), False)
        o.deps = tail
        self.ops["sp"].append(o)
        for r in Res.ALL:
            r.w = None
            r.r = []

    def emit(self, block):
        for e in ENGS:
            for o in self.ops[e]:
                if o.dma:
                    nd = self.ndma[e]
                    o.tok = (self.dsems[e][nd % NROT], 16 * (nd // NROT + 1))
                    self.ndma[e] = nd + 1
                elif o.needs_inc:
                    self.cnt[e] += 1
                    o.tok = (self.sems[e], self.cnt[e])

        def run(e, engine):
            waited = self.waited[e]
            for o in self.ops[e]:
                need = {}
                for p in o.deps:
                    s, v = p.tok
                    k = s.num
                    if waited.get(k, 0) >= v:
                        continue
                    if k not in need or need[k][1] < v:
                        need[k] = (s, v)
                for k, (s, v) in need.items():
                    engine.wait_ge(s, v)
                    waited[k] = v
                ins = o.fn(engine)
                if o.dma:
                    ins.then_inc(o.tok[0], 16)
                elif o.needs_inc:
                    ins.then_inc(o.tok[0], 1)

        block.tensor(lambda pe: run("pe", pe))
        block.scalar(lambda act: run("act", act))
        block.vector(lambda dve: run("dve", dve))
        block.gpsimd(lambda pool: run("pool", pool))
        block.sync(lambda sp: run("sp", sp))
        self.reset()


class Ring:
    def __init__(self, K, name, shape, dt, n, psum=False):
        self.t = [(K.ps if psum else K.sb)("%s%d" % (name, i), shape, dt) for i in range(n)]
        self.r = [Res() for _ in range(n)]
        self.i = 0

    def next(self):
        j = self.i % len(self.t)
        self.i += 1
        return self.t[j], self.r[j]


class Kern:
    pass


def build(phases=("mod", "attn0", "ffn0", "attn1", "ffn1"), dbg=False):
    nc = bass.Bass("TRN2", target_bir_lowering=False)
    K = Kern()
    K.nc = nc

    def din(name, shape):
        return nc.dram_tensor(name, list(shape), F32, kind="ExternalInput").ap()

    K.x2 = din("x2", [NB, SEQ, D])
    K.ctx2 = din("ctx2", [NB, NCTX, D])
    K.cvec = din("cvec", [NB + 1, D])
    K.w_mod = din("w_mod", [DEPTH, D, 6 * D])
    K.b_mod = din("b_mod", [DEPTH, 6 * D])
    K.w_in = din("w_in", [DEPTH, D, INW])
    K.gains = din("gains", [DEPTH, 4, HD])
    K.rpbT = din("rpbT", [DEPTH, 15, 64, 4, 64])
    K.conv_w = din("conv_w", [DEPTH, 3, 256])
    K.conv_b = din("conv_b", [DEPTH, 256])
    K.g_out = din("g_out", [DEPTH, D])
    K.w_out = din("w_out", [DEPTH, D, D])
    K.ffn_w1 = din("ffn_w1", [1, D, DFF])
    K.ffn_w3 = din("ffn_w3", [1, D, DFF])
    K.ffn_w2 = din("ffn_w2", [1, DFF, D])
    K.moe_router = din("moe_router", [1, D, NE])
    K.moe_router_b = din("moe_router_b", [1, NE])
    K.moe_w1 = din("moe_w1", [1, NE, D, DFF])
    K.moe_w3 = din("moe_w3", [1, NE, D, DFF])
    K.moe_w2 = din("moe_w2", [1, NE, DFF, D])
    K.ident = din("ident", [128, 128])
    K.rope = din("rope", [NKEY, 128])
    K.y = nc.dram_tensor("y", [NB, SEQ, D], F32, kind="ExternalOutput").ap()
    K.xcur = nc.dram_tensor("xcur", [NB, NKEY, D], F32, kind="Internal").ap()
    K.modv = nc.dram_tensor("modv", [DEPTH, NB + 1, 6 * D], F32, kind="Internal").ap()
    K.czT = nc.dram_tensor("czT", [2, 256, 2308], F32, kind="Internal").ap()
    K.dbg = {}
    if dbg:
        K.dbg["modv"] = nc.dram_tensor("dbg_modv", [DEPTH, NB + 1, 6 * D], F32, kind="ExternalOutput").ap()
        K.dbg["xcur"] = nc.dram_tensor("dbg_xcur", [NB, NKEY, D], F32, kind="ExternalOutput").ap()
        K.dbg["oT"] = nc.dram_tensor("dbg_oT", [NB, D, NKEY], F32, kind="ExternalOutput").ap()

    with ExitStack() as gst:
        sems = {e: gst.enter_context(nc.semaphore("s_" + e)) for e in ENGS}
        dsems = {}
        for e in ("sp", "pool", "act"):
            dsems[e] = [gst.enter_context(nc.semaphore("d_%s%d" % (e, i))) for i in range(NROT)]
        dsems["pe"] = dsems["sp"]
        dsems["dve"] = dsems["sp"]
        K.S = Sched(sems, dsems)
        K.outs = []

        for ph in phases:
            with ExitStack() as st:
                K.st = st
                K.stk = [st]
                K.uid = getattr(K, "uid", 0)

                def _sb(name, shape, dt, ph=ph):
                    K.uid += 1
                    return K.stk[-1].enter_context(nc.sbuf_tensor("%s_%s_%d" % (ph, name, K.uid), list(shape), dt))

                def _ps(name, shape, dt, ph=ph):
                    K.uid += 1
                    return K.stk[-1].enter_context(nc.psum_tensor("%s_%s_%d" % (ph, name, K.uid), list(shape), dt))

                def _flush():
                    if sum(len(v) for v in K.S.ops.values()) == 0:
                        return
                    K.S.finish_block()
                    with nc.Block() as block:
                        K.S.emit(block)
                K.sb, K.ps, K.flush = _sb, _ps, _flush
                if ph == "mod":
                    phase_mod(K)
                elif ph.startswith("attn"):
                    phase_attn(K, int(ph[4:]))
                elif ph.startswith("ffn"):
                    phase_ffn(K, int(ph[3:]))
                elif ph == "dbgcopy":
                    phase_dbgcopy(K)
                elif ph == "initx":
                    phase_initx(K)
                K.flush()
    return nc


def xsrc(K, l, b, t):
    if l == 0:
        if t < LT:
            return K.x2[b, t * 128:(t + 1) * 128, :]
        return K.ctx2[b, (t - LT) * 128:(t - LT + 1) * 128, :]
    return K.xcur[b, t * 128:(t + 1) * 128, :]


def phase_mod(K):
    nc, S = K.nc, K.S
    NV = NB + 1
    cT = K.sb("cT", [128, 8, NV], F32)
    r_cT = Res()
    scT = K.sb("scT", [128, 8, NV], BF16)
    r_scT = Res()
    for v in range(NV):
        S.op("sp", lambda e, v=v: e.dma_start(out=cT[:, :, v], in_=K.cvec[v].rearrange("(k p) -> p k", p=128),
                                              allow_slow_non_contiguous=True), writes=[r_cT], dma=True)
    S.op("act", lambda e: e.activation(out=scT[:], in_=cT[:], func=AF.Silu), reads=[r_cT], writes=[r_scT])
    wring = Ring(K, "wm", [128, 8, 512], BF16, 3)
    bring = Ring(K, "bm", [NV, 512], F32, 2)
    oring = Ring(K, "om", [NV, 512], F32, 2)
    pring = Ring(K, "pm", [NV, 512], F32, 2, psum=True)
    for l in range(DEPTH):
        for cb in range(12):
            wt, wr = wring.next()
            S.op("pool", lambda e, wt=wt, l=l, cb=cb: e.dma_start(
                out=wt[:], in_=K.w_mod[l, :, cb * 512:(cb + 1) * 512].rearrange("(k p) c -> p k c", p=128)),
                writes=[wr], dma=True)
            bt, br = bring.next()
            S.op("sp", lambda e, bt=bt, l=l, cb=cb: e.dma_start(
                out=bt[:], in_=K.b_mod[l, cb * 512:(cb + 1) * 512].partition_broadcast(NV)),
                writes=[br], dma=True)
            pt, pr = pring.next()
            for k in range(8):
                S.op("pe", lambda e, pt=pt, wt=wt, k=k: e.matmul(pt[:], lhsT=scT[:, k, :], rhs=wt[:, k, :],
                                                                 start=(k == 0), stop=(k == 7)),
                     reads=[r_scT, wr], writes=[pr])
            ot, orr = oring.next()
            S.op("dve", lambda e, ot=ot, pt=pt, bt=bt: e.tensor_tensor(out=ot[:], in0=pt[:], in1=bt[:], op=ALU.add),
                 reads=[pr, br], writes=[orr])
            S.op("sp", lambda e, ot=ot, l=l, cb=cb: e.dma_start(out=K.modv[l, :, cb * 512:(cb + 1) * 512], in_=ot[:]),
                 reads=[orr], writes=[Res()], dma=True)
            if "modv" in K.dbg:
                S.op("sp", lambda e, ot=ot, l=l, cb=cb: e.dma_start(
                    out=K.dbg["modv"][l, :, cb * 512:(cb + 1) * 512], in_=ot[:]),
                    reads=[orr], writes=[Res()], dma=True)


def load_modcols(K, l, v, idx, name, plus1):
    S = K.S
    t = K.sb(name, [128, 8], F32)
    r = Res()
    S.op("sp", lambda e: e.dma_start(out=t[:], in_=K.modv[l, v, idx * D:(idx + 1) * D].rearrange("(k p) -> p k", p=128),
                                     allow_slow_non_contiguous=True), writes=[r], dma=True)
    if plus1:
        S.op("dve", lambda e: e.tensor_scalar_add(out=t[:], in0=t[:], scalar1=1.0), reads=[r], writes=[r])
    return t, r


def load_bcast(K, src_row, n, name, dt=F32, eng="sp"):
    S = K.S
    t = K.sb(name, [128, n], dt)
    r = Res()
    S.op(eng, lambda e: e.dma_start(out=t[:], in_=src_row.partition_broadcast(128)), writes=[r], dma=True)
    return t, r


def phase_ffn(K, l):
    nc, S = K.nc, K.S
    moe = (l % 2 == 1)
    last = (l == DEPTH - 1)
    E = NE if moe else 1
    if moe:
        W1, W3, W2 = K.moe_w1[0], K.moe_w3[0], K.moe_w2[0]
    else:
        W1, W3, W2 = K.ffn_w1, K.ffn_w3, K.ffn_w2

    ident = K.sb("identf", [128, 128], F32)
    r_ident = Res()
    S.op("sp", lambda e: e.dma_start(out=ident[:], in_=K.ident), writes=[r_ident], dma=True)

    mods = {}
    for v in range(NB + 1):
        if v == NB and last:
            continue
        sh, r_sh = load_modcols(K, l, v, 3, "shf%d" % v, False)
        sc, r_sc = load_modcols(K, l, v, 4, "scf%d" % v, True)
        gf, r_gf = load_bcast(K, K.modv[l, v, 5 * D:6 * D], D, "gf%d" % v)
        mods[v] = (sh, r_sh, sc, r_sc, gf, r_gf)
    if moe:
        wr_t = K.sb("wrt", [128, 8, NE], F32)
        r_wr = Res()
        S.op("sp", lambda e: e.dma_start(out=wr_t[:], in_=K.moe_router[0].rearrange("(k p) e -> p k e", p=128)),
             writes=[r_wr], dma=True)
        br_t, r_br = load_bcast(K, K.moe_router_b[0], NE, "brt")

    TB = 8
    hT = K.sb("hT", [128, 8, TB * 128], BF16)
    r_hT = [Res() for _ in range(TB)]
    uT = K.sb("uT", [128, NF, TB * 128], BF16)
    r_uT = [[Res() for _ in range(2)] for _ in range(NF)]
    acc = K.sb("acc", [128, TB, D], F32)
    r_acc = [[Res(), Res()] for _ in range(TB)]
    G = K.sb("G", [128, TB, NE], F32)
    r_G = [Res() for _ in range(TB)]
    w2b = K.sb("w2b", [128, NF, D], BF16)
    r_w2 = [Res() for _ in range(NF)]
    xring = Ring(K, "xt", [128, D], F32, 2)
    nring = Ring(K, "xn", [128, D], F32, 2)
    jring = Ring(K, "jk", [128, D], BF16, 1)
    sring = Ring(K, "st", [128, 4], F32, 3)
    hfring = Ring(K, "hf", [128, 8, 128], F32, 2)
    w13ring = Ring(K, "w13", [128, 2, 8, 128], BF16, 3)
    sgring = Ring(K, "sg", [128, 512], F32, 2)
    tring = Ring(K, "tp", [128, 4, 128], F32, 1, psum=True)
    lgring = Ring(K, "lg", [128, NE], F32, 1, psum=True)
    gvring = Ring(K, "gv", [128, 2, 512], F32, 2, psum=True)
    oring = Ring(K, "op", [128, 512], F32, 2, psum=True)
    smring = Ring(K, "sm", [128, 4 * NE + 8], F32, 2)
    yring = Ring(K, "yt", [128, D], F32, 2)

    blocks = []
    for b in range(NB):
        blocks.append((b, b, 0, 8))
        blocks.append((b, b, 8, 8))
        if not last:
            blocks.append((b, NB, LT, CT))

    def router(ti, lg, lr):
        sm, mr = smring.next()
        L0, E1, L2, E2, M = sm[:, 0:8], sm[:, 8:16], sm[:, 16:24], sm[:, 24:32], sm[:, 32:40]
        seq = [
            lambda e: e.tensor_tensor(out=L0, in0=lg[:], in1=br_t[:], op=ALU.add),
            lambda e: e.reduce_max(out=M[:, 0:1], in_=L0, axis=AX.X),
            lambda e: e.tensor_scalar(out=E1, in0=L0, scalar1=M[:, 0:1], scalar2=None, op0=ALU.is_equal),
            lambda e: e.scalar_tensor_tensor(out=L2, in0=E1, scalar=-1e30, in1=L0, op0=ALU.mult, op1=ALU.add),
            lambda e: e.reduce_max(out=M[:, 1:2], in_=L2, axis=AX.X),
            lambda e: e.tensor_scalar(out=E2, in0=L2, scalar1=M[:, 1:2], scalar2=None, op0=ALU.is_equal),
            lambda e: e.tensor_tensor(out=M[:, 2:3], in0=M[:, 1:2], in1=M[:, 0:1], op=ALU.subtract),
        ]
        for i, fn in enumerate(seq):
            S.op("dve", (lambda fn: (lambda e: fn(e)))(fn), reads=[mr, lr, r_br] if i == 0 else [mr], writes=[mr])
        S.op("act", lambda e, M=M: e.activation(out=M[:, 3:4], in_=M[:, 2:3], func=AF.Exp), reads=[mr], writes=[mr])
        seq2 = [
            lambda e: e.tensor_scalar_add(out=M[:, 4:5], in0=M[:, 3:4], scalar1=1.0),
            lambda e: e.reciprocal(out=M[:, 5:6], in_=M[:, 4:5]),
            lambda e: e.tensor_tensor(out=M[:, 6:7], in0=M[:, 3:4], in1=M[:, 5:6], op=ALU.mult),
            lambda e: e.tensor_scalar(out=E1, in0=E1, scalar1=M[:, 5:6], scalar2=None, op0=ALU.mult),
        ]
        for fn in seq2:
            S.op("dve", (lambda fn: (lambda e: fn(e)))(fn), reads=[mr], writes=[mr])
        S.op("dve", lambda e, E1=E1, E2=E2, M=M, ti=ti: e.scalar_tensor_tensor(
            out=G[:, ti, :], in0=E2, scalar=M[:, 6:7], in1=E1, op0=ALU.mult, op1=ALU.add),
            reads=[mr], writes=[r_G[ti]])


    def ffn_block(b, v, t0, ntl):
        sh, r_sh, sc, r_sc, gf, r_gf = mods[v]
        for ti in range(ntl):
            t = t0 + ti
            xt, xr = xring.next()
            S.op("sp", lambda e, xt=xt, t=t: e.dma_start(out=xt[:], in_=xsrc(K, 1, b, t)), writes=[xr], dma=True)
            jk, jr = jring.next()
            stt, sr = sring.next()
            S.op("act", lambda e, jk=jk, xt=xt, stt=stt: e.activation(out=jk[:], in_=xt[:], func=AF.Square,
                                                                     scale=float(D ** -0.5), accum_out=stt[:, 0:1]),
                 reads=[xr], writes=[jr, sr])
            S.op("act", lambda e, stt=stt: e.activation(out=stt[:, 1:2], in_=stt[:, 0:1], func=AF.Sqrt, bias=EPS, scale=1.0),
                 reads=[sr], writes=[sr])
            S.op("dve", lambda e, stt=stt: e.reciprocal(out=stt[:, 2:3], in_=stt[:, 1:2]), reads=[sr], writes=[sr])
            xn, nr = nring.next()
            S.op("dve", lambda e, xn=xn, xt=xt, stt=stt: e.tensor_scalar(out=xn[:], in0=xt[:], scalar1=stt[:, 2:3],
                                                                       scalar2=None, op0=ALU.mult),
                 reads=[xr, sr], writes=[nr])
            hf, hr = hfring.next()
            for hh in range(2):
                tp, tr = tring.next()
                for kk in range(4):
                    k = hh * 4 + kk
                    S.op("pe", lambda e, tp=tp, xn=xn, k=k, kk=kk: e.transpose(out=tp[:, kk, :], in_=xn[:, k * 128:(k + 1) * 128],
                                                                             identity=ident[:]),
                         reads=[nr, r_ident], writes=[tr])
                for kk in range(4):
                    k = hh * 4 + kk
                    if moe:
                        S.op("dve", lambda e, hf=hf, tp=tp, k=k, kk=kk: e.tensor_scalar(
                            out=hf[:, k, :], in0=tp[:, kk, :], scalar1=sc[:, k:k + 1], scalar2=sh[:, k:k + 1],
                            op0=ALU.mult, op1=ALU.add), reads=[tr, r_sc, r_sh], writes=[hr])
                    else:
                        S.op("dve", lambda e, tp=tp, k=k, kk=kk, ti=ti: e.tensor_scalar(
                            out=hT[:, k, ti * 128:(ti + 1) * 128], in0=tp[:, kk, :], scalar1=sc[:, k:k + 1],
                            scalar2=sh[:, k:k + 1], op0=ALU.mult, op1=ALU.add),
                            reads=[tr, r_sc, r_sh], writes=[r_hT[ti]])
            if moe:
                S.op("pool", lambda e, hf=hf, ti=ti: e.tensor_copy(out=hT[:, :, ti * 128:(ti + 1) * 128], in_=hf[:]),
                     reads=[hr], writes=[r_hT[ti]])
                lg, lr = lgring.next()
                for k in range(8):
                    S.op("pe", lambda e, lg=lg, hf=hf, k=k: e.matmul(lg[:], lhsT=hf[:, k, :], rhs=wr_t[:, k, :],
                                                                     start=(k == 0), stop=(k == 7)),
                         reads=[hr, r_wr], writes=[lr])
                router(ti, lg, lr)
        NTOK = ntl * 128
        halves = [(h0, min(512, NTOK - h0)) for h0 in range(0, NTOK, 512)]
        for ex in range(E):
            w1e = W1[ex] if moe else W1[0]
            w3e = W3[ex] if moe else W3[0]
            w2e = W2[ex] if moe else W2[0]
            for f in range(NF):
                wt, wr = w13ring.next()
                S.op("pool", lambda e, wt=wt, w1e=w1e, f=f: e.dma_start(
                    out=wt[:, 0, :, :], in_=w1e[:, f * 128:(f + 1) * 128].rearrange("(k p) c -> p k c", p=128)),
                    writes=[wr], dma=True)
                r2 = Res()
                S.op("pool", lambda e, wt=wt, w3e=w3e, f=f: e.dma_start(
                    out=wt[:, 1, :, :], in_=w3e[:, f * 128:(f + 1) * 128].rearrange("(k p) c -> p k c", p=128)),
                    writes=[r2], dma=True)
                S.op("pool", lambda e, w2e=w2e, f=f: e.dma_start(out=w2b[:, f, :], in_=w2e[f * 128:(f + 1) * 128, :]),
                     writes=[r_w2[f]], dma=True)
                for hi, (h0, hn) in enumerate(halves):
                    gv, gr = gvring.next()
                    tiles_in = list(range(h0 // 128, (h0 + hn) // 128))
                    for j in range(2):
                        for k in range(8):
                            S.op("pe", lambda e, gv=gv, wt=wt, j=j, k=k, h0=h0, hn=hn: e.matmul(
                                gv[:, j, 0:hn], lhsT=wt[:, j, k, :], rhs=hT[:, k, h0:h0 + hn], start=(k == 0), stop=(k == 7)),
                                reads=[wr, r2] + [r_hT[i] for i in tiles_in], writes=[gr])
                    sg, sr2 = sgring.next()
                    S.op("act", lambda e, sg=sg, gv=gv, hn=hn: e.activation(out=sg[:, 0:hn], in_=gv[:, 0, 0:hn], func=AF.Silu),
                         reads=[gr], writes=[sr2])
                    S.op("dve", lambda e, sg=sg, gv=gv, f=f, h0=h0, hn=hn: e.tensor_tensor(
                        out=uT[:, f, h0:h0 + hn], in0=gv[:, 1, 0:hn], in1=sg[:, 0:hn], op=ALU.mult),
                        reads=[gr, sr2], writes=[r_uT[f][hi]])
            for ti in range(ntl):
                for cb in range(2):
                    ot, orr = oring.next()
                    for f in range(NF):
                        S.op("pe", lambda e, ot=ot, f=f, ti=ti, cb=cb: e.matmul(
                            ot[:], lhsT=uT[:, f, ti * 128:(ti + 1) * 128], rhs=w2b[:, f, cb * 512:(cb + 1) * 512],
                            start=(f == 0), stop=(f == NF - 1)),
                            reads=[r_uT[f][ti // 4], r_w2[f]], writes=[orr])
                    asl = acc[:, ti, cb * 512:(cb + 1) * 512]
                    if not moe:
                        S.op("dve", lambda e, asl=asl, ot=ot: e.tensor_copy(out=asl, in_=ot[:]),
                             reads=[orr], writes=[r_acc[ti][cb]])
                    elif ex == 0:
                        S.op("dve", lambda e, asl=asl, ot=ot, ti=ti, ex=ex: e.tensor_scalar(
                            out=asl, in0=ot[:], scalar1=G[:, ti, ex:ex + 1], scalar2=None, op0=ALU.mult),
                            reads=[orr, r_G[ti]], writes=[r_acc[ti][cb]])
                    else:
                        S.op("dve", lambda e, asl=asl, ot=ot, ti=ti, ex=ex: e.scalar_tensor_tensor(
                            out=asl, in0=ot[:], scalar=G[:, ti, ex:ex + 1], in1=asl, op0=ALU.mult, op1=ALU.add),
                            reads=[orr, r_G[ti], r_acc[ti][cb]], writes=[r_acc[ti][cb]])
        for ti in range(ntl):
            t = t0 + ti
            xt, xr = xring.next()
            S.op("sp", lambda e, xt=xt, t=t: e.dma_start(out=xt[:], in_=xsrc(K, 1, b, t)), writes=[xr], dma=True)
            yt, yr = yring.next()
            S.op("pool", lambda e, yt=yt, ti=ti: e.tensor_tensor(out=yt[:], in0=acc[:, ti, :], in1=gf[:], op=ALU.mult),
                 reads=[r_acc[ti][0], r_acc[ti][1], r_gf], writes=[yr])
            S.op("dve", lambda e, yt=yt, xt=xt: e.tensor_tensor(out=yt[:], in0=yt[:], in1=xt[:], op=ALU.add),
                 reads=[yr, xr], writes=[yr])
            if last:
                dst = K.y[b, t * 128:(t + 1) * 128, :]
            else:
                dst = K.xcur[b, t * 128:(t + 1) * 128, :]
            S.op("sp", lambda e, yt=yt, dst=dst: e.dma_start(out=dst, in_=yt[:]), reads=[yr], writes=[Res()], dma=True)

    for blk in blocks:
        ffn_block(*blk)


def phase_dbgcopy(K):
    S = K.S
    ring = Ring(K, "dc", [128, D], F32, 2)
    for b in range(NB):
        for t in range(NT):
            tt, tr = ring.next()
            S.op("sp", lambda e, tt=tt, b=b, t=t: e.dma_start(out=tt[:], in_=K.xcur[b, t * 128:(t + 1) * 128, :]),
                 writes=[tr], dma=True)
            S.op("sp", lambda e, tt=tt, b=b, t=t: e.dma_start(out=K.dbg["xcur"][b, t * 128:(t + 1) * 128, :], in_=tt[:]),
                 reads=[tr], writes=[Res()], dma=True)


def phase_initx(K):
    S = K.S
    ring = Ring(K, "ix", [128, D], F32, 2)
    for b in range(NB):
        for t in range(NT):
            tt, tr = ring.next()
            S.op("sp", lambda e, tt=tt, b=b, t=t: e.dma_start(out=tt[:], in_=xsrc(K, 0, b, t)), writes=[tr], dma=True)
            S.op("sp", lambda e, tt=tt, b=b, t=t: e.dma_start(out=K.xcur[b, t * 128:(t + 1) * 128, :], in_=tt[:]),
                 reads=[tr], writes=[Res()], dma=True)


def phase_attn(K, l):
    nc, S = K.nc, K.S
    last = (l == DEPTH - 1)
    op = S.op
    ZW = 2308

    identb = K.sb("identb", [128, 128], BF16)
    r_id = Res()
    op("pool", lambda e: e.dma_start(out=identb[:], in_=K.ident), writes=[r_id], dma=True)
    Wt = K.sb("Wt", [128, 8 * INW], BF16)
    w_in = Wt[:, :].rearrange("p (k c) -> p k c", k=8)
    woA = Wt[0:64, 0:8 * D].rearrange("p (h c) -> p h c", h=8)
    woB = Wt[0:64, 8 * D:12 * D].rearrange("p (h c) -> p h c", h=4)
    woC = Wt[:, 12 * D:14 * D].rearrange("p (h c) -> p h c", h=2)
    r_win = [Res() for _ in range(8)]
    gcol = K.sb("gcol", [128, 16], F32)
    r_gc = Res()
    op("sp", lambda e: e.dma_start(out=gcol[0:64, 0:8], in_=K.g_out[l, 0:512].rearrange("(h d) -> d h", d=64),
                                   allow_slow_non_contiguous=True), writes=[r_gc], dma=True)
    op("sp", lambda e: e.dma_start(out=gcol[0:64, 8:12], in_=K.g_out[l, 512:768].rearrange("(h d) -> d h", d=64),
                                   allow_slow_non_contiguous=True), writes=[r_gc], dma=True)
    op("sp", lambda e: e.dma_start(out=gcol[:, 12:14], in_=K.g_out[l, 768:1024].rearrange("(k p) -> p k", p=128),
                                   allow_slow_non_contiguous=True), writes=[r_gc], dma=True)
    stg = Ring(K, "wstg", [128, D], F32, 2)

    def load_win():
        for k in range(8):
            op("pool", lambda e, k=k: e.dma_start(out=w_in[:, k, :], in_=K.w_in[l, k * 128:(k + 1) * 128, :]),
               writes=[r_win[k]], dma=True)

    def load_wout():
        pieces = [(woA, h, 64, h * 64, h) for h in range(8)] + [(woB, h, 64, 512 + h * 64, 8 + h) for h in range(4)] + \
                 [(woC, k, 128, 768 + k * 128, 12 + k) for k in range(2)]
        for (dst, idx, np_, row0, gc) in pieces:
            st_, sr_ = stg.next()
            op("sp", lambda e, st_=st_, np_=np_, row0=row0: e.dma_start(out=st_[0:np_, :], in_=K.w_out[l, row0:row0 + np_, :]),
               writes=[sr_], dma=True)
            op("dve", lambda e, st_=st_, dst=dst, idx=idx, np_=np_, gc=gc: e.tensor_scalar(
                out=dst[0:np_, idx, :], in0=st_[0:np_, :], scalar1=gcol[0:np_, gc:gc + 1], scalar2=None, op0=ALU.mult),
                reads=[sr_, r_gc], writes=r_win)

    gn = K.sb("gn", [128, 4, 64], F32)
    gsw = K.sb("gsw", [128, 2, 64], F32)
    r_gn = Res()
    for i in range(4):
        op("sp", lambda e, i=i: e.dma_start(out=gn[:, i, :], in_=K.gains[l, i].partition_broadcast(128)), writes=[r_gn], dma=True)
    for i in range(2):
        for (d0, s0) in ((0, 16), (16, 0), (32, 48), (48, 32)):
            op("sp", lambda e, i=i, d0=d0, s0=s0: e.dma_start(out=gsw[:, i, d0:d0 + 16],
                                                              in_=K.gains[l, i, s0:s0 + 16].partition_broadcast(128)),
               writes=[r_gn], dma=True)
    tbl = K.sb("tbl", [128, 16, 4, 64], BF16)
    r_tbl = Res()
    for dr in range(15):
        for s_ in range(2):
            op("pool", lambda e, dr=dr, s_=s_: e.dma_start(out=tbl[s_ * 64:(s_ + 1) * 64, dr, :, :], in_=K.rpbT[l, dr]),
               writes=[r_tbl], dma=True)
    op("pool", lambda e: e.memset(tbl[:, 15, :, :], NEG), writes=[r_tbl])
    cwc = K.sb("cwc", [128, 2, 4], F32)
    r_cw = Res()
    for w in range(3):
        op("sp", lambda e, w=w: e.dma_start(out=cwc[:, :, w], in_=K.conv_w[l, w].rearrange("(k p) -> p k", p=128),
                                            allow_slow_non_contiguous=True), writes=[r_cw], dma=True)
    op("sp", lambda e: e.dma_start(out=cwc[:, :, 3], in_=K.conv_b[l].rearrange("(k p) -> p k", p=128),
                                   allow_slow_non_contiguous=True), writes=[r_cw], dma=True)
    ones_b = K.sb("ones_b", [128, 2], BF16)
    ones_f = K.sb("ones_f", [128, 64], F32)
    zt = K.sb("zt", [128, 2, 4], F32)
    r_one = Res()
    op("pool", lambda e: e.memset(ones_b[:], 1.0), writes=[r_one])
    op("pool", lambda e: e.memset(ones_f[:], 1.0), writes=[r_one])
    op("pool", lambda e: e.memset(zt[:], 0.0), writes=[r_one])
    r_cz = Res()
    for a in range(2):
        for (c0, n) in ((0, 1), (2049, 2), (2307, 1)):
            op("sp", lambda e, a=a, c0=c0, n=n: e.dma_start(
                out=K.czT[a, :, c0:c0 + n].rearrange("(k p) c -> p k c", p=128), in_=zt[:, :, 0:n], allow_slow_non_contiguous=True),
                reads=[r_one], writes=[r_cz], dma=True)
    mods = {}
    for v in range(NB + 1):
        sh, r_sh = load_modcols(K, l, v, 0, "sha%d" % v, False)
        sc, r_sc = load_modcols(K, l, v, 1, "sca%d" % v, True)
        mods[v] = (sh, r_sh, sc, r_sc)

    qTa = K.sb("qTa", [128, 4, NKEY], BF16)
    kTa = K.sb("kTa", [128, NKEY], BF16)
    Va = K.sb("Va", [128, NT, 2, 65], BF16)
    qTb = K.sb("qTb", [128, 2, NKEY], BF16)
    kTb = K.sb("kTb", [128, 2, NKEY], BF16)
    Vb = K.sb("Vb", [128, NT, 4, 65], BF16)
    r_q = [Res() for _ in range(NT)]
    op("pool", lambda e: e.memset(Va[:, :, :, 64:65], 1.0), writes=r_q)
    op("pool", lambda e: e.memset(Vb[:, :, :, 64:65], 1.0), writes=r_q)

    stp = [K.ps("stp%d" % i, [128, 2, 512], F32) for i in range(2)]
    bkm = [K.ps("bk%d" % i, [128, 512], F32) for i in range(2, 6)]
    bk = [stp[0][:, 0, :], stp[0][:, 1, :]] + [t_[:, :] for t_ in bkm] + [stp[1][:, 0, :], stp[1][:, 1, :]]
    r_bk = [Res() for _ in range(8)]
    tpA = bk[6][:, :].bitcast(BF16).rearrange("p (k c) -> p k c", k=8)
    tpQ = bk[7][:, :].bitcast(BF16).rearrange("p (k c) -> p k c", k=8)
    r_tpA, r_tpQ = r_bk[6], r_bk[7]
    ST = [(bk[0], r_bk[0]), (bk[1], r_bk[1])]
    ST4 = [(bk[0], r_bk[0]), (bk[1], r_bk[1]), (bk[6], r_bk[6]), (bk[7], r_bk[7])]
    OB = [(bk[2], r_bk[2]), (bk[3], r_bk[3])]
    OP_, r_OP = bk[4], r_bk[4]
    OPR = [(bk[4], r_bk[4]), (bk[6], r_bk[6]), (bk[7], r_bk[7])]
    BC, r_BC = bk[5], r_bk[5]
    r_SS = Res()

    ST = None
    mctr = [0]

    def zcol(t):
        return 1 + t * 128 if t < LT else 2051 + (t - LT) * 128

    def pre_stage(b):
        xring = Ring(K, "xa", [128, D], F32, 3)
        sring = Ring(K, "sa", [128, 4], F32, 4)
        nring = Ring(K, "na", [128, D], BF16, 2)
        hTs = [K.sb("hTa%d" % i, [128, 8, 512], BF16) for i in range(2)]
        r_hTs = [[Res() for _ in range(4)] for _ in range(2)]
        rpring = Ring(K, "rp", [128, 128], F32, 2)
        tabring = Ring(K, "tb", [128, 4, 64], F32, 3)
        q32ring = Ring(K, "q32", [128, 1152], F32, 3)
        q32res = [[Res() for _ in range(4)] for _ in range(3)]
        sqring = Ring(K, "sq32", [128, 1152], F32, 2)
        t1ring = Ring(K, "t1", [128, 1152], F32, 2)
        t2ring = Ring(K, "t2", [128, 640], F32, 2)
        smring = Ring(K, "sma", [128, 96], F32, 3)
        qbring = Ring(K, "qb", [128, 1152], BF16, 2)
        czring = Ring(K, "cz", [128, 512], F32, 6)
        tpQ2 = bk[5][:, :].bitcast(BF16).rearrange("p (k c) -> p k c", k=8)
        r_tpQ2 = r_bk[5]
        CG, r_CG = bk[4], r_bk[4]
        state = {}

        def stage_a1(t):
            xt, xr = xring.next()
            op("sp", lambda e: e.dma_start(out=xt[:], in_=xsrc(K, l, b, t)), writes=[xr], dma=True)
            xn, nr = nring.next()
            stt, sr = sring.next()
            op("act", lambda e: e.activation(out=xn[:], in_=xt[:], func=AF.Square, scale=float(D ** -0.5), accum_out=stt[:, 0:1]),
               reads=[xr], writes=[nr, sr])
            op("act", lambda e: e.activation(out=stt[:, 1:2], in_=stt[:, 0:1], func=AF.Sqrt, bias=EPS, scale=1.0), reads=[sr], writes=[sr])
            op("dve", lambda e: e.reciprocal(out=stt[:, 2:3], in_=stt[:, 1:2]), reads=[sr], writes=[sr])
            op("dve", lambda e: e.tensor_scalar(out=xn[:], in0=xt[:], scalar1=stt[:, 2:3], scalar2=None, op0=ALU.mult),
               reads=[xr, sr], writes=[nr])
            state[t] = dict(xn=xn, nr=nr)

        def stage_a2(t):
            grp, ti = (t // 4) % 2, t % 4
            hT, r_hT = hTs[grp], r_hTs[grp]
            v = b if t < LT else NB
            sh, r_sh, sc, r_sc = mods[v]
            xn, nr = state[t]["xn"], state[t]["nr"]
            for k in range(8):
                op("pe", lambda e, k=k: e.transpose(out=tpA[:, k, :], in_=xn[:, k * 128:(k + 1) * 128], identity=identb[:]),
                   reads=[nr, r_id], writes=[r_tpA])
            for k in range(8):
                if k % 2 == 0:
                    op("act", lambda e, k=k: e.activation(out=hT[:, k, ti * 128:(ti + 1) * 128], in_=tpA[:, k, :], func=AF.Identity,
                                                          bias=sh[:, k:k + 1], scale=sc[:, k:k + 1]),
                       reads=[r_tpA, r_sh, r_sc], writes=[r_hT[ti]])
                else:
                    op("dve", lambda e, k=k: e.tensor_scalar(out=hT[:, k, ti * 128:(ti + 1) * 128], in0=tpA[:, k, :],
                                                             scalar1=sc[:, k:k + 1], scalar2=sh[:, k:k + 1], op0=ALU.mult, op1=ALU.add),
                       reads=[r_tpA, r_sh, r_sc], writes=[r_hT[ti]])
            rp, rpr = rpring.next()
            op("sp", lambda e: e.dma_start(out=rp[:], in_=K.rope[t * 128:(t + 1) * 128, :]), writes=[rpr], dma=True)
            tab, tabr = tabring.next()
            for i in range(2):
                op("pool", lambda e, i=i: e.tensor_tensor(out=tab[:, 2 * i, :], in0=rp[:, 0:64], in1=gn[:, i, :], op=ALU.mult),
                   reads=[rpr, r_gn], writes=[tabr])
                op("pool", lambda e, i=i: e.tensor_tensor(out=tab[:, 2 * i + 1, :], in0=rp[:, 64:128], in1=gsw[:, i, :], op=ALU.mult),
                   reads=[rpr, r_gn], writes=[tabr])
            state[t].update(tab=tab, tabr=tabr, hT=hT, r_hT=r_hT, ti=ti)

        def stage_b(t):
            st_ = state[t]
            hT, r_hT, ti = st_["hT"], st_["r_hT"], st_["ti"]
            need_q = not (last and t >= LT)
            qi = q32ring.i % 3
            q32, _ = q32ring.next()
            qr = q32res[qi]

            def proj(c0, n, pb, prr):
                for k in range(8):
                    op("pe", lambda e, k=k: e.matmul(pb[:, 0:n], lhsT=hT[:, k, ti * 128:(ti + 1) * 128], rhs=w_in[:, k, c0:c0 + n],
                                                     start=(k == 0), stop=(k == 7)),
                       reads=[r_hT[ti], r_win[k]], writes=[prr])
            if need_q:
                proj(0, 512, bk[0], r_bk[0])
                proj(512, 256, bk[1], r_bk[1])
            proj(1536, 512, bk[2], r_bk[2])
            proj(2048, 256, bk[3], r_bk[3])
            if "nocp" in SKIP:
                st_.update(q32=q32, qr=qr, need_q=need_q)
                return
            if need_q:
                op("act", lambda e: e.activation(out=q32[:, 0:512], in_=bk[0][:, 0:512], func=AF.Copy), reads=[r_bk[0]], writes=[qr[0]])
                op("dve", lambda e: e.tensor_copy(out=q32[:, 512:768], in_=bk[1][:, 0:256]), reads=[r_bk[1]], writes=[qr[1]])
            op("dve", lambda e: e.tensor_copy(out=q32[:, 768:896], in_=bk[2][:, 0:128]), reads=[r_bk[2]], writes=[qr[2]])
            op("dve", lambda e: e.tensor_copy(out=q32[:, 896:1152], in_=bk[2][:, 256:512]), reads=[r_bk[2]], writes=[qr[3]])
            if "nov" not in SKIP:
                op("dve", lambda e: e.tensor_copy(out=Va[:, t, :, 0:64], in_=bk[2][:, 128:256].rearrange("p (g d) -> p g d", d=64)),
                   reads=[r_bk[2]], writes=[r_q[t]])
                op("dve" if "vdve" in SKIP else "act", (lambda e: e.tensor_copy(out=Vb[:, t, :, 0:64], in_=bk[3][:, 0:256].rearrange("p (g d) -> p g d", d=64))) if "vdve" in SKIP else
                   (lambda e: e.activation(out=Vb[:, t, :, 0:64], in_=bk[3][:, 0:256].rearrange("p (g d) -> p g d", d=64), func=AF.Copy)),
                   reads=[r_bk[3]], writes=[r_q[t]])
            st_.update(q32=q32, qr=qr, need_q=need_q)

        def stage_c(t):
            st_ = state[t]
            q32, qr, need_q, tab, tabr = st_["q32"], st_["qr"], st_["need_q"], st_["tab"], st_["tabr"]
            lo = 0 if need_q else 768
            hl = lo // 64
            qrs = qr if need_q else qr[2:4]
            sq, sqr = sqring.next()
            sm, smr = smring.next()
            op("act", lambda e: e.activation(out=sq[:, lo:1152], in_=q32[:, lo:1152], func=AF.Square), reads=qrs, writes=[sqr])
            op("dve", lambda e: e.tensor_reduce(out=sm[:, hl:18], in_=sq[:, lo:1152].rearrange("p (h d) -> p h d", d=64),
                                                axis=AX.X, op=ALU.add), reads=[sqr], writes=[smr])
            op("act", lambda e: e.activation(out=sm[:, 32 + hl:50], in_=sm[:, hl:18], func=AF.Sqrt, bias=EPS, scale=1.0 / 64),
               reads=[smr], writes=[smr])
            op("dve", lambda e: e.reciprocal(out=sm[:, 64 + hl:82], in_=sm[:, 32 + hl:50]), reads=[smr], writes=[smr])
            t1, t1r = t1ring.next()
            t2, t2r = t2ring.next()

            def v3(ap, c0, nh):
                return ap[:, c0:c0 + nh * 64].rearrange("p (h d) -> p h d", d=64)

            def bc(ap2, nh):
                return ap2.unsqueeze(1).to_broadcast([128, nh, 64])
            segs = []
            if need_q:
                segs += [(0, 8, tab[:, 0, :], [tabr], qr[0]), (512, 4, gn[:, 2, :], [r_gn], qr[1])]
            segs += [(768, 2, tab[:, 2, :], [tabr], qr[2]), (896, 4, gn[:, 3, :], [r_gn], qr[3])]
            for (c0, nh, tb_, tres, qres_) in segs:
                op("pool", lambda e, c0=c0, nh=nh, tb_=tb_: e.tensor_tensor(out=v3(t1, c0, nh), in0=v3(q32, c0, nh), in1=bc(tb_, nh), op=ALU.mult),
                   reads=[qres_] + tres, writes=[t1r])
            ropes = ([(0, 0, 8, 1, qr[0])] if need_q else []) + [(768, 512, 2, 3, qr[2])]
            for (c0, d0_, nh, ci, qres_) in ropes:
                s5 = q32[:, c0:c0 + nh * 64].rearrange("p (h a s d) -> p h a s d", a=2, s=2, d=16)
                t25 = t2[:, d0_:d0_ + nh * 64].rearrange("p (h a s d) -> p h a s d", a=2, s=2, d=16)
                sn5 = tab[:, ci, :].rearrange("p (a s d) -> p a s d", a=2, s=2, d=16)
                for s_ in range(2):
                    for a_ in range(2):
                        snb = sn5[:, a_, s_, :].unsqueeze(1).to_broadcast([128, nh, 16])
                        op("dve", lambda e, s_=s_, a_=a_, snb=snb, s5=s5, t25=t25: e.tensor_tensor(
                            out=t25[:, :, a_, s_, :], in0=s5[:, :, a_, 1 - s_, :], in1=snb, op=ALU.mult),
                            reads=[qres_, tabr], writes=[t2r])
                op("pool", lambda e, c0=c0, d0_=d0_, nh=nh: e.tensor_tensor(out=t1[:, c0:c0 + nh * 64], in0=t1[:, c0:c0 + nh * 64],
                                                                           in1=t2[:, d0_:d0_ + nh * 64], op=ALU.add),
                   reads=[t1r, t2r], writes=[t1r])
            qb_, qbr = qbring.next()
            if need_q:
                dv = qb_[:, 0:512].rearrange("p (j g d) -> p g j d", j=4, g=2)
                iv = t1[:, 0:512].rearrange("p (g j d) -> p g j d", j=4, g=2)
                rv = sm[:, 64:72].rearrange("p (g j) -> p g j", g=2).unsqueeze(3).to_broadcast([128, 2, 4, 64])
                op("dve", lambda e: e.tensor_tensor(out=dv, in0=iv, in1=rv, op=ALU.mult), reads=[t1r, smr], writes=[qbr])
                lo2, hl2 = 512, 8
            else:
                lo2, hl2 = 768, 12
            nh2 = 18 - hl2
            op("dve", lambda e: e.tensor_tensor(out=v3(qb_, lo2, nh2), in0=v3(t1, lo2, nh2),
                                                in1=sm[:, 64 + hl2:82].unsqueeze(2).to_broadcast([128, nh2, 64]), op=ALU.mult),
               reads=[t1r, smr], writes=[qbr])
            st_.update(qb=qb_, qbr=qbr)

        def stage_d(t):
            st_ = state.pop(t)
            qb_, qbr, need_q = st_["qb"], st_["qbr"], st_["need_q"]
            tsl = slice(t * 128, (t + 1) * 128)
            blocks = ([(j, j * 128) for j in range(4)] + [(4, 512), (5, 640)] if need_q else []) + [(6, 768), (7, 896)]
            for (slot, c0) in blocks:
                op("pe", lambda e, slot=slot, c0=c0: e.transpose(out=tpQ[:, slot, :], in_=qb_[:, c0:c0 + 128], identity=identb[:]),
                   reads=[qbr, r_id], writes=[r_tpQ])
            op("pe", lambda e: e.transpose(out=tpQ2[:, 0, :], in_=qb_[:, 1024:1152], identity=identb[:]), reads=[qbr, r_id], writes=[r_tpQ2])
            if need_q:
                op("dve", lambda e: e.tensor_copy(out=qTa[:, :, tsl], in_=tpQ[:, 0:4, :]), reads=[r_tpQ], writes=[r_q[t]])
                op("act", lambda e: e.activation(out=qTb[:, :, tsl], in_=tpQ[:, 4:6, :], func=AF.Copy), reads=[r_tpQ], writes=[r_q[t]])
            op("dve", lambda e: e.tensor_copy(out=kTa[:, tsl], in_=tpQ[:, 6, :]), reads=[r_tpQ], writes=[r_q[t]])
            op("act", lambda e: e.activation(out=kTb[:, 0, tsl], in_=tpQ[:, 7, :], func=AF.Copy), reads=[r_tpQ], writes=[r_q[t]])
            op("dve", lambda e: e.tensor_copy(out=kTb[:, 1, tsl], in_=tpQ2[:, 0, :]), reads=[r_tpQ2], writes=[r_q[t]])

        def cgroup(t0, ntl):
            grp = (t0 // 4) % 2
            hT, r_hT = hTs[grp], r_hTs[grp]
            ntok = ntl * 128
            z0 = zcol(t0)

            def mm(chunk):
                c0 = 768 + chunk * 128
                for k in range(8):
                    op("pe", lambda e, k=k: e.matmul(CG[:, 0:ntok], lhsT=w_in[:, k, c0:c0 + 128], rhs=hT[:, k, 0:ntok],
                                                     start=(k == 0), stop=(k == 7)),
                       reads=[r_win[k]] + r_hT[0:ntl], writes=[r_CG])

            def one(j):
                mm(2 + j)
                c1, c1r = czring.next()
                op("act", lambda e: e.activation(out=c1[:, 0:ntok], in_=CG[:, 0:ntok], func=AF.Copy), reads=[r_CG], writes=[c1r])
                mm(4 + j)
                c2, c2r = czring.next()
                op("dve", lambda e: e.tensor_tensor(out=c2[:, 0:ntok], in0=CG[:, 0:ntok], in1=c1[:, 0:ntok], op=ALU.mult),
                   reads=[r_CG, c1r], writes=[c2r])
                op("sp", lambda e: e.dma_start(out=K.czT[0, j * 128:(j + 1) * 128, z0:z0 + ntok], in_=c2[:, 0:ntok]),
                   reads=[c2r], writes=[r_cz], dma=True)
                mm(j)
                c3, c3r = czring.next()
                op("act", lambda e: e.activation(out=c3[:, 0:ntok], in_=CG[:, 0:ntok], func=AF.Copy), reads=[r_CG], writes=[c3r])
                op("sp", lambda e: e.dma_start(out=K.czT[1, j * 128:(j + 1) * 128, z0:z0 + ntok], in_=c3[:, 0:ntok]),
                   reads=[c3r], writes=[r_cz], dma=True)
            for j in range(2):
                one(j)

        stage_a1(0)
        stage_a2(0)
        for t in range(NT):
            if t + 1 < NT:
                stage_a1(t + 1)
            if t >= 1:
                stage_c(t - 1)
            if t + 1 < NT:
                stage_a2(t + 1)
            stage_b(t)
            if t >= 1:
                stage_d(t - 1)
            if t % 4 == 3 or t == NT - 1:
                cgroup(t - t % 4, t % 4 + 1)
        stage_c(NT - 1)
        stage_d(NT - 1)

    pt2ring = ptring = sbring = ptbring = rdring = bcring = ocring = sqbring = sqcring = OaT = ObT = ocT = r_Oa = r_Ob = r_Oc = zring = cpring = yring = accring = rsring = xring = garing = None

    def alloc_attn_bufs():
        nonlocal pt2ring, ptring, sbring, ptbring, rdring, bcring, ocring, sqbring, sqcring, OaT, ObT, ocT, r_Oa, r_Ob, r_Oc, zring, cpring, yring, accring, rsring, xring, garing
        ptring = Ring(K, "pt", [128, 512], BF16, 1)
        pt2ring = Ring(K, "pt2", [128, 2, 512], BF16, 3)
        sbring = Ring(K, "sbb", [128, 256], F32, 2)
        ptbring = Ring(K, "ptb", [128, 256], BF16, 16)
        rdring = Ring(K, "rd", [128, 512], F32, 2)
        bcring = Ring(K, "bcs", [64, 512], F32, 2)
        ocring = Ring(K, "ocp", [128, 512], F32, 2)
        sqbring = Ring(K, "sqb", [128, 512], BF16, 2)
        sqcring = Ring(K, "sqc", [128, 512], BF16, 2)
        for i_ in range(2):
            op("pool", lambda e, i_=i_: e.memset(sqbring.t[i_][:], 0.0), writes=[sqbring.r[i_]])
        OaT = K.sb("OaT", [64, 8, 512], BF16)
        ObT = K.sb("ObT", [64, 4, 512], BF16)
        ocT = K.sb("ocT", [128, 2, 512], BF16)
        r_Oa = [Res() for _ in range(8)]
        r_Ob = [Res() for _ in range(4)]
        r_Oc = [Res() for _ in range(2)]
        zring = Ring(K, "zz", [128, 516], F32, 2)
        cpring = Ring(K, "cp", [128, 512], F32, 2)
        yring = Ring(K, "ya", [128, 512], F32, 2)
        accring = Ring(K, "aca", [128, D], F32, 2)
        rsring = Ring(K, "rsa", [128, 16], F32, 2)
        xring = Ring(K, "xb", [128, D], F32, 2)
        garing = Ring(K, "ga", [128, D], F32, 1)

    def normalise(ob, obr, n, dst_fn, dst_res):
        oc_, ocr = ocring.next()
        op("dve", lambda e: e.tensor_copy(out=oc_[0:65, 0:n], in_=ob[0:65, 0:n]), reads=[obr], writes=[ocr])
        rd, rdr = rdring.next()
        op("dve", lambda e: e.reciprocal(out=rd[64:65, 0:n], in_=oc_[64:65, 0:n]), reads=[ocr], writes=[rdr])
        bcs, bcr = bcring.next()
        for h0 in range(0, n, 256):
            hn = min(256, n - h0)
            op("pe", lambda e, h0=h0, hn=hn: e.matmul(BC[0:64, 0:hn], lhsT=ones_f[64:65, 0:64], rhs=rd[64:65, h0:h0 + hn],
                                                      start=True, stop=True), reads=[rdr, r_one], writes=[r_BC])
            op("dve", lambda e, h0=h0, hn=hn: e.tensor_copy(out=bcs[:, h0:h0 + hn], in_=BC[0:64, 0:hn]), reads=[r_BC], writes=[bcr])
        dst_fn(oc_, bcs, ocr, bcr)

    def ss_cols(srcT, r_src, np_, n, col0, stride):
        if "noss" in SKIP:
            return
        sqb, sqr = (sqbring if np_ == 64 else sqcring).next()
        op("dve", lambda e: e.tensor_tensor(out=sqb[0:np_, 0:n], in0=srcT, in1=srcT, op=ALU.mult), reads=[r_src], writes=[sqr])
        for ti in range(n // 128):
            c = 256 + 2 * (col0 + ti * stride)
            op("pe", lambda e, ti=ti, c=c: e.matmul(BC[:, c:c + 2], lhsT=sqb[:, ti * 128:(ti + 1) * 128], rhs=ones_b[:, 0:2],
                                                    start=True, stop=True), reads=[sqr, r_one], writes=[r_SS])

    def attn_block(b, q0, n, ctxq):
        ntl = n // 128
        t_first = q0 // 128
        qres = [r_q[t_first + i] for i in range(ntl)]
        kcs = list(range(LT, NT)) if ctxq else list(range(NT))
        items = [(j, kc) for j in range(4) for kc in kcs]

        def a_st(i):
            j, kc = items[i]
            sp_ = stp[i % 2]
            r0_, r1_ = (r_bk[0], r_bk[1]) if i % 2 == 0 else (r_bk[6], r_bk[7])
            for g, rr_ in ((0, r0_), (1, r1_)):
                op("pe", lambda e, g=g: e.matmul(sp_[:, g, 0:n], lhsT=kTa[64 * g:64 * g + 64, kc * 128:(kc + 1) * 128],
                                                 rhs=qTa[64 * g:64 * g + 64, j, q0:q0 + n], start=True, stop=True),
                   reads=[r_q[kc]] + qres, writes=[rr_])
            pt, ptr = pt2ring.next()
            op("act", lambda e: e.activation(out=pt[:, :, 0:n], in_=sp_[:, :, 0:n], func=AF.Exp, scale=0.125),
               reads=[r0_, r1_], writes=[ptr])
            return pt, ptr

        def a_pv(i, ptp):
            j, kc = items[i]
            pt, ptr = ptp
            for g in range(2):
                ob, obr = OB[g]
                if "nopv" not in SKIP:
                    op("pe", lambda e, g=g, ob=ob: e.matmul(ob[0:65, 0:n], lhsT=Va[:, kc, g, :], rhs=pt[:, g, 0:n],
                                                           start=(kc == kcs[0]), stop=(kc == kcs[-1])),
                       reads=[ptr, r_q[kc]], writes=[obr])
            if kc == kcs[-1] and "nonorm" not in SKIP:
                for g in range(2):
                    h = 4 * g + j
                    ob, obr = OB[g]

                    def fin(ob_, bcs, obr_, bcr, h=h):
                        op("dve", lambda e: e.tensor_tensor(out=OaT[:, h, 0:n], in0=ob_[0:64, 0:n], in1=bcs[:, 0:n], op=ALU.mult),
                           reads=[obr_, bcr], writes=[r_Oa[h]])
                    normalise(ob, obr, n, fin, None)
                    ss_cols(OaT[:, h, 0:n], r_Oa[h], 64, n, h, 8)

        if "noA" in SKIP:
            items = []
        cur = a_st(0) if items else None
        for i in range(len(items)):
            nxt = a_st(i + 1) if i + 1 < len(items) else None
            a_pv(i, cur)
            cur = nxt

        nrow = n // 64
        bitems = []
        for rr in range(nrow):
            if ctxq:
                chunks = [(LT, None, None), (LT + 1, None, None)]
            else:
                r = q0 // 64 + rr
                rs = min(max(r - 4, 0), 24)
                kt0 = rs // 2
                nch = 5 if rs % 2 else 4
                chunks = []
                for c in range(nch):
                    drs = []
                    for s_ in range(2):
                        kr = 2 * (kt0 + c) + s_
                        drs.append(kr - r + 7 if rs <= kr <= rs + 7 else 15)
                    chunks.append((kt0 + c, drs[0], drs[1]))
                chunks += [(LT, None, None), (LT + 1, None, None)]
            for ci, ch in enumerate(chunks):
                bitems.append((rr, ch, ci == 0, ci == len(chunks) - 1))

        def b_st(i):
            rr, (kt, d0, d1), first, lastc = bitems[i]
            qa = q0 + rr * 64
            for h in (0, 2, 1, 3):
                pr, hf = h // 2, h % 2
                stb, str_ = ST4[2 * (i % 2) + hf]
                op("pe", lambda e, h=h, pr=pr, hf=hf, stb=stb: e.matmul(stb[:, pr * 64:(pr + 1) * 64], lhsT=kTb[64 * hf:64 * hf + 64, pr, kt * 128:(kt + 1) * 128],
                                                                        rhs=qTb[64 * hf:64 * hf + 64, pr, qa:qa + 64], start=True, stop=True),
                   reads=[r_q[kt], r_q[qa // 128]], writes=[str_])
            pt, ptr = ptbring.next()
            ptv = pt[:, 0:256].rearrange("p (pr hf c) -> p hf pr c", pr=2, hf=2)
            for hf in range(2):
                stb, str_ = ST4[2 * (i % 2) + hf]
                sv = stb[:, 0:128].rearrange("p (pr c) -> p pr c", pr=2)
                if d0 is None:
                    op("act", lambda e, hf=hf, sv=sv: e.activation(out=ptv[:, hf], in_=sv, func=AF.Exp, scale=0.125), reads=[str_], writes=[ptr])
                else:
                    sbb, sbr = sbring.next()
                    bv_ = sbb[:, 0:128].rearrange("p (pr c) -> p pr c", pr=2)
                    for s_, dd in ((0, d0), (1, d1)):
                        ps_ = slice(64 * s_, 64 * s_ + 64)
                        tv = tbl[ps_, dd, :, :].rearrange("p (pr hf) c -> p hf pr c", hf=2)[:, hf]
                        op("dve", lambda e, ps_=ps_, tv=tv, sv=sv, bv_=bv_: e.scalar_tensor_tensor(
                            out=bv_[ps_], in0=sv[ps_], scalar=0.125, in1=tv, op0=ALU.mult, op1=ALU.add), reads=[str_, r_tbl], writes=[sbr])
                    op("act", lambda e, hf=hf, bv_=bv_: e.activation(out=ptv[:, hf], in_=bv_, func=AF.Exp), reads=[sbr], writes=[ptr])
            return pt, ptr

        def b_pv_row(rr, pts):
            ob, obr = OB[(rr // 2) % 2]
            cbase = (rr % 2) * 256
            nchk = len(pts)
            for h in range(4):
                for ci, (kt, pt, ptr) in enumerate(pts):
                    op("pe", lambda e, h=h, kt=kt, pt=pt, ci=ci: e.matmul(ob[0:65, cbase + h * 64:cbase + (h + 1) * 64], lhsT=Vb[:, kt, h, :],
                                                                        rhs=pt[:, h * 64:(h + 1) * 64], start=(ci == 0), stop=(ci == nchk - 1)),
                       reads=[ptr, r_q[kt]], writes=[obr])
            if rr % 2 == 1 and "nobnorm" not in SKIP:
                r0 = rr - 1

                def fin(ob_, bcs, obr_, bcr):
                    for h in range(4):
                        ov = ObT[:, h, r0 * 64:r0 * 64 + 128].rearrange("p (r c) -> p r c", r=2)
                        iv = ob_[0:64, :].rearrange("p (r h c) -> p r h c", r=2, h=4)[:, :, h, :]
                        bv = bcs[:, :].rearrange("p (r h c) -> p r h c", r=2, h=4)[:, :, h, :]
                        op("dve", lambda e, ov=ov, iv=iv, bv=bv: e.tensor_tensor(out=ov, in0=iv, in1=bv, op=ALU.mult),
                           reads=[obr_, bcr], writes=[r_Ob[h]])
                normalise(ob, obr, 512, fin, None)

        def b_st_row(rr):
            idxs = [i for i in range(len(bitems)) if bitems[i][0] == rr]
            out = []
            for i in idxs:
                pt, ptr = b_st(i)
                out.append((bitems[i][1][0], pt, ptr))
            return out

        if "noB" in SKIP:
            bitems = []
        nrows_b = nrow if bitems else 0
        curp = b_st_row(0) if nrows_b else None
        for rr in range(nrows_b):
            nxtp = b_st_row(rr + 1) if rr + 1 < nrows_b else None
            b_pv_row(rr, curp)
            curp = nxtp
        for h in range(4):
            if "noB" not in SKIP:
                ss_cols(ObT[:, h, 0:n], r_Ob[h], 64, n, 32 + h, 4)

        z0 = zcol(t_first)
        for j in range(2 if "noC" not in SKIP else 0):
            zz, zr = zring.next()
            op("sp", lambda e, j=j, zz=zz: e.dma_start(out=zz[:, 0:n + 2], in_=K.czT[0, j * 128:(j + 1) * 128, z0 - 1:z0 + n + 1]),
               reads=[r_cz], writes=[zr], dma=True)
            cp, cpr = cpring.next()
            op("sp", lambda e, j=j, cp=cp: e.dma_start(out=cp[:, 0:n], in_=K.czT[1, j * 128:(j + 1) * 128, z0:z0 + n]),
               reads=[r_cz], writes=[cpr], dma=True)
            yy, yr = yring.next()
            op("dve", lambda e, j=j, zz=zz, yy=yy: e.tensor_scalar(out=yy[:, 0:n], in0=zz[:, 0:n], scalar1=cwc[:, j, 0:1], scalar2=None, op0=ALU.mult),
               reads=[zr, r_cw], writes=[yr])
            for w in (1, 2):
                op("dve", lambda e, j=j, zz=zz, yy=yy, w=w: e.scalar_tensor_tensor(out=yy[:, 0:n], in0=zz[:, w:w + n], scalar=cwc[:, j, w:w + 1],
                                                                                   in1=yy[:, 0:n], op0=ALU.mult, op1=ALU.add),
                   reads=[zr, r_cw, yr], writes=[yr])
            op("dve", lambda e, j=j, cp=cp, yy=yy: e.scalar_tensor_tensor(out=ocT[:, j, 0:n], in0=yy[:, 0:n], scalar=cwc[:, j, 3:4], in1=cp[:, 0:n],
                                                                          op0=ALU.add, op1=ALU.mult), reads=[yr, cpr, r_cw], writes=[r_Oc[j]])
            ss_cols(ocT[:, j, 0:n], r_Oc[j], 128, n, 48 + j, 2)

        if "oT" in K.dbg:
            for h in range(8):
                op("pool", lambda e, h=h: e.dma_start(out=K.dbg["oT"][b, h * 64:(h + 1) * 64, q0:q0 + n], in_=OaT[:, h, 0:n]),
                   reads=[r_Oa[h]], writes=[Res()], dma=True)
            for h in range(4):
                op("pool", lambda e, h=h: e.dma_start(out=K.dbg["oT"][b, 512 + h * 64:512 + (h + 1) * 64, q0:q0 + n], in_=ObT[:, h, 0:n]),
                   reads=[r_Ob[h]], writes=[Res()], dma=True)
            for j in range(2):
                op("pool", lambda e, j=j: e.dma_start(out=K.dbg["oT"][b, 768 + j * 128:768 + (j + 1) * 128, q0:q0 + n], in_=ocT[:, j, 0:n]),
                   reads=[r_Oc[j]], writes=[Res()], dma=True)
        if "nomerge" in SKIP:
            return
        v = NB if ctxq else b
        gat, gar = garing.next()
        op("sp", lambda e: e.dma_start(out=gat[:], in_=K.modv[l, v, 2 * D:3 * D].partition_broadcast(128)), writes=[gar], dma=True)
        rs_, rsr = rsring.next()
        for gi_, (c0, nh, width) in enumerate(((0, 8, 512.0), (32, 4, 256.0), (48, 2, 256.0))):
            op("dve", lambda e, gi_=gi_, c0=c0, nh=nh: e.tensor_reduce(
                out=rs_[:, gi_ * 4:gi_ * 4 + ntl], in_=BC[:, 256 + 2 * c0:256 + 2 * (c0 + ntl * nh)].rearrange("p (t h two) -> p t h two", h=nh, two=2)[:, :, :, 0],
                axis=AX.X, op=ALU.add), reads=[r_SS], writes=[rsr])
            op("act", lambda e, gi_=gi_, width=width: e.activation(out=rs_[:, gi_ * 4:gi_ * 4 + ntl], in_=rs_[:, gi_ * 4:gi_ * 4 + ntl],
                                                                  func=AF.Sqrt, bias=EPS, scale=1.0 / width), reads=[rsr], writes=[rsr])
        op("dve", lambda e: e.reciprocal(out=rs_[:, 0:12], in_=rs_[:, 0:12]), reads=[rsr], writes=[rsr])
        for ti in range(ntl):
            t = t_first + ti
            tk = slice(ti * 128, (ti + 1) * 128)
            ac, acr = accring.next()
            xt, xr = xring.next()
            op("sp", lambda e, xt=xt, t=t: e.dma_start(out=xt[:], in_=xsrc(K, l, b, t)), writes=[xr], dma=True)
            for cb in range(2):
                cs = slice(cb * 512, (cb + 1) * 512)
                groups = (([(OaT[:, h, tk], woA[:, h, cs], r_Oa[h]) for h in range(8)], 0),
                          ([(ObT[:, h, tk], woB[:, h, cs], r_Ob[h]) for h in range(4)], 1),
                          ([(ocT[:, j, tk], woC[:, j, cs], r_Oc[j]) for j in range(2)], 2))
                for mm, gi_ in groups:
                    opb, opr = OPR[mctr[0] % 3]
                    mctr[0] += 1
                    for i, (lt, rh, rr_) in enumerate(mm):
                        op("pe", lambda e, lt=lt, rh=rh, i=i, nmm=len(mm), opb=opb: e.matmul(opb[:, :], lhsT=lt, rhs=rh, start=(i == 0), stop=(i == nmm - 1)),
                           reads=[rr_] + r_win, writes=[opr])
                    sc1 = rs_[:, gi_ * 4 + ti:gi_ * 4 + ti + 1]
                    if gi_ == 0:
                        op("dve", lambda e, ac=ac, cs=cs, sc1=sc1, opb=opb: e.tensor_scalar(out=ac[:, cs], in0=opb[:, :], scalar1=sc1, scalar2=None, op0=ALU.mult),
                           reads=[opr, rsr], writes=[acr])
                    else:
                        op("dve", lambda e, ac=ac, cs=cs, sc1=sc1, opb=opb: e.scalar_tensor_tensor(out=ac[:, cs], in0=opb[:, :], scalar=sc1, in1=ac[:, cs],
                                                                                          op0=ALU.mult, op1=ALU.add),
                           reads=[opr, rsr, acr], writes=[acr])
            op("pool", lambda e, ac=ac: e.tensor_tensor(out=ac[:], in0=ac[:], in1=gat[:], op=ALU.mult), reads=[acr, gar], writes=[acr])
            op("dve", lambda e, ac=ac, xt=xt: e.tensor_tensor(out=ac[:], in0=ac[:], in1=xt[:], op=ALU.add), reads=[acr, xr], writes=[acr])
            op("sp", lambda e, ac=ac, t=t: e.dma_start(out=K.xcur[b, t * 128:(t + 1) * 128, :], in_=ac[:]), reads=[acr], writes=[Res()], dma=True)

    for b in range(NB):
        load_win()
        with ExitStack() as st2:
            K.stk.append(st2)
            pre_stage(b)
            K.flush()
            K.stk.pop()
        load_wout()
        if "noattn" in SKIP:
            continue
        with ExitStack() as st2:
            K.stk.append(st2)
            alloc_attn_bufs()
            for qb in range(4):
                attn_block(b, qb * 512, 512, False)
            if not last:
                attn_block(b, SEQ, NCTX, True)
            K.flush()
            K.stk.pop()


def _rope_table():
    t = np.arange(SEQ)
    row = (t // GW).astype(np.float32)
    col = (t % GW).astype(np.float32)
    half = HD // 2
    inv = (np.float32(10000.0) ** (-np.arange(0, half, 2, dtype=np.float32) / np.float32(half))).astype(np.float32)
    ar = row[:, None] * inv
    ac = col[:, None] * inv
    cr, sr, cc, sc = np.cos(ar), np.sin(ar), np.cos(ac), np.sin(ac)
    tab = np.zeros((NKEY, 128), np.float32)
    tab[:SEQ, 0:64] = np.concatenate([cr, cr, cc, cc], 1)
    tab[:SEQ, 64:128] = np.concatenate([-sr, sr, -sc, sc], 1)
    tab[SEQ:, 0:64] = 1.0
    return tab


def _rpb_layout(rpb):
    cc = np.arange(GW)
    cs = np.clip(cc - 8, 0, GW - 16)
    dc_idx = np.clip(cc[None, :] - cc[:, None] + 15, 0, 30)
    in_win = (cc[None, :] >= cs[:, None]) & (cc[None, :] < cs[:, None] + 16)
    g = rpb[:, :, :, dc_idx]
    g = np.where(in_win[None, None, None], g, np.float32(NEG))
    return np.ascontiguousarray(g.transpose(0, 2, 4, 1, 3)).astype(np.float32)


def make_in_maps(inp, cores=range(8)):
    f = lambda a: np.ascontiguousarray(np.asarray(a, dtype=np.float32))
    shared = {
        "w_mod": f(inp["w_mod"]), "b_mod": f(inp["b_mod"]), "w_in": f(inp["w_in"]),
        "gains": f(np.stack([inp["gq_a"], inp["gk_a"], inp["gq_b"], inp["gk_b"]], 1)),
        "rpbT": _rpb_layout(np.asarray(inp["rpb"], np.float32)),
        "conv_w": f(inp["conv_w"]), "conv_b": f(inp["conv_b"]), "g_out": f(inp["g_out"]), "w_out": f(inp["w_out"]),
        "ffn_w1": f(inp["ffn_w1"]), "ffn_w3": f(inp["ffn_w3"]), "ffn_w2": f(inp["ffn_w2"]),
        "moe_router": f(inp["moe_router"]), "moe_router_b": f(inp["moe_router_b"]),
        "moe_w1": f(inp["moe_w1"]), "moe_w3": f(inp["moe_w3"]), "moe_w2": f(inp["moe_w2"]),
        "ident": np.eye(128, dtype=np.float32), "rope": _rope_table(),
    }
    maps = []
    for i in cores:
        m = dict(shared)
        m["x2"] = f(inp["x"][NB * i:NB * i + NB])
        m["ctx2"] = f(inp["ctx"][NB * i:NB * i + NB])
        m["cvec"] = f(np.concatenate([inp["c"][NB * i:NB * i + NB], np.asarray(inp["c_ctx"])[None]], 0))
        maps.append(m)
    return maps


_NC_CACHE = {}


def kernel(**inputs):
    if "nc" not in _NC_CACHE:
        _NC_CACHE["nc"] = build()
    nc = _NC_CACHE["nc"]
    maps = make_in_maps(inputs)
    res = run_bass_kernel_spmd(nc, maps, core_ids=list(range(8)))
    return np.concatenate([np.asarray(r["y"], dtype=np.float32) for r in res.results], axis=0)
```

```python
from contextlib import ExitStack

import numpy as np
import concourse.bass as bass
import concourse.mybir as mybir
from concourse.bass_utils import run_bass_kernel_spmd

F32 = mybir.dt.float32
BF16 = mybir.dt.bfloat16
AF = mybir.ActivationFunctionType
ALU = mybir.AluOpType
AX = mybir.AxisListType

D = 1024
SEQ = 2048
NCTX = 256
NB = 2
DEPTH = 2
GW = 64
HD = 64
INW = 2304
DFF = 2816
NF = DFF // 128
NE = 8
EPS = 1e-6
NEG = -30000.0
LT = SEQ // 128
CT = NCTX // 128
NT = LT + CT
NKEY = SEQ + NCTX

ENGS = ("pe", "act", "dve", "pool", "sp")
SKIP = set()
NROT = 8


class Res:
    __slots__ = ("w", "r")
    ALL = []

    def __init__(self):
        self.w = None
        self.r = []
        Res.ALL.append(self)


class Op:
    __slots__ = ("eng", "fn", "deps", "dma", "needs_inc", "tok")

    def __init__(self, eng, fn, dma):
        self.eng = eng
        self.fn = fn
        self.dma = dma
        self.deps = []
        self.needs_inc = False
        self.tok = None


class Sched:
    def __init__(self, sems, dsems):
        self.sems = sems
        self.dsems = dsems
        self.cnt = {e: 0 for e in ENGS}
        self.ndma = {e: 0 for e in ENGS}
        self.waited = {e: {} for e in ENGS}
        self.total = 0
        self.reset()

    def reset(self):
        self.ops = {e: [] for e in ENGS}
        self.dma_hist = {e: [] for e in ENGS}

    def op(self, eng, fn, reads=(), writes=(), dma=False):
        o = Op(eng, fn, dma)
        deps = {}
        for r in reads:
            if r.w is not None:
                deps[id(r.w)] = (r.w, True)
        for w in writes:
            if w.w is not None and id(w.w) not in deps:
                deps[id(w.w)] = (w.w, False)
            for rd in w.r:
                if id(rd) not in deps:
                    deps[id(rd)] = (rd, False)
        for p, raw in deps.values():
            if p is o:
                continue
            if p.eng == eng and not p.dma and not dma:
                if eng == "pe" or not raw:
                    continue
            o.deps.append(p)
            p.needs_inc = True
        if dma:
            h = self.dma_hist[eng]
            if len(h) >= NROT:
                o.deps.append(h[-NROT])
            h.append(o)
            o.needs_inc = True
        for r in reads:
            r.r.append(o)
        for w in writes:
            w.w = o
            w.r = []
        self.ops[eng].append(o)
        self.total += 1
        return o

    def finish_block(self):
        tail = []
        for e in ENGS:
            tail += self.dma_hist[e][-NROT:]
        o = Op("sp", lambda e: e.nop(), False)
        o.deps = tail
        self.ops["sp"].append(o)
        for r in Res.ALL:
            r.w = None
            r.r = []

    def emit(self, block):
        for e in ENGS:
            for o in self.ops[e]:
                if o.dma:
                    nd = self.ndma[e]
                    o.tok = (self.dsems[e][nd % NROT], 16 * (nd // NROT + 1))
                    self.ndma[e] = nd + 1
                elif o.needs_inc:
                    self.cnt[e] += 1
                    o.tok = (self.sems[e], self.cnt[e])

        def run(e, engine):
            waited = self.waited[e]
            for o in self.ops[e]:
                need = {}
                for p in o.deps:
                    s, v = p.tok
                    k = s.num
                    if waited.get(k, 0) >= v:
                        continue
                    if k not in need or need[k][1] < v:
                        need[k] = (s, v)
                for k, (s, v) in need.items():
                    engine.wait_ge(s, v)
                    waited[k] = v
                ins = o.fn(engine)
                if o.dma:
                    ins.then_inc(o.tok[0], 16)
                elif o.needs_inc:
                    ins.then_inc(o.tok[0], 1)

        block.tensor(lambda pe: run("pe", pe))
        block.scalar(lambda act: run("act", act))
        block.vector(lambda dve: run("dve", dve))
        block.gpsimd(lambda pool: run("pool", pool))
        block.sync(lambda sp: run("sp", sp))
        self.reset()


class Ring:
    def __init__(self, K, name, shape, dt, n, psum=False):
        self.t = [(K.ps if psum else K.sb)("%s%d" % (name, i), shape, dt) for i in range(n)]
        self.r = [Res() for _ in range(n)]
        self.i = 0

    def next(self):
        j = self.i % len(self.t)
        self.i += 1
        return self.t[j], self.r[j]


class Kern:
    pass


def build(phases=("mod", "attn0", "ffn0", "attn1", "ffn1"), dbg=False):
    nc = bass.Bass("TRN2", target_bir_lowering=False)
    K = Kern()
    K.nc = nc

    def din(name, shape):
        return nc.dram_tensor(name, list(shape), F32, kind="ExternalInput").ap()

    K.x2 = din("x2", [NB, SEQ, D])
    K.ctx2 = din("ctx2", [NB, NCTX, D])
    K.cvec = din("cvec", [NB + 1, D])
    K.w_mod = din("w_mod", [DEPTH, D, 6 * D])
    K.b_mod = din("b_mod", [DEPTH, 6 * D])
    K.w_in = din("w_in", [DEPTH, D, INW])
    K.gains = din("gains", [DEPTH, 4, HD])
    K.rpbT = din("rpbT", [DEPTH, 15, 64, 4, 64])
    K.conv_w = din("conv_w", [DEPTH, 3, 256])
    K.conv_b = din("conv_b", [DEPTH, 256])
    K.g_out = din("g_out", [DEPTH, D])
    K.w_out = din("w_out", [DEPTH, D, D])
    K.ffn_w1 = din("ffn_w1", [1, D, DFF])
    K.ffn_w3 = din("ffn_w3", [1, D, DFF])
    K.ffn_w2 = din("ffn_w2", [1, DFF, D])
    K.moe_router = din("moe_router", [1, D, NE])
    K.moe_router_b = din("moe_router_b", [1, NE])
    K.moe_w1 = din("moe_w1", [1, NE, D, DFF])
    K.moe_w3 = din("moe_w3", [1, NE, D, DFF])
    K.moe_w2 = din("moe_w2", [1, NE, DFF, D])
    K.ident = din("ident", [128, 128])
    K.rope = din("rope", [NKEY, 128])
    K.y = nc.dram_tensor("y", [NB, SEQ, D], F32, kind="ExternalOutput").ap()
    K.xcur = nc.dram_tensor("xcur", [NB, NKEY, D], F32, kind="Internal").ap()
    K.modv = nc.dram_tensor("modv", [DEPTH, NB + 1, 6 * D], F32, kind="Internal").ap()
    K.czT = nc.dram_tensor("czT", [2, 256, 2308], F32, kind="Internal").ap()
    K.dbg = {}
    if dbg:
        K.dbg["modv"] = nc.dram_tensor("dbg_modv", [DEPTH, NB + 1, 6 * D], F32, kind="ExternalOutput").ap()
        K.dbg["xcur"] = nc.dram_tensor("dbg_xcur", [NB, NKEY, D], F32, kind="ExternalOutput").ap()
        K.dbg["oT"] = nc.dram_tensor("dbg_oT", [NB, D, NKEY], F32, kind="ExternalOutput").ap()

    with ExitStack() as gst:
        sems = {e: gst.enter_context(nc.semaphore("s_" + e)) for e in ENGS}
        dsems = {}
        for e in ("sp", "pool", "act"):
            dsems[e] = [gst.enter_context(nc.semaphore("d_%s%d" % (e, i))) for i in range(NROT)]
        dsems["pe"] = dsems["sp"]
        dsems["dve"] = dsems["sp"]
        K.S = Sched(sems, dsems)
        K.outs = []

        for ph in phases:
            with ExitStack() as st:
                K.st = st
                K.stk = [st]
                K.uid = getattr(K, "uid", 0)

                def _sb(name, shape, dt, ph=ph):
                    K.uid += 1
                    return K.stk[-1].enter_context(nc.sbuf_tensor("%s_%s_%d" % (ph, name, K.uid), list(shape), dt))

                def _ps(name, shape, dt, ph=ph):
                    K.uid += 1
                    return K.stk[-1].enter_context(nc.psum_tensor("%s_%s_%d" % (ph, name, K.uid), list(shape), dt))

                def _flush():
                    if sum(len(v) for v in K.S.ops.values()) == 0:
                        return
                    K.S.finish_block()
                    with nc.Block() as block:
                        K.S.emit(block)
                K.sb, K.ps, K.flush = _sb, _ps, _flush
                if ph == "mod":
                    phase_mod(K)
                elif ph.startswith("attn"):
                    phase_attn(K, int(ph[4:]))
                elif ph.startswith("ffn"):
                    phase_ffn(K, int(ph[3:]))
                elif ph == "dbgcopy":
                    phase_dbgcopy(K)
                elif ph == "initx":
                    phase_initx(K)
                K.flush()
    return nc


def xsrc(K, l, b, t):
    if l == 0:
        if t < LT:
            return K.x2[b, t * 128:(t + 1) * 128, :]
        return K.ctx2[b, (t - LT) * 128:(t - LT + 1) * 128, :]
    return K.xcur[b, t * 128:(t + 1) * 128, :]


def phase_mod(K):
    nc, S = K.nc, K.S
    NV = NB + 1
    cT = K.sb("cT", [128, 8, NV], F32)
    r_cT = Res()
    scT = K.sb("scT", [128, 8, NV], BF16)
    r_scT = Res()
    for v in range(NV):
        S.op("sp", lambda e, v=v: e.dma_start(out=cT[:, :, v], in_=K.cvec[v].rearrange("(k p) -> p k", p=128),
                                              allow_slow_non_contiguous=True), writes=[r_cT], dma=True)
    S.op("act", lambda e: e.activation(out=scT[:], in_=cT[:], func=AF.Silu), reads=[r_cT], writes=[r_scT])
    wring = Ring(K, "wm", [128, 8, 512], BF16, 3)
    bring = Ring(K, "bm", [NV, 512], F32, 2)
    oring = Ring(K, "om", [NV, 512], F32, 2)
    pring = Ring(K, "pm", [NV, 512], F32, 2, psum=True)
    for l in range(DEPTH):
        for cb in range(12):
            wt, wr = wring.next()
            S.op("pool", lambda e, wt=wt, l=l, cb=cb: e.dma_start(
                out=wt[:], in_=K.w_mod[l, :, cb * 512:(cb + 1) * 512].rearrange("(k p) c -> p k c", p=128)),
                writes=[wr], dma=True)
            bt, br = bring.next()
            S.op("sp", lambda e, bt=bt, l=l, cb=cb: e.dma_start(
                out=bt[:], in_=K.b_mod[l, cb * 512:(cb + 1) * 512].partition_broadcast(NV)),
                writes=[br], dma=True)
            pt, pr = pring.next()
            for k in range(8):
                S.op("pe", lambda e, pt=pt, wt=wt, k=k: e.matmul(pt[:], lhsT=scT[:, k, :], rhs=wt[:, k, :],
                                                                 start=(k == 0), stop=(k == 7)),
                     reads=[r_scT, wr], writes=[pr])
            ot, orr = oring.next()
            S.op("dve", lambda e, ot=ot, pt=pt, bt=bt: e.tensor_tensor(out=ot[:], in0=pt[:], in1=bt[:], op=ALU.add),
                 reads=[pr, br], writes=[orr])
            S.op("sp", lambda e, ot=ot, l=l, cb=cb: e.dma_start(out=K.modv[l, :, cb * 512:(cb + 1) * 512], in_=ot[:]),
                 reads=[orr], writes=[Res()], dma=True)
            if "modv" in K.dbg:
                S.op("sp", lambda e, ot=ot, l=l, cb=cb: e.dma_start(
                    out=K.dbg["modv"][l, :, cb * 512:(cb + 1) * 512], in_=ot[:]),
                    reads=[orr], writes=[Res()], dma=True)


def load_modcols(K, l, v, idx, name, plus1):
    S = K.S
    t = K.sb(name, [128, 8], F32)
    r = Res()
    S.op("sp", lambda e: e.dma_start(out=t[:], in_=K.modv[l, v, idx * D:(idx + 1) * D].rearrange("(k p) -> p k", p=128),
                                     allow_slow_non_contiguous=True), writes=[r], dma=True)
    if plus1:
        S.op("dve", lambda e: e.tensor_scalar_add(out=t[:], in0=t[:], scalar1=1.0), reads=[r], writes=[r])
    return t, r


def load_bcast(K, src_row, n, name, dt=F32, eng="sp"):
    S = K.S
    t = K.sb(name, [128, n], dt)
    r = Res()
    S.op(eng, lambda e: e.dma_start(out=t[:], in_=src_row.partition_broadcast(128)), writes=[r], dma=True)
    return t, r


def phase_ffn(K, l):
    nc, S = K.nc, K.S
    moe = (l % 2 == 1)
    last = (l == DEPTH - 1)
    E = NE if moe else 1
    if moe:
        W1, W3, W2 = K.moe_w1[0], K.moe_w3[0], K.moe_w2[0]
    else:
        W1, W3, W2 = K.ffn_w1, K.ffn_w3, K.ffn_w2

    ident = K.sb("identf", [128, 128], F32)
    r_ident = Res()
    S.op("sp", lambda e: e.dma_start(out=ident[:], in_=K.ident), writes=[r_ident], dma=True)

    mods = {}
    for v in range(NB + 1):
        if v == NB and last:
            continue
        sh, r_sh = load_modcols(K, l, v, 3, "shf%d" % v, False)
        sc, r_sc = load_modcols(K, l, v, 4, "scf%d" % v, True)
        gf, r_gf = load_bcast(K, K.modv[l, v, 5 * D:6 * D], D, "gf%d" % v)
        mods[v] = (sh, r_sh, sc, r_sc, gf, r_gf)
    if moe:
        wr_t = K.sb("wrt", [128, 8, NE], F32)
        r_wr = Res()
        S.op("sp", lambda e: e.dma_start(out=wr_t[:], in_=K.moe_router[0].rearrange("(k p) e -> p k e", p=128)),
             writes=[r_wr], dma=True)
        br_t, r_br = load_bcast(K, K.moe_router_b[0], NE, "brt")

    TB = 8
    hT = K.sb("hT", [128, 8, TB * 128], BF16)
    r_hT = [Res() for _ in range(TB)]
    uT = K.sb("uT", [128, NF, TB * 128], BF16)
    r_uT = [[Res() for _ in range(2)] for _ in range(NF)]
    acc = K.sb("acc", [128, TB, D], F32)
    r_acc = [[Res(), Res()] for _ in range(TB)]
    G = K.sb("G", [128, TB, NE], F32)
    r_G = [Res() for _ in range(TB)]
    w2b = K.sb("w2b", [128, NF, D], BF16)
    r_w2 = [Res() for _ in range(NF)]
    xring = Ring(K, "xt", [128, D], F32, 2)
    nring = Ring(K, "xn", [128, D], F32, 2)
    jring = Ring(K, "jk", [128, D], BF16, 1)
    sring = Ring(K, "st", [128, 4], F32, 3)
    hfring = Ring(K, "hf", [128, 8, 128], F32, 2)
    w13ring = Ring(K, "w13", [128, 2, 8, 128], BF16, 3)
    sgring = Ring(K, "sg", [128, 512], F32, 2)
    tring = Ring(K, "tp", [128, 4, 128], F32, 1, psum=True)
    lgring = Ring(K, "lg", [128, NE], F32, 1, psum=True)
    gvring = Ring(K, "gv", [128, 2, 512], F32, 2, psum=True)
    oring = Ring(K, "op", [128, 512], F32, 2, psum=True)
    smring = Ring(K, "sm", [128, 4 * NE + 8], F32, 2)
    yring = Ring(K, "yt", [128, D], F32, 2)

    blocks = []
    for b in range(NB):
        blocks.append((b, b, 0, 8))
        blocks.append((b, b, 8, 8))
        if not last:
            blocks.append((b, NB, LT, CT))

    def router(ti, lg, lr):
        sm, mr = smring.next()
        L0, E1, L2, E2, M = sm[:, 0:8], sm[:, 8:16], sm[:, 16:24], sm[:, 24:32], sm[:, 32:40]
        seq = [
            lambda e: e.tensor_tensor(out=L0, in0=lg[:], in1=br_t[:], op=ALU.add),
            lambda e: e.reduce_max(out=M[:, 0:1], in_=L0, axis=AX.X),
            lambda e: e.tensor_scalar(out=E1, in0=L0, scalar1=M[:, 0:1], scalar2=None, op0=ALU.is_equal),
            lambda e: e.scalar_tensor_tensor(out=L2, in0=E1, scalar=-1e30, in1=L0, op0=ALU.mult, op1=ALU.add),
            lambda e: e.reduce_max(out=M[:, 1:2], in_=L2, axis=AX.X),
            lambda e: e.tensor_scalar(out=E2, in0=L2, scalar1=M[:, 1:2], scalar2=None, op0=ALU.is_equal),
            lambda e: e.tensor_tensor(out=M[:, 2:3], in0=M[:, 1:2], in1=M[:, 0:1], op=ALU.subtract),
        ]
        for i, fn in enumerate(seq):
            S.op("dve", (lambda fn: (lambda e: fn(e)))(fn), reads=[mr, lr, r_br] if i == 0 else [mr], writes=[mr])
        S.op("act", lambda e, M=M: e.activation(out=M[:, 3:4], in_=M[:, 2:3], func=AF.Exp), reads=[mr], writes=[mr])
        seq2 = [
            lambda e: e.tensor_scalar_add(out=M[:, 4:5], in0=M[:, 3:4], scalar1=1.0),
            lambda e: e.reciprocal(out=M[:, 5:6], in_=M[:, 4:5]),
            lambda e: e.tensor_tensor(out=M[:, 6:7], in0=M[:, 3:4], in1=M[:, 5:6], op=ALU.mult),
            lambda e: e.tensor_scalar(out=E1, in0=E1, scalar1=M[:, 5:6], scalar2=None, op0=ALU.mult),
        ]
        for fn in seq2:
            S.op("dve", (lambda fn: (lambda e: fn(e)))(fn), reads=[mr], writes=[mr])
        S.op("dve", lambda e, E1=E1, E2=E2, M=M, ti=ti: e.scalar_tensor_tensor(
            out=G[:, ti, :], in0=E2, scalar=M[:, 6:7], in1=E1, op0=ALU.mult, op1=ALU.add),
            reads=[mr], writes=[r_G[ti]])


    def ffn_block(b, v, t0, ntl):
        sh, r_sh, sc, r_sc, gf, r_gf = mods[v]
        for ti in range(ntl):
            t = t0 + ti
            xt, xr = xring.next()
            S.op("sp", lambda e, xt=xt, t=t: e.dma_start(out=xt[:], in_=xsrc(K, 1, b, t)), writes=[xr], dma=True)
            jk, jr = jring.next()
            stt, sr = sring.next()
            S.op("act", lambda e, jk=jk, xt=xt, stt=stt: e.activation(out=jk[:], in_=xt[:], func=AF.Square,
                                                                     scale=float(D ** -0.5), accum_out=stt[:, 0:1]),
                 reads=[xr], writes=[jr, sr])
            S.op("act", lambda e, stt=stt: e.activation(out=stt[:, 1:2], in_=stt[:, 0:1], func=AF.Sqrt, bias=EPS, scale=1.0),
                 reads=[sr], writes=[sr])
            S.op("dve", lambda e, stt=stt: e.reciprocal(out=stt[:, 2:3], in_=stt[:, 1:2]), reads=[sr], writes=[sr])
            xn, nr = nring.next()
            S.op("dve", lambda e, xn=xn, xt=xt, stt=stt: e.tensor_scalar(out=xn[:], in0=xt[:], scalar1=stt[:, 2:3],
                                                                       scalar2=None, op0=ALU.mult),
                 reads=[xr, sr], writes=[nr])
            hf, hr = hfring.next()
            for hh in range(2):
                tp, tr = tring.next()
                for kk in range(4):
                    k = hh * 4 + kk
                    S.op("pe", lambda e, tp=tp, xn=xn, k=k, kk=kk: e.transpose(out=tp[:, kk, :], in_=xn[:, k * 128:(k + 1) * 128],
                                                                             identity=ident[:]),
                         reads=[nr, r_ident], writes=[tr])
                for kk in range(4):
                    k = hh * 4 + kk
                    if moe:
                        S.op("dve", lambda e, hf=hf, tp=tp, k=k, kk=kk: e.tensor_scalar(
                            out=hf[:, k, :], in0=tp[:, kk, :], scalar1=sc[:, k:k + 1], scalar2=sh[:, k:k + 1],
                            op0=ALU.mult, op1=ALU.add), reads=[tr, r_sc, r_sh], writes=[hr])
                    else:
                        S.op("dve", lambda e, tp=tp, k=k, kk=kk, ti=ti: e.tensor_scalar(
                            out=hT[:, k, ti * 128:(ti + 1) * 128], in0=tp[:, kk, :], scalar1=sc[:, k:k + 1],
                            scalar2=sh[:, k:k + 1], op0=ALU.mult, op1=ALU.add),
                            reads=[tr, r_sc, r_sh], writes=[r_hT[ti]])
            if moe:
                S.op("pool", lambda e, hf=hf, ti=ti: e.tensor_copy(out=hT[:, :, ti * 128:(ti + 1) * 128], in_=hf[:]),
                     reads=[hr], writes=[r_hT[ti]])
                lg, lr = lgring.next()
                for k in range(8):
                    S.op("pe", lambda e, lg=lg, hf=hf, k=k: e.matmul(lg[:], lhsT=hf[:, k, :], rhs=wr_t[:, k, :],
                                                                     start=(k == 0), stop=(k == 7)),
                         reads=[hr, r_wr], writes=[lr])
                router(ti, lg, lr)
        NTOK = ntl * 128
        halves = [(h0, min(512, NTOK - h0)) for h0 in range(0, NTOK, 512)]
        for ex in range(E):
            w1e = W1[ex] if moe else W1[0]
            w3e = W3[ex] if moe else W3[0]
            w2e = W2[ex] if moe else W2[0]
            for f in range(NF):
                wt, wr = w13ring.next()
                S.op("pool", lambda e, wt=wt, w1e=w1e, f=f: e.dma_start(
                    out=wt[:, 0, :, :], in_=w1e[:, f * 128:(f + 1) * 128].rearrange("(k p) c -> p k c", p=128)),
                    writes=[wr], dma=True)
                r2 = Res()
                S.op("pool", lambda e, wt=wt, w3e=w3e, f=f: e.dma_start(
                    out=wt[:, 1, :, :], in_=w3e[:, f * 128:(f + 1) * 128].rearrange("(k p) c -> p k c", p=128)),
                    writes=[r2], dma=True)
                S.op("pool", lambda e, w2e=w2e, f=f: e.dma_start(out=w2b[:, f, :], in_=w2e[f * 128:(f + 1) * 128, :]),
                     writes=[r_w2[f]], dma=True)
                for hi, (h0, hn) in enumerate(halves):
                    gv, gr = gvring.next()
                    tiles_in = list(range(h0 // 128, (h0 + hn) // 128))
                    for j in range(2):
                        for k in range(8):
                            S.op("pe", lambda e, gv=gv, wt=wt, j=j, k=k, h0=h0, hn=hn: e.matmul(
                                gv[:, j, 0:hn], lhsT=wt[:, j, k, :], rhs=hT[:, k, h0:h0 + hn], start=(k == 0), stop=(k == 7)),
                                reads=[wr, r2] + [r_hT[i] for i in tiles_in], writes=[gr])
                    sg, sr2 = sgring.next()
                    S.op("act", lambda e, sg=sg, gv=gv, hn=hn: e.activation(out=sg[:, 0:hn], in_=gv[:, 0, 0:hn], func=AF.Silu),
                         reads=[gr], writes=[sr2])
                    S.op("dve", lambda e, sg=sg, gv=gv, f=f, h0=h0, hn=hn: e.tensor_tensor(
                        out=uT[:, f, h0:h0 + hn], in0=gv[:, 1, 0:hn], in1=sg[:, 0:hn], op=ALU.mult),
                        reads=[gr, sr2], writes=[r_uT[f][hi]])
            for ti in range(ntl):
                for cb in range(2):
                    ot, orr = oring.next()
                    for f in range(NF):
                        S.op("pe", lambda e, ot=ot, f=f, ti=ti, cb=cb: e.matmul(
                            ot[:], lhsT=uT[:, f, ti * 128:(ti + 1) * 128], rhs=w2b[:, f, cb * 512:(cb + 1) * 512],
                            start=(f == 0), stop=(f == NF - 1)),
                            reads=[r_uT[f][ti // 4], r_w2[f]], writes=[orr])
                    asl = acc[:, ti, cb * 512:(cb + 1) * 512]
                    if not moe:
                        S.op("dve", lambda e, asl=asl, ot=ot: e.tensor_copy(out=asl, in_=ot[:]),
                             reads=[orr], writes=[r_acc[ti][cb]])
                    elif ex == 0:
                        S.op("dve", lambda e, asl=asl, ot=ot, ti=ti, ex=ex: e.tensor_scalar(
                            out=asl, in0=ot[:], scalar1=G[:, ti, ex:ex + 1], scalar2=None, op0=ALU.mult),
                            reads=[orr, r_G[ti]], writes=[r_acc[ti][cb]])
                    else:
                        S.op("dve", lambda e, asl=asl, ot=ot, ti=ti, ex=ex: e.scalar_tensor_tensor(
                            out=asl, in0=ot[:], scalar=G[:, ti, ex:ex + 1], in1=asl, op0=ALU.mult, op1=ALU.add),
                            reads=[orr, r_G[ti], r_acc[ti][cb]], writes=[r_acc[ti][cb]])
        for ti in range(ntl):
            t = t0 + ti
            xt, xr = xring.next()
            S.op("sp", lambda e, xt=xt, t=t: e.dma_start(out=xt[:], in_=xsrc(K, 1, b, t)), writes=[xr], dma=True)
            yt, yr = yring.next()
            S.op("pool", lambda e, yt=yt, ti=ti: e.tensor_tensor(out=yt[:], in0=acc[:, ti, :], in1=gf[:], op=ALU.mult),
                 reads=[r_acc[ti][0], r_acc[ti][1], r_gf], writes=[yr])
            S.op("dve", lambda e, yt=yt, xt=xt: e.tensor_tensor(out=yt[:], in0=yt[:], in1=xt[:], op=ALU.add),
                 reads=[yr, xr], writes=[yr])
            if last:
                dst = K.y[b, t * 128:(t + 1) * 128, :]
            else:
                dst = K.xcur[b, t * 128:(t + 1) * 128, :]
            S.op("sp", lambda e, yt=yt, dst=dst: e.dma_start(out=dst, in_=yt[:]), reads=[yr], writes=[Res()], dma=True)

    for blk in blocks:
        ffn_block(*blk)


def phase_dbgcopy(K):
    S = K.S
    ring = Ring(K, "dc", [128, D], F32, 2)
    for b in range(NB):
        for t in range(NT):
            tt, tr = ring.next()
            S.op("sp", lambda e, tt=tt, b=b, t=t: e.dma_start(out=tt[:], in_=K.xcur[b, t * 128:(t + 1) * 128, :]),
                 writes=[tr], dma=True)
            S.op("sp", lambda e, tt=tt, b=b, t=t: e.dma_start(out=K.dbg["xcur"][b, t * 128:(t + 1) * 128, :], in_=tt[:]),
                 reads=[tr], writes=[Res()], dma=True)


def phase_initx(K):
    S = K.S
    ring = Ring(K, "ix", [128, D], F32, 2)
    for b in range(NB):
        for t in range(NT):
            tt, tr = ring.next()
            S.op("sp", lambda e, tt=tt, b=b, t=t: e.dma_start(out=tt[:], in_=xsrc(K, 0, b, t)), writes=[tr], dma=True)
            S.op("sp", lambda e, tt=tt, b=b, t=t: e.dma_start(out=K.xcur[b, t * 128:(t + 1) * 128, :], in_=tt[:]),
                 reads=[tr], writes=[Res()], dma=True)


def phase_attn(K, l):
    nc, S = K.nc, K.S
    last = (l == DEPTH - 1)
    op = S.op
    ZW = 2308

    identb = K.sb("identb", [128, 128], BF16)
    r_id = Res()
    op("pool", lambda e: e.dma_start(out=identb[:], in_=K.ident), writes=[r_id], dma=True)
    Wt = K.sb("Wt", [128, 8 * INW], BF16)
    w_in = Wt[:, :].rearrange("p (k c) -> p k c", k=8)
    woA = Wt[0:64, 0:8 * D].rearrange("p (h c) -> p h c", h=8)
    woB = Wt[0:64, 8 * D:12 * D].rearrange("p (h c) -> p h c", h=4)
    woC = Wt[:, 12 * D:14 * D].rearrange("p (h c) -> p h c", h=2)
    r_win = [Res() for _ in range(8)]
    gcol = K.sb("gcol", [128, 16], F32)
    r_gc = Res()
    op("sp", lambda e: e.dma_start(out=gcol[0:64, 0:8], in_=K.g_out[l, 0:512].rearrange("(h d) -> d h", d=64),
                                   allow_slow_non_contiguous=True), writes=[r_gc], dma=True)
    op("sp", lambda e: e.dma_start(out=gcol[0:64, 8:12], in_=K.g_out[l, 512:768].rearrange("(h d) -> d h", d=64),
                                   allow_slow_non_contiguous=True), writes=[r_gc], dma=True)
    op("sp", lambda e: e.dma_start(out=gcol[:, 12:14], in_=K.g_out[l, 768:1024].rearrange("(k p) -> p k", p=128),
                                   allow_slow_non_contiguous=True), writes=[r_gc], dma=True)
    stg = Ring(K, "wstg", [128, D], F32, 2)

    def load_win():
        for k in range(8):
            op("pool", lambda e, k=k: e.dma_start(out=w_in[:, k, :], in_=K.w_in[l, k * 128:(k + 1) * 128, :]),
               writes=[r_win[k]], dma=True)

    def load_wout():
        pieces = [(woA, h, 64, h * 64, h) for h in range(8)] + [(woB, h, 64, 512 + h * 64, 8 + h) for h in range(4)] + \
                 [(woC, k, 128, 768 + k * 128, 12 + k) for k in range(2)]
        for (dst, idx, np_, row0, gc) in pieces:
            st_, sr_ = stg.next()
            op("sp", lambda e, st_=st_, np_=np_, row0=row0: e.dma_start(out=st_[0:np_, :], in_=K.w_out[l, row0:row0 + np_, :]),
               writes=[sr_], dma=True)
            op("dve", lambda e, st_=st_, dst=dst, idx=idx, np_=np_, gc=gc: e.tensor_scalar(
                out=dst[0:np_, idx, :], in0=st_[0:np_, :], scalar1=gcol[0:np_, gc:gc + 1], scalar2=None, op0=ALU.mult),
                reads=[sr_, r_gc], writes=r_win)

    gn = K.sb("gn", [128, 4, 64], F32)
    gsw = K.sb("gsw", [128, 2, 64], F32)
    r_gn = Res()
    for i in range(4):
        op("sp", lambda e, i=i: e.dma_start(out=gn[:, i, :], in_=K.gains[l, i].partition_broadcast(128)), writes=[r_gn], dma=True)
    for i in range(2):
        for (d0, s0) in ((0, 16), (16, 0), (32, 48), (48, 32)):
            op("sp", lambda e, i=i, d0=d0, s0=s0: e.dma_start(out=gsw[:, i, d0:d0 + 16],
                                                              in_=K.gains[l, i, s0:s0 + 16].partition_broadcast(128)),
               writes=[r_gn], dma=True)
    tbl = K.sb("tbl", [128, 16, 4, 64], BF16)
    r_tbl = Res()
    for dr in range(15):
        for s_ in range(2):
            op("pool", lambda e, dr=dr, s_=s_: e.dma_start(out=tbl[s_ * 64:(s_ + 1) * 64, dr, :, :], in_=K.rpbT[l, dr]),
               writes=[r_tbl], dma=True)
    op("pool", lambda e: e.memset(tbl[:, 15, :, :], NEG), writes=[r_tbl])
    cwc = K.sb("cwc", [128, 2, 4], F32)
    r_cw = Res()
    for w in range(3):
        op("sp", lambda e, w=w: e.dma_start(out=cwc[:, :, w], in_=K.conv_w[l, w].rearrange("(k p) -> p k", p=128),
                                            allow_slow_non_contiguous=True), writes=[r_cw], dma=True)
    op("sp", lambda e: e.dma_start(out=cwc[:, :, 3], in_=K.conv_b[l].rearrange("(k p) -> p k", p=128),
                                   allow_slow_non_contiguous=True), writes=[r_cw], dma=True)
    ones_b = K.sb("ones_b", [128, 2], BF16)
    ones_f = K.sb("ones_f", [128, 64], F32)
    zt = K.sb("zt", [128, 2, 4], F32)
    r_one = Res()
    op("pool", lambda e: e.memset(ones_b[:], 1.0), writes=[r_one])
    op("pool", lambda e: e.memset(ones_f[:], 1.0), writes=[r_one])
    op("pool", lambda e: e.memset(zt[:], 0.0), writes=[r_one])
    r_cz = Res()
    for a in range(2):
        for (c0, n) in ((0, 1), (2049, 2), (2307, 1)):
            op("sp", lambda e, a=a, c0=c0, n=n: e.dma_start(
                out=K.czT[a, :, c0:c0 + n].rearrange("(k p) c -> p k c", p=128), in_=zt[:, :, 0:n], allow_slow_non_contiguous=True),
                reads=[r_one], writes=[r_cz], dma=True)
    mods = {}
    for v in range(NB + 1):
        sh, r_sh = load_modcols(K, l, v, 0, "sha%d" % v, False)
        sc, r_sc = load_modcols(K, l, v, 1, "sca%d" % v, True)
        mods[v] = (sh, r_sh, sc, r_sc)

    qTa = K.sb("qTa", [128, 4, NKEY], BF16)
    kTa = K.sb("kTa", [128, NKEY], BF16)
    Va = K.sb("Va", [128, NT, 2, 65], BF16)
    qTb = K.sb("qTb", [128, 2, NKEY], BF16)
    kTb = K.sb("kTb", [128, 2, NKEY], BF16)
    Vb = K.sb("Vb", [128, NT, 4, 65], BF16)
    r_q = [Res() for _ in range(NT)]
    op("pool", lambda e: e.memset(Va[:, :, :, 64:65], 1.0), writes=r_q)
    op("pool", lambda e: e.memset(Vb[:, :, :, 64:65], 1.0), writes=r_q)

    stp = [K.ps("stp%d" % i, [128, 2, 512], F32) for i in range(2)]
    bkm = [K.ps("bk%d" % i, [128, 512], F32) for i in range(2, 6)]
    bk = [stp[0][:, 0, :], stp[0][:, 1, :]] + [t_[:, :] for t_ in bkm] + [stp[1][:, 0, :], stp[1][:, 1, :]]
    r_bk = [Res() for _ in range(8)]
    tpA = bk[6][:, :].bitcast(BF16).rearrange("p (k c) -> p k c", k=8)
    tpQ = bk[7][:, :].bitcast(BF16).rearrange("p (k c) -> p k c", k=8)
    r_tpA, r_tpQ = r_bk[6], r_bk[7]
    ST = [(bk[0], r_bk[0]), (bk[1], r_bk[1])]
    ST4 = [(bk[0], r_bk[0]), (bk[1], r_bk[1]), (bk[6], r_bk[6]), (bk[7], r_bk[7])]
    OB = [(bk[2], r_bk[2]), (bk[3], r_bk[3])]
    OP_, r_OP = bk[4], r_bk[4]
    OPR = [(bk[4], r_bk[4]), (bk[6], r_bk[6]), (bk[7], r_bk[7])]
    BC, r_BC = bk[5], r_bk[5]
    r_SS = Res()

    ST = None
    mctr = [0]

    def zcol(t):
        return 1 + t * 128 if t < LT else 2051 + (t - LT) * 128

    def pre_stage(b):
        xring = Ring(K, "xa", [128, D], F32, 3)
        sring = Ring(K, "sa", [128, 4], F32, 4)
        nring = Ring(K, "na", [128, D], BF16, 2)
        hTs = [K.sb("hTa%d" % i, [128, 8, 512], BF16) for i in range(2)]
        r_hTs = [[Res() for _ in range(4)] for _ in range(2)]
        rpring = Ring(K, "rp", [128, 128], F32, 2)
        tabring = Ring(K, "tb", [128, 4, 64], F32, 3)
        q32ring = Ring(K, "q32", [128, 1152], F32, 3)
        q32res = [[Res() for _ in range(4)] for _ in range(3)]
        sqring = Ring(K, "sq32", [128, 1152], F32, 2)
        t1ring = Ring(K, "t1", [128, 1152], F32, 2)
        t2ring = Ring(K, "t2", [128, 640], F32, 2)
        smring = Ring(K, "sma", [128, 96], F32, 3)
        qbring = Ring(K, "qb", [128, 1152], BF16, 2)
        czring = Ring(K, "cz", [128, 512], F32, 6)
        tpQ2 = bk[5][:, :].bitcast(BF16).rearrange("p (k c) -> p k c", k=8)
        r_tpQ2 = r_bk[5]
        CG, r_CG = bk[4], r_bk[4]
        state = {}

        def stage_a1(t):
            xt, xr = xring.next()
            op("sp", lambda e: e.dma_start(out=xt[:], in_=xsrc(K, l, b, t)), writes=[xr], dma=True)
            xn, nr = nring.next()
            stt, sr = sring.next()
            op("act", lambda e: e.activation(out=xn[:], in_=xt[:], func=AF.Square, scale=float(D ** -0.5), accum_out=stt[:, 0:1]),
               reads=[xr], writes=[nr, sr])
            op("act", lambda e: e.activation(out=stt[:, 1:2], in_=stt[:, 0:1], func=AF.Sqrt, bias=EPS, scale=1.0), reads=[sr], writes=[sr])
            op("dve", lambda e: e.reciprocal(out=stt[:, 2:3], in_=stt[:, 1:2]), reads=[sr], writes=[sr])
            op("dve", lambda e: e.tensor_scalar(out=xn[:], in0=xt[:], scalar1=stt[:, 2:3], scalar2=None, op0=ALU.mult),
               reads=[xr, sr], writes=[nr])
            state[t] = dict(xn=xn, nr=nr)

        def stage_a2(t):
            grp, ti = (t // 4) % 2, t % 4
            hT, r_hT = hTs[grp], r_hTs[grp]
            v = b if t < LT else NB
            sh, r_sh, sc, r_sc = mods[v]
            xn, nr = state[t]["xn"], state[t]["nr"]
            for k in range(8):
                op("pe", lambda e, k=k: e.transpose(out=tpA[:, k, :], in_=xn[:, k * 128:(k + 1) * 128], identity=identb[:]),
                   reads=[nr, r_id], writes=[r_tpA])
            for k in range(8):
                if k % 2 == 0:
                    op("act", lambda e, k=k: e.activation(out=hT[:, k, ti * 128:(ti + 1) * 128], in_=tpA[:, k, :], func=AF.Identity,
                                                          bias=sh[:, k:k + 1], scale=sc[:, k:k + 1]),
                       reads=[r_tpA, r_sh, r_sc], writes=[r_hT[ti]])
                else:
                    op("dve", lambda e, k=k: e.tensor_scalar(out=hT[:, k, ti * 128:(ti + 1) * 128], in0=tpA[:, k, :],
                                                             scalar1=sc[:, k:k + 1], scalar2=sh[:, k:k + 1], op0=ALU.mult, op1=ALU.add),
                       reads=[r_tpA, r_sh, r_sc], writes=[r_hT[ti]])
            rp, rpr = rpring.next()
            op("sp", lambda e: e.dma_start(out=rp[:], in_=K.rope[t * 128:(t + 1) * 128, :]), writes=[rpr], dma=True)
            tab, tabr = tabring.next()
            for i in range(2):
                op("pool", lambda e, i=i: e.tensor_tensor(out=tab[:, 2 * i, :], in0=rp[:, 0:64], in1=gn[:, i, :], op=ALU.mult),
                   reads=[rpr, r_gn], writes=[tabr])
                op("pool", lambda e, i=i: e.tensor_tensor(out=tab[:, 2 * i + 1, :], in0=rp[:, 64:128], in1=gsw[:, i, :], op=ALU.mult),
                   reads=[rpr, r_gn], writes=[tabr])
            state[t].update(tab=tab, tabr=tabr, hT=hT, r_hT=r_hT, ti=ti)

        def stage_b(t):
            st_ = state[t]
            hT, r_hT, ti = st_["hT"], st_["r_hT"], st_["ti"]
            need_q = not (last and t >= LT)
            qi = q32ring.i % 3
            q32, _ = q32ring.next()
            qr = q32res[qi]

            def proj(c0, n, pb, prr):
                for k in range(8):
                    op("pe", lambda e, k=k: e.matmul(pb[:, 0:n], lhsT=hT[:, k, ti * 128:(ti + 1) * 128], rhs=w_in[:, k, c0:c0 + n],
                                                     start=(k == 0), stop=(k == 7)),
                       reads=[r_hT[ti], r_win[k]], writes=[prr])
            if need_q:
                proj(0, 512, bk[0], r_bk[0])
                proj(512, 256, bk[1], r_bk[1])
            proj(1536, 512, bk[2], r_bk[2])
            proj(2048, 256, bk[3], r_bk[3])
            if "nocp" in SKIP:
                st_.update(q32=q32, qr=qr, need_q=need_q)
                return
            if need_q:
                op("act", lambda e: e.activation(out=q32[:, 0:512], in_=bk[0][:, 0:512], func=AF.Copy), reads=[r_bk[0]], writes=[qr[0]])
                op("dve", lambda e: e.tensor_copy(out=q32[:, 512:768], in_=bk[1][:, 0:256]), reads=[r_bk[1]], writes=[qr[1]])
            op("dve", lambda e: e.tensor_copy(out=q32[:, 768:896], in_=bk[2][:, 0:128]), reads=[r_bk[2]], writes=[qr[2]])
            op("dve", lambda e: e.tensor_copy(out=q32[:, 896:1152], in_=bk[2][:, 256:512]), reads=[r_bk[2]], writes=[qr[3]])
            if "nov" not in SKIP:
                op("dve", lambda e: e.tensor_copy(out=Va[:, t, :, 0:64], in_=bk[2][:, 128:256].rearrange("p (g d) -> p g d", d=64)),
                   reads=[r_bk[2]], writes=[r_q[t]])
                op("dve" if "vdve" in SKIP else "act", (lambda e: e.tensor_copy(out=Vb[:, t, :, 0:64], in_=bk[3][:, 0:256].rearrange("p (g d) -> p g d", d=64))) if "vdve" in SKIP else
                   (lambda e: e.activation(out=Vb[:, t, :, 0:64], in_=bk[3][:, 0:256].rearrange("p (g d) -> p g d", d=64), func=AF.Copy)),
                   reads=[r_bk[3]], writes=[r_q[t]])
            st_.update(q32=q32, qr=qr, need_q=need_q)

        def stage_c(t):
            st_ = state[t]
            q32, qr, need_q, tab, tabr = st_["q32"], st_["qr"], st_["need_q"], st_["tab"], st_["tabr"]
            lo = 0 if need_q else 768
            hl = lo // 64
            qrs = qr if need_q else qr[2:4]
            sq, sqr = sqring.next()
            sm, smr = smring.next()
            op("act", lambda e: e.activation(out=sq[:, lo:1152], in_=q32[:, lo:1152], func=AF.Square), reads=qrs, writes=[sqr])
            op("dve", lambda e: e.tensor_reduce(out=sm[:, hl:18], in_=sq[:, lo:1152].rearrange("p (h d) -> p h d", d=64),
                                                axis=AX.X, op=ALU.add), reads=[sqr], writes=[smr])
            op("act", lambda e: e.activation(out=sm[:, 32 + hl:50], in_=sm[:, hl:18], func=AF.Sqrt, bias=EPS, scale=1.0 / 64),
               reads=[smr], writes=[smr])
            op("dve", lambda e: e.reciprocal(out=sm[:, 64 + hl:82], in_=sm[:, 32 + hl:50]), reads=[smr], writes=[smr])
            t1, t1r = t1ring.next()
            t2, t2r = t2ring.next()

            def v3(ap, c0, nh):
                return ap[:, c0:c0 + nh * 64].rearrange("p (h d) -> p h d", d=64)

            def bc(ap2, nh):
                return ap2.unsqueeze(1).to_broadcast([128, nh, 64])
            segs = []
            if need_q:
                segs += [(0, 8, tab[:, 0, :], [tabr], qr[0]), (512, 4, gn[:, 2, :], [r_gn], qr[1])]
            segs += [(768, 2, tab[:, 2, :], [tabr], qr[2]), (896, 4, gn[:, 3, :], [r_gn], qr[3])]
            for (c0, nh, tb_, tres, qres_) in segs:
                op("pool", lambda e, c0=c0, nh=nh, tb_=tb_: e.tensor_tensor(out=v3(t1, c0, nh), in0=v3(q32, c0, nh), in1=bc(tb_, nh), op=ALU.mult),
                   reads=[qres_] + tres, writes=[t1r])
            ropes = ([(0, 0, 8, 1, qr[0])] if need_q else []) + [(768, 512, 2, 3, qr[2])]
            for (c0, d0_, nh, ci, qres_) in ropes:
                s5 = q32[:, c0:c0 + nh * 64].rearrange("p (h a s d) -> p h a s d", a=2, s=2, d=16)
                t25 = t2[:, d0_:d0_ + nh * 64].rearrange("p (h a s d) -> p h a s d", a=2, s=2, d=16)
                sn5 = tab[:, ci, :].rearrange("p (a s d) -> p a s d", a=2, s=2, d=16)
                for s_ in range(2):
                    for a_ in range(2):
                        snb = sn5[:, a_, s_, :].unsqueeze(1).to_broadcast([128, nh, 16])
                        op("dve", lambda e, s_=s_, a_=a_, snb=snb, s5=s5, t25=t25: e.tensor_tensor(
                            out=t25[:, :, a_, s_, :], in0=s5[:, :, a_, 1 - s_, :], in1=snb, op=ALU.mult),
                            reads=[qres_, tabr], writes=[t2r])
                op("pool", lambda e, c0=c0, d0_=d0_, nh=nh: e.tensor_tensor(out=t1[:, c0:c0 + nh * 64], in0=t1[:, c0:c0 + nh * 64],
                                                                           in1=t2[:, d0_:d0_ + nh * 64], op=ALU.add),
                   reads=[t1r, t2r], writes=[t1r])
            qb_, qbr = qbring.next()
            if need_q:
                dv = qb_[:, 0:512].rearrange("p (j g d) -> p g j d", j=4, g=2)
                iv = t1[:, 0:512].rearrange("p (g j d) -> p g j d", j=4, g=2)
                rv = sm[:, 64:72].rearrange("p (g j) -> p g j", g=2).unsqueeze(3).to_broadcast([128, 2, 4, 64])
                op("dve", lambda e: e.tensor_tensor(out=dv, in0=iv, in1=rv, op=ALU.mult), reads=[t1r, smr], writes=[qbr])
                lo2, hl2 = 512, 8
            else:
                lo2, hl2 = 768, 12
            nh2 = 18 - hl2
            op("dve", lambda e: e.tensor_tensor(out=v3(qb_, lo2, nh2), in0=v3(t1, lo2, nh2),
                                                in1=sm[:, 64 + hl2:82].unsqueeze(2).to_broadcast([128, nh2, 64]), op=ALU.mult),
               reads=[t1r, smr], writes=[qbr])
            st_.update(qb=qb_, qbr=qbr)

        def stage_d(t):
            st_ = state.pop(t)
            qb_, qbr, need_q = st_["qb"], st_["qbr"], st_["need_q"]
            tsl = slice(t * 128, (t + 1) * 128)
            blocks = ([(j, j * 128) for j in range(4)] + [(4, 512), (5, 640)] if need_q else []) + [(6, 768), (7, 896)]
            for (slot, c0) in blocks:
                op("pe", lambda e, slot=slot, c0=c0: e.transpose(out=tpQ[:, slot, :], in_=qb_[:, c0:c0 + 128], identity=identb[:]),
                   reads=[qbr, r_id], writes=[r_tpQ])
            op("pe", lambda e: e.transpose(out=tpQ2[:, 0, :], in_=qb_[:, 1024:1152], identity=identb[:]), reads=[qbr, r_id], writes=[r_tpQ2])
            if need_q:
                op("dve", lambda e: e.tensor_copy(out=qTa[:, :, tsl], in_=tpQ[:, 0:4, :]), reads=[r_tpQ], writes=[r_q[t]])
                op("act", lambda e: e.activation(out=qTb[:, :, tsl], in_=tpQ[:, 4:6, :], func=AF.Copy), reads=[r_tpQ], writes=[r_q[t]])
            op("dve", lambda e: e.tensor_copy(out=kTa[:, tsl], in_=tpQ[:, 6, :]), reads=[r_tpQ], writes=[r_q[t]])
            op("act", lambda e: e.activation(out=kTb[:, 0, tsl], in_=tpQ[:, 7, :], func=AF.Copy), reads=[r_tpQ], writes=[r_q[t]])
            op("dve", lambda e: e.tensor_copy(out=kTb[:, 1, tsl], in_=tpQ2[:, 0, :]), reads=[r_tpQ2], writes=[r_q[t]])

        def cgroup(t0, ntl):
            grp = (t0 // 4) % 2
            hT, r_hT = hTs[grp], r_hTs[grp]
            ntok = ntl * 128
            z0 = zcol(t0)

            def mm(chunk):
                c0 = 768 + chunk * 128
                for k in range(8):
                    op("pe", lambda e, k=k: e.matmul(CG[:, 0:ntok], lhsT=w_in[:, k, c0:c0 + 128], rhs=hT[:, k, 0:ntok],
                                                     start=(k == 0), stop=(k == 7)),
                       reads=[r_win[k]] + r_hT[0:ntl], writes=[r_CG])

            def one(j):
                mm(2 + j)
                c1, c1r = czring.next()
                op("act", lambda e: e.activation(out=c1[:, 0:ntok], in_=CG[:, 0:ntok], func=AF.Copy), reads=[r_CG], writes=[c1r])
                mm(4 + j)
                c2, c2r = czring.next()
                op("dve", lambda e: e.tensor_tensor(out=c2[:, 0:ntok], in0=CG[:, 0:ntok], in1=c1[:, 0:ntok], op=ALU.mult),
                   reads=[r_CG, c1r], writes=[c2r])
                op("sp", lambda e: e.dma_start(out=K.czT[0, j * 128:(j + 1) * 128, z0:z0 + ntok], in_=c2[:, 0:ntok]),
                   reads=[c2r], writes=[r_cz], dma=True)
                mm(j)
                c3, c3r = czring.next()
                op("act", lambda e: e.activation(out=c3[:, 0:ntok], in_=CG[:, 0:ntok], func=AF.Copy), reads=[r_CG], writes=[c3r])
                op("sp", lambda e: e.dma_start(out=K.czT[1, j * 128:(j + 1) * 128, z0:z0 + ntok], in_=c3[:, 0:ntok]),
                   reads=[c3r], writes=[r_cz], dma=True)
            for j in range(2):
                one(j)

        stage_a1(0)
        stage_a2(0)
        for t in range(NT):
            if t + 1 < NT:
                stage_a1(t + 1)
            if t >= 1:
                stage_c(t - 1)
            if t + 1 < NT:
                stage_a2(t + 1)
            stage_b(t)
            if t >= 1:
                stage_d(t - 1)
            if t % 4 == 3 or t == NT - 1:
                cgroup(t - t % 4, t % 4 + 1)
        stage_c(NT - 1)
        stage_d(NT - 1)

    pt2ring = ptring = sbring = ptbring = rdring = bcring = ocring = sqbring = sqcring = OaT = ObT = ocT = r_Oa = r_Ob = r_Oc = zring = cpring = yring = accring = rsring = xring = garing = None

    def alloc_attn_bufs():
        nonlocal pt2ring, ptring, sbring, ptbring, rdring, bcring, ocring, sqbring, sqcring, OaT, ObT, ocT, r_Oa, r_Ob, r_Oc, zring, cpring, yring, accring, rsring, xring, garing
        ptring = Ring(K, "pt", [128, 512], BF16, 1)
        pt2ring = Ring(K, "pt2", [128, 2, 512], BF16, 4)
        sbring = Ring(K, "sbb", [128, 256], F32, 2)
        ptbring = Ring(K, "ptb", [128, 256], BF16, 16)
        rdring = Ring(K, "rd", [128, 512], F32, 3)
        bcring = Ring(K, "bcs", [64, 512], F32, 2)
        ocring = Ring(K, "ocp", [128, 512], F32, 3)
        sqbring = Ring(K, "sqb", [128, 512], BF16, 3)
        sqcring = Ring(K, "sqc", [128, 512], BF16, 2)
        for i_ in range(3):
            op("pool", lambda e, i_=i_: e.memset(sqbring.t[i_][:], 0.0), writes=[sqbring.r[i_]])
        OaT = K.sb("OaT", [64, 8, 512], BF16)
        ObT = K.sb("ObT", [64, 4, 512], BF16)
        ocT = K.sb("ocT", [128, 2, 512], BF16)
        r_Oa = [Res() for _ in range(8)]
        r_Ob = [Res() for _ in range(4)]
        r_Oc = [Res() for _ in range(2)]
        zring = Ring(K, "zz", [128, 516], F32, 2)
        cpring = Ring(K, "cp", [128, 512], F32, 2)
        yring = Ring(K, "ya", [128, 512], F32, 2)
        accring = Ring(K, "aca", [128, D], F32, 2)
        rsring = Ring(K, "rsa", [128, 16], F32, 2)
        xring = Ring(K, "xb", [128, D], F32, 2)
        garing = Ring(K, "ga", [128, D], F32, 1)

    dq = []

    def defer(k, fn):
        dq.append([k, fn])

    def tick():
        for it in dq:
            it[0] -= 1
        while dq and dq[0][0] <= 0:
            dq.pop(0)[1]()

    def flush_deferred():
        while dq:
            dq.pop(0)[1]()

    def normalise(ob, obr, n, dst_fn, pe_fn=None, d1=0, d2=0):
        oc_, ocr = ocring.next()
        op("dve", lambda e: e.tensor_copy(out=oc_[0:65, 0:n], in_=ob[0:65, 0:n]), reads=[obr], writes=[ocr])
        rd, rdr = rdring.next()
        op("dve", lambda e: e.reciprocal(out=rd[64:65, 0:n], in_=oc_[64:65, 0:n]), reads=[ocr], writes=[rdr])

        def p2():
            bcs, bcr = bcring.next()
            for h0 in range(0, n, 256):
                hn = min(256, n - h0)
                op("pe", lambda e, h0=h0, hn=hn: e.matmul(BC[0:64, 0:hn], lhsT=ones_f[64:65, 0:64], rhs=rd[64:65, h0:h0 + hn],
                                                          start=True, stop=True), reads=[rdr, r_one], writes=[r_BC])
                op("dve", lambda e, h0=h0, hn=hn: e.tensor_copy(out=bcs[:, h0:h0 + hn], in_=BC[0:64, 0:hn]), reads=[r_BC], writes=[bcr])
            ctx_ = dst_fn(oc_, bcs, ocr, bcr)
            if pe_fn is not None:
                if d2 > 0:
                    defer(d2, lambda: pe_fn(ctx_))
                else:
                    pe_fn(ctx_)
        if d1 > 0:
            defer(d1, p2)
        else:
            p2()

    def ss_sq(srcT, r_src, np_, n):
        sqb, sqr = (sqbring if np_ == 64 else sqcring).next()
        op("dve", lambda e: e.tensor_tensor(out=sqb[0:np_, 0:n], in0=srcT, in1=srcT, op=ALU.mult), reads=[r_src], writes=[sqr])
        return sqb, sqr

    def ss_mm(sqb, sqr, n, col0, stride):
        for ti in range(n // 128):
            c = 256 + 2 * (col0 + ti * stride)
            op("pe", lambda e, ti=ti, c=c: e.matmul(BC[:, c:c + 2], lhsT=sqb[:, ti * 128:(ti + 1) * 128], rhs=ones_b[:, 0:2],
                                                    start=True, stop=True), reads=[sqr, r_one], writes=[r_SS])

    def ss_cols(srcT, r_src, np_, n, col0, stride):
        if "noss" in SKIP:
            return
        sqb, sqr = ss_sq(srcT, r_src, np_, n)
        ss_mm(sqb, sqr, n, col0, stride)

    def attn_block(b, q0, n, ctxq):
        ntl = n // 128
        t_first = q0 // 128
        qres = [r_q[t_first + i] for i in range(ntl)]
        kcs = list(range(LT, NT)) if ctxq else list(range(NT))
        items = [(j, kc) for j in range(4) for kc in kcs]

        def a_st(i):
            j, kc = items[i]
            sp_ = stp[i % 2]
            r0_, r1_ = (r_bk[0], r_bk[1]) if i % 2 == 0 else (r_bk[6], r_bk[7])
            for g, rr_ in ((0, r0_), (1, r1_)):
                op("pe", lambda e, g=g: e.matmul(sp_[:, g, 0:n], lhsT=kTa[64 * g:64 * g + 64, kc * 128:(kc + 1) * 128],
                                                 rhs=qTa[64 * g:64 * g + 64, j, q0:q0 + n], start=True, stop=True),
                   reads=[r_q[kc]] + qres, writes=[rr_])
            pt, ptr = pt2ring.next()
            op("act", lambda e: e.activation(out=pt[:, :, 0:n], in_=sp_[:, :, 0:n], func=AF.Exp, scale=0.125),
               reads=[r0_, r1_], writes=[ptr])
            return pt, ptr

        def a_pv(i, ptp):
            j, kc = items[i]
            pt, ptr = ptp
            for g in range(2):
                ob, obr = OB[g]
                if "nopv" not in SKIP:
                    op("pe", lambda e, g=g, ob=ob: e.matmul(ob[0:65, 0:n], lhsT=Va[:, kc, g, :], rhs=pt[:, g, 0:n],
                                                           start=(kc == kcs[0]), stop=(kc == kcs[-1])),
                       reads=[ptr, r_q[kc]], writes=[obr])
            if kc == kcs[-1] and "nonorm" not in SKIP:
                for g in range(2):
                    h = 4 * g + j
                    ob, obr = OB[g]

                    def fin(ob_, bcs, obr_, bcr, h=h):
                        op("dve", lambda e: e.tensor_tensor(out=OaT[:, h, 0:n], in0=ob_[0:64, 0:n], in1=bcs[:, 0:n], op=ALU.mult),
                           reads=[obr_, bcr], writes=[r_Oa[h]])
                        return ss_sq(OaT[:, h, 0:n], r_Oa[h], 64, n)

                    def pef(c_, h=h):
                        ss_mm(c_[0], c_[1], n, h, 8)
                    normalise(ob, obr, n, fin, pef, d1=2 + g, d2=2)

        if "noA" in SKIP:
            items = []
        pend = [a_st(i) for i in range(min(2, len(items)))]
        for i in range(len(items)):
            if i + 2 < len(items):
                pend.append(a_st(i + 2))
            a_pv(i, pend.pop(0))
            tick()
        flush_deferred()

        nrow = n // 64
        bitems = []
        for rr in range(nrow):
            if ctxq:
                chunks = [(LT, None, None), (LT + 1, None, None)]
            else:
                r = q0 // 64 + rr
                rs = min(max(r - 4, 0), 24)
                kt0 = rs // 2
                nch = 5 if rs % 2 else 4
                chunks = []
                for c in range(nch):
                    drs = []
                    for s_ in range(2):
                        kr = 2 * (kt0 + c) + s_
                        drs.append(kr - r + 7 if rs <= kr <= rs + 7 else 15)
                    chunks.append((kt0 + c, drs[0], drs[1]))
                chunks += [(LT, None, None), (LT + 1, None, None)]
            for ci, ch in enumerate(chunks):
                bitems.append((rr, ch, ci == 0, ci == len(chunks) - 1))

        def b_st(i):
            rr, (kt, d0, d1), first, lastc = bitems[i]
            qa = q0 + rr * 64
            for h in (0, 2, 1, 3):
                pr, hf = h // 2, h % 2
                stb, str_ = ST4[2 * (i % 2) + hf]
                op("pe", lambda e, h=h, pr=pr, hf=hf, stb=stb: e.matmul(stb[:, pr * 64:(pr + 1) * 64], lhsT=kTb[64 * hf:64 * hf + 64, pr, kt * 128:(kt + 1) * 128],
                                                                        rhs=qTb[64 * hf:64 * hf + 64, pr, qa:qa + 64], start=True, stop=True),
                   reads=[r_q[kt], r_q[qa // 128]], writes=[str_])
            pt, ptr = ptbring.next()
            ptv = pt[:, 0:256].rearrange("p (pr hf c) -> p hf pr c", pr=2, hf=2)
            for hf in range(2):
                stb, str_ = ST4[2 * (i % 2) + hf]
                sv = stb[:, 0:128].rearrange("p (pr c) -> p pr c", pr=2)
                if d0 is None:
                    op("act", lambda e, hf=hf, sv=sv: e.activation(out=ptv[:, hf], in_=sv, func=AF.Exp, scale=0.125), reads=[str_], writes=[ptr])
                else:
                    sbb, sbr = sbring.next()
                    bv_ = sbb[:, 0:128].rearrange("p (pr c) -> p pr c", pr=2)
                    for s_, dd in ((0, d0), (1, d1)):
                        ps_ = slice(64 * s_, 64 * s_ + 64)
                        tv = tbl[ps_, dd, :, :].rearrange("p (pr hf) c -> p hf pr c", hf=2)[:, hf]
                        op("dve", lambda e, ps_=ps_, tv=tv, sv=sv, bv_=bv_: e.scalar_tensor_tensor(
                            out=bv_[ps_], in0=sv[ps_], scalar=0.125, in1=tv, op0=ALU.mult, op1=ALU.add), reads=[str_, r_tbl], writes=[sbr])
                    op("act", lambda e, hf=hf, bv_=bv_: e.activation(out=ptv[:, hf], in_=bv_, func=AF.Exp), reads=[sbr], writes=[ptr])
            return pt, ptr

        def b_pv_row(rr, pts):
            ob, obr = OB[(rr // 2) % 2]
            cbase = (rr % 2) * 256
            nchk = len(pts)
            for h in range(4):
                for ci, (kt, pt, ptr) in enumerate(pts):
                    op("pe", lambda e, h=h, kt=kt, pt=pt, ci=ci: e.matmul(ob[0:65, cbase + h * 64:cbase + (h + 1) * 64], lhsT=Vb[:, kt, h, :],
                                                                        rhs=pt[:, h * 64:(h + 1) * 64], start=(ci == 0), stop=(ci == nchk - 1)),
                       reads=[ptr, r_q[kt]], writes=[obr])
            if rr % 2 == 1 and "nobnorm" not in SKIP:
                r0 = rr - 1

                def fin(ob_, bcs, obr_, bcr):
                    for h in range(4):
                        ov = ObT[:, h, r0 * 64:r0 * 64 + 128].rearrange("p (r c) -> p r c", r=2)
                        iv = ob_[0:64, :].rearrange("p (r h c) -> p r h c", r=2, h=4)[:, :, h, :]
                        bv = bcs[:, :].rearrange("p (r h c) -> p r h c", r=2, h=4)[:, :, h, :]
                        op("dve", lambda e, ov=ov, iv=iv, bv=bv: e.tensor_tensor(out=ov, in0=iv, in1=bv, op=ALU.mult),
                           reads=[obr_, bcr], writes=[r_Ob[h]])
                    return None
                normalise(ob, obr, 512, fin, None, d1=1)

        def b_st_row(rr):
            idxs = [i for i in range(len(bitems)) if bitems[i][0] == rr]
            out = []
            for i in idxs:
                pt, ptr = b_st(i)
                out.append((bitems[i][1][0], pt, ptr))
            return out

        if "noB" in SKIP:
            bitems = []
        nrows_b = nrow if bitems else 0
        curp = b_st_row(0) if nrows_b else None
        for rr in range(nrows_b):
            nxtp = b_st_row(rr + 1) if rr + 1 < nrows_b else None
            b_pv_row(rr, curp)
            tick()
            curp = nxtp
        flush_deferred()
        for h in range(4):
            if "noB" not in SKIP:
                ss_cols(ObT[:, h, 0:n], r_Ob[h], 64, n, 32 + h, 4)

        z0 = zcol(t_first)
        for j in range(2 if "noC" not in SKIP else 0):
            zz, zr = zring.next()
            op("sp", lambda e, j=j, zz=zz: e.dma_start(out=zz[:, 0:n + 2], in_=K.czT[0, j * 128:(j + 1) * 128, z0 - 1:z0 + n + 1]),
               reads=[r_cz], writes=[zr], dma=True)
            cp, cpr = cpring.next()
            op("sp", lambda e, j=j, cp=cp: e.dma_start(out=cp[:, 0:n], in_=K.czT[1, j * 128:(j + 1) * 128, z0:z0 + n]),
               reads=[r_cz], writes=[cpr], dma=True)
            yy, yr = yring.next()
            op("dve", lambda e, j=j, zz=zz, yy=yy: e.tensor_scalar(out=yy[:, 0:n], in0=zz[:, 0:n], scalar1=cwc[:, j, 0:1], scalar2=None, op0=ALU.mult),
               reads=[zr, r_cw], writes=[yr])
            for w in (1, 2):
                op("dve", lambda e, j=j, zz=zz, yy=yy, w=w: e.scalar_tensor_tensor(out=yy[:, 0:n], in0=zz[:, w:w + n], scalar=cwc[:, j, w:w + 1],
                                                                                   in1=yy[:, 0:n], op0=ALU.mult, op1=ALU.add),
                   reads=[zr, r_cw, yr], writes=[yr])
            op("dve", lambda e, j=j, cp=cp, yy=yy: e.scalar_tensor_tensor(out=ocT[:, j, 0:n], in0=yy[:, 0:n], scalar=cwc[:, j, 3:4], in1=cp[:, 0:n],
                                                                          op0=ALU.add, op1=ALU.mult), reads=[yr, cpr, r_cw], writes=[r_Oc[j]])
            ss_cols(ocT[:, j, 0:n], r_Oc[j], 128, n, 48 + j, 2)

        if "oT" in K.dbg:
            for h in range(8):
                op("pool", lambda e, h=h: e.dma_start(out=K.dbg["oT"][b, h * 64:(h + 1) * 64, q0:q0 + n], in_=OaT[:, h, 0:n]),
                   reads=[r_Oa[h]], writes=[Res()], dma=True)
            for h in range(4):
                op("pool", lambda e, h=h: e.dma_start(out=K.dbg["oT"][b, 512 + h * 64:512 + (h + 1) * 64, q0:q0 + n], in_=ObT[:, h, 0:n]),
                   reads=[r_Ob[h]], writes=[Res()], dma=True)
            for j in range(2):
                op("pool", lambda e, j=j: e.dma_start(out=K.dbg["oT"][b, 768 + j * 128:768 + (j + 1) * 128, q0:q0 + n], in_=ocT[:, j, 0:n]),
                   reads=[r_Oc[j]], writes=[Res()], dma=True)
        if "nomerge" in SKIP:
            return
        v = NB if ctxq else b
        gat, gar = garing.next()
        op("sp", lambda e: e.dma_start(out=gat[:], in_=K.modv[l, v, 2 * D:3 * D].partition_broadcast(128)), writes=[gar], dma=True)
        rs_, rsr = rsring.next()
        for gi_, (c0, nh, width) in enumerate(((0, 8, 512.0), (32, 4, 256.0), (48, 2, 256.0))):
            op("dve", lambda e, gi_=gi_, c0=c0, nh=nh: e.tensor_reduce(
                out=rs_[:, gi_ * 4:gi_ * 4 + ntl], in_=BC[:, 256 + 2 * c0:256 + 2 * (c0 + ntl * nh)].rearrange("p (t h two) -> p t h two", h=nh, two=2)[:, :, :, 0],
                axis=AX.X, op=ALU.add), reads=[r_SS], writes=[rsr])
            op("act", lambda e, gi_=gi_, width=width: e.activation(out=rs_[:, gi_ * 4:gi_ * 4 + ntl], in_=rs_[:, gi_ * 4:gi_ * 4 + ntl],
                                                                  func=AF.Sqrt, bias=EPS, scale=1.0 / width), reads=[rsr], writes=[rsr])
        op("dve", lambda e: e.reciprocal(out=rs_[:, 0:12], in_=rs_[:, 0:12]), reads=[rsr], writes=[rsr])
        for ti in range(ntl):
            t = t_first + ti
            tk = slice(ti * 128, (ti + 1) * 128)
            ac, acr = accring.next()
            xt, xr = xring.next()
            op("sp", lambda e, xt=xt, t=t: e.dma_start(out=xt[:], in_=xsrc(K, l, b, t)), writes=[xr], dma=True)
            for cb in range(2):
                cs = slice(cb * 512, (cb + 1) * 512)
                groups = (([(OaT[:, h, tk], woA[:, h, cs], r_Oa[h]) for h in range(8)], 0),
                          ([(ObT[:, h, tk], woB[:, h, cs], r_Ob[h]) for h in range(4)], 1),
                          ([(ocT[:, j, tk], woC[:, j, cs], r_Oc[j]) for j in range(2)], 2))
                for mm, gi_ in groups:
                    opb, opr = OPR[mctr[0] % 3]
                    mctr[0] += 1
                    for i, (lt, rh, rr_) in enumerate(mm):
                        op("pe", lambda e, lt=lt, rh=rh, i=i, nmm=len(mm), opb=opb: e.matmul(opb[:, :], lhsT=lt, rhs=rh, start=(i == 0), stop=(i == nmm - 1)),
                           reads=[rr_] + r_win, writes=[opr])
                    sc1 = rs_[:, gi_ * 4 + ti:gi_ * 4 + ti + 1]
                    if gi_ == 0:
                        op("dve", lambda e, ac=ac, cs=cs, sc1=sc1, opb=opb: e.tensor_scalar(out=ac[:, cs], in0=opb[:, :], scalar1=sc1, scalar2=None, op0=ALU.mult),
                           reads=[opr, rsr], writes=[acr])
                    else:
                        op("dve", lambda e, ac=ac, cs=cs, sc1=sc1, opb=opb: e.scalar_tensor_tensor(out=ac[:, cs], in0=opb[:, :], scalar=sc1, in1=ac[:, cs],
                                                                                          op0=ALU.mult, op1=ALU.add),
                           reads=[opr, rsr, acr], writes=[acr])
            op("pool", lambda e, ac=ac: e.tensor_tensor(out=ac[:], in0=ac[:], in1=gat[:], op=ALU.mult), reads=[acr, gar], writes=[acr])
            op("dve", lambda e, ac=ac, xt=xt: e.tensor_tensor(out=ac[:], in0=ac[:], in1=xt[:], op=ALU.add), reads=[acr, xr], writes=[acr])
            op("sp", lambda e, ac=ac, t=t: e.dma_start(out=K.xcur[b, t * 128:(t + 1) * 128, :], in_=ac[:]), reads=[acr], writes=[Res()], dma=True)

    for b in range(NB):
        load_win()
        with ExitStack() as st2:
            K.stk.append(st2)
            pre_stage(b)
            K.flush()
            K.stk.pop()
        load_wout()
        if "noattn" in SKIP:
            continue
        with ExitStack() as st2:
            K.stk.append(st2)
            alloc_attn_bufs()
            for qb in range(4):
                attn_block(b, qb * 512, 512, False)
            if not last:
                attn_block(b, SEQ, NCTX, True)
            K.flush()
            K.stk.pop()


def _rope_table():
    t = np.arange(SEQ)
    row = (t // GW).astype(np.float32)
    col = (t % GW).astype(np.float32)
    half = HD // 2
    inv = (np.float32(10000.0) ** (-np.arange(0, half, 2, dtype=np.float32) / np.float32(half))).astype(np.float32)
    ar = row[:, None] * inv
    ac = col[:, None] * inv
    cr, sr, cc, sc = np.cos(ar), np.sin(ar), np.cos(ac), np.sin(ac)
    tab = np.zeros((NKEY, 128), np.float32)
    tab[:SEQ, 0:64] = np.concatenate([cr, cr, cc, cc], 1)
    tab[:SEQ, 64:128] = np.concatenate([-sr, sr, -sc, sc], 1)
    tab[SEQ:, 0:64] = 1.0
    return tab


def _rpb_layout(rpb):
    cc = np.arange(GW)
    cs = np.clip(cc - 8, 0, GW - 16)
    dc_idx = np.clip(cc[None, :] - cc[:, None] + 15, 0, 30)
    in_win = (cc[None, :] >= cs[:, None]) & (cc[None, :] < cs[:, None] + 16)
    g = rpb[:, :, :, dc_idx]
    g = np.where(in_win[None, None, None], g, np.float32(NEG))
    return np.ascontiguousarray(g.transpose(0, 2, 4, 1, 3)).astype(np.float32)


def make_in_maps(inp, cores=range(8)):
    f = lambda a: np.ascontiguousarray(np.asarray(a, dtype=np.float32))
    shared = {
        "w_mod": f(inp["w_mod"]), "b_mod": f(inp["b_mod"]), "w_in": f(inp["w_in"]),
        "gains": f(np.stack([inp["gq_a"], inp["gk_a"], inp["gq_b"], inp["gk_b"]], 1)),
        "rpbT": _rpb_layout(np.asarray(inp["rpb"], np.float32)),
        "conv_w": f(inp["conv_w"]), "conv_b": f(inp["conv_b"]), "g_out": f(inp["g_out"]), "w_out": f(inp["w_out"]),
        "ffn_w1": f(inp["ffn_w1"]), "ffn_w3": f(inp["ffn_w3"]), "ffn_w2": f(inp["ffn_w2"]),
        "moe_router": f(inp["moe_router"]), "moe_router_b": f(inp["moe_router_b"]),
        "moe_w1": f(inp["moe_w1"]), "moe_w3": f(inp["moe_w3"]), "moe_w2": f(inp["moe_w2"]),
        "ident": np.eye(128, dtype=np.float32), "rope": _rope_table(),
    }
    maps = []
    for i in cores:
        m = dict(shared)
        m["x2"] = f(inp["x"][NB * i:NB * i + NB])
        m["ctx2"] = f(inp["ctx"][NB * i:NB * i + NB])
        m["cvec"] = f(np.concatenate([inp["c"][NB * i:NB * i + NB], np.asarray(inp["c_ctx"])[None]], 0))
        maps.append(m)
    return maps


_NC_CACHE = {}


def kernel(**inputs):
    if "nc" not in _NC_CACHE:
        _NC_CACHE["nc"] = build()
    nc = _NC_CACHE["nc"]
    maps = make_in_maps(inputs)
    res = run_bass_kernel_spmd(nc, maps, core_ids=list(range(8)))
    return np.concatenate([np.asarray(r["y"], dtype=np.float32) for r in res.results], axis=0)
```

```python
from contextlib import ExitStack

import numpy as np
import concourse.bass as bass
import concourse.mybir as mybir
from concourse.bass_utils import run_bass_kernel_spmd

F32 = mybir.dt.float32
BF16 = mybir.dt.bfloat16
AF = mybir.ActivationFunctionType
ALU = mybir.AluOpType
AX = mybir.AxisListType

D = 1024
SEQ = 2048
NCTX = 256
NB = 2
DEPTH = 2
GW = 64
HD = 64
INW = 2304
DFF = 2816
NF = DFF // 128
NE = 8
EPS = 1e-6
NEG = -30000.0
LT = SEQ // 128
CT = NCTX // 128
NT = LT + CT
NKEY = SEQ + NCTX

ENGS = ("pe", "act", "dve", "pool", "sp")
SKIP = set()
NROT = 8


class Res:
    __slots__ = ("w", "r")
    ALL = []

    def __init__(self):
        self.w = None
        self.r = []
        Res.ALL.append(self)


class Op:
    __slots__ = ("eng", "fn", "deps", "dma", "needs_inc", "tok")

    def __init__(self, eng, fn, dma):
        self.eng = eng
        self.fn = fn
        self.dma = dma
        self.deps = []
        self.needs_inc = False
        self.tok = None


class Sched:
    def __init__(self, sems, dsems):
        self.sems = sems
        self.dsems = dsems
        self.cnt = {e: 0 for e in ENGS}
        self.ndma = {e: 0 for e in ENGS}
        self.waited = {e: {} for e in ENGS}
        self.total = 0
        self.reset()

    def reset(self):
        self.ops = {e: [] for e in ENGS}
        self.dma_hist = {e: [] for e in ENGS}

    def op(self, eng, fn, reads=(), writes=(), dma=False):
        o = Op(eng, fn, dma)
        deps = {}
        for r in reads:
            if r.w is not None:
                deps[id(r.w)] = (r.w, True)
        for w in writes:
            if w.w is not None and id(w.w) not in deps:
                deps[id(w.w)] = (w.w, False)
            for rd in w.r:
                if id(rd) not in deps:
                    deps[id(rd)] = (rd, False)
        for p, raw in deps.values():
            if p is o:
                continue
            if p.eng == eng and not p.dma and not dma:
                if eng == "pe" or not raw:
                    continue
            o.deps.append(p)
            p.needs_inc = True
        if dma:
            h = self.dma_hist[eng]
            if len(h) >= NROT:
                o.deps.append(h[-NROT])
            h.append(o)
            o.needs_inc = True
        for r in reads:
            r.r.append(o)
        for w in writes:
            w.w = o
            w.r = []
        self.ops[eng].append(o)
        self.total += 1
        return o

    def finish_block(self):
        tail = []
        for e in ENGS:
            tail += self.dma_hist[e][-NROT:]
        o = Op("sp", lambda e: e.nop(), False)
        o.deps = tail
        self.ops["sp"].append(o)
        for r in Res.ALL:
            r.w = None
            r.r = []

    def emit(self, block):
        for e in ENGS:
            for o in self.ops[e]:
                if o.dma:
                    nd = self.ndma[e]
                    o.tok = (self.dsems[e][nd % NROT], 16 * (nd // NROT + 1))
                    self.ndma[e] = nd + 1
                elif o.needs_inc:
                    self.cnt[e] += 1
                    o.tok = (self.sems[e], self.cnt[e])

        def run(e, engine):
            waited = self.waited[e]
            for o in self.ops[e]:
                need = {}
                for p in o.deps:
                    s, v = p.tok
                    k = s.num
                    if waited.get(k, 0) >= v:
                        continue
                    if k not in need or need[k][1] < v:
                        need[k] = (s, v)
                for k, (s, v) in need.items():
                    engine.wait_ge(s, v)
                    waited[k] = v
                ins = o.fn(engine)
                if o.dma:
                    ins.then_inc(o.tok[0], 16)
                elif o.needs_inc:
                    ins.then_inc(o.tok[0], 1)

        block.tensor(lambda pe: run("pe", pe))
        block.scalar(lambda act: run("act", act))
        block.vector(lambda dve: run("dve", dve))
        block.gpsimd(lambda pool: run("pool", pool))
        block.sync(lambda sp: run("sp", sp))
        self.reset()


class Ring:
    def __init__(self, K, name, shape, dt, n, psum=False):
        self.t = [(K.ps if psum else K.sb)("%s%d" % (name, i), shape, dt) for i in range(n)]
        self.r = [Res() for _ in range(n)]
        self.i = 0

    def next(self):
        j = self.i % len(self.t)
        self.i += 1
        return self.t[j], self.r[j]


class Kern:
    pass


def build(phases=("mod", "attn0", "ffn0", "attn1", "ffn1"), dbg=False):
    nc = bass.Bass("TRN2", target_bir_lowering=False)
    K = Kern()
    K.nc = nc

    def din(name, shape):
        return nc.dram_tensor(name, list(shape), F32, kind="ExternalInput").ap()

    K.x2 = din("x2", [NB, SEQ, D])
    K.ctx2 = din("ctx2", [NB, NCTX, D])
    K.cvec = din("cvec", [NB + 1, D])
    K.w_mod = din("w_mod", [DEPTH, D, 6 * D])
    K.b_mod = din("b_mod", [DEPTH, 6 * D])
    K.w_in = din("w_in", [DEPTH, D, INW])
    K.gains = din("gains", [DEPTH, 4, HD])
    K.rpbT = din("rpbT", [DEPTH, 15, 64, 4, 64])
    K.conv_w = din("conv_w", [DEPTH, 3, 256])
    K.conv_b = din("conv_b", [DEPTH, 256])
    K.g_out = din("g_out", [DEPTH, D])
    K.w_out = din("w_out", [DEPTH, D, D])
    K.ffn_w1 = din("ffn_w1", [1, D, DFF])
    K.ffn_w3 = din("ffn_w3", [1, D, DFF])
    K.ffn_w2 = din("ffn_w2", [1, DFF, D])
    K.moe_router = din("moe_router", [1, D, NE])
    K.moe_router_b = din("moe_router_b", [1, NE])
    K.moe_w1 = din("moe_w1", [1, NE, D, DFF])
    K.moe_w3 = din("moe_w3", [1, NE, D, DFF])
    K.moe_w2 = din("moe_w2", [1, NE, DFF, D])
    K.ident = din("ident", [128, 128])
    K.rope = din("rope", [NKEY, 128])
    K.y = nc.dram_tensor("y", [NB, SEQ, D], F32, kind="ExternalOutput").ap()
    K.xcur = nc.dram_tensor("xcur", [NB, NKEY, D], F32, kind="Internal").ap()
    K.modv = nc.dram_tensor("modv", [DEPTH, NB + 1, 6 * D], F32, kind="Internal").ap()
    K.czT = nc.dram_tensor("czT", [2, 256, 2308], F32, kind="Internal").ap()
    K.dbg = {}
    if dbg:
        K.dbg["modv"] = nc.dram_tensor("dbg_modv", [DEPTH, NB + 1, 6 * D], F32, kind="ExternalOutput").ap()
        K.dbg["xcur"] = nc.dram_tensor("dbg_xcur", [NB, NKEY, D], F32, kind="ExternalOutput").ap()
        K.dbg["oT"] = nc.dram_tensor("dbg_oT", [NB, D, NKEY], F32, kind="ExternalOutput").ap()

    with ExitStack() as gst:
        sems = {e: gst.enter_context(nc.semaphore("s_" + e)) for e in ENGS}
        dsems = {}
        for e in ("sp", "pool", "act"):
            dsems[e] = [gst.enter_context(nc.semaphore("d_%s%d" % (e, i))) for i in range(NROT)]
        dsems["pe"] = dsems["sp"]
        dsems["dve"] = dsems["sp"]
        K.S = Sched(sems, dsems)
        K.outs = []

        for ph in phases:
            with ExitStack() as st:
                K.st = st
                K.stk = [st]
                K.uid = getattr(K, "uid", 0)

                def _sb(name, shape, dt, ph=ph):
                    K.uid += 1
                    return K.stk[-1].enter_context(nc.sbuf_tensor("%s_%s_%d" % (ph, name, K.uid), list(shape), dt))

                def _ps(name, shape, dt, ph=ph):
                    K.uid += 1
                    return K.stk[-1].enter_context(nc.psum_tensor("%s_%s_%d" % (ph, name, K.uid), list(shape), dt))

                def _flush():
                    if sum(len(v) for v in K.S.ops.values()) == 0:
                        return
                    K.S.finish_block()
                    with nc.Block() as block:
                        K.S.emit(block)
                K.sb, K.ps, K.flush = _sb, _ps, _flush
                if ph == "mod":
                    phase_mod(K)
                elif ph.startswith("attn"):
                    phase_attn(K, int(ph[4:]))
                elif ph.startswith("ffn"):
                    phase_ffn(K, int(ph[3:]))
                elif ph == "dbgcopy":
                    phase_dbgcopy(K)
                elif ph == "initx":
                    phase_initx(K)
                K.flush()
    return nc


def xsrc(K, l, b, t):
    if l == 0:
        if t < LT:
            return K.x2[b, t * 128:(t + 1) * 128, :]
        return K.ctx2[b, (t - LT) * 128:(t - LT + 1) * 128, :]
    return K.xcur[b, t * 128:(t + 1) * 128, :]


def phase_mod(K):
    nc, S = K.nc, K.S
    NV = NB + 1
    cT = K.sb("cT", [128, 8, NV], F32)
    r_cT = Res()
    scT = K.sb("scT", [128, 8, NV], BF16)
    r_scT = Res()
    for v in range(NV):
        S.op("sp", lambda e, v=v: e.dma_start(out=cT[:, :, v], in_=K.cvec[v].rearrange("(k p) -> p k", p=128),
                                              allow_slow_non_contiguous=True), writes=[r_cT], dma=True)
    S.op("act", lambda e: e.activation(out=scT[:], in_=cT[:], func=AF.Silu), reads=[r_cT], writes=[r_scT])
    wring = Ring(K, "wm", [128, 8, 512], BF16, 3)
    bring = Ring(K, "bm", [NV, 512], F32, 2)
    oring = Ring(K, "om", [NV, 512], F32, 2)
    pring = Ring(K, "pm", [NV, 512], F32, 2, psum=True)
    for l in range(DEPTH):
        for cb in range(12):
            wt, wr = wring.next()
            S.op("pool", lambda e, wt=wt, l=l, cb=cb: e.dma_start(
                out=wt[:], in_=K.w_mod[l, :, cb * 512:(cb + 1) * 512].rearrange("(k p) c -> p k c", p=128)),
                writes=[wr], dma=True)
            bt, br = bring.next()
            S.op("sp", lambda e, bt=bt, l=l, cb=cb: e.dma_start(
                out=bt[:], in_=K.b_mod[l, cb * 512:(cb + 1) * 512].partition_broadcast(NV)),
                writes=[br], dma=True)
            pt, pr = pring.next()
            for k in range(8):
                S.op("pe", lambda e, pt=pt, wt=wt, k=k: e.matmul(pt[:], lhsT=scT[:, k, :], rhs=wt[:, k, :],
                                                                 start=(k == 0), stop=(k == 7)),
                     reads=[r_scT, wr], writes=[pr])
            ot, orr = oring.next()
            S.op("dve", lambda e, ot=ot, pt=pt, bt=bt: e.tensor_tensor(out=ot[:], in0=pt[:], in1=bt[:], op=ALU.add),
                 reads=[pr, br], writes=[orr])
            S.op("sp", lambda e, ot=ot, l=l, cb=cb: e.dma_start(out=K.modv[l, :, cb * 512:(cb + 1) * 512], in_=ot[:]),
                 reads=[orr], writes=[Res()], dma=True)
            if "modv" in K.dbg:
                S.op("sp", lambda e, ot=ot, l=l, cb=cb: e.dma_start(
                    out=K.dbg["modv"][l, :, cb * 512:(cb + 1) * 512], in_=ot[:]),
                    reads=[orr], writes=[Res()], dma=True)


def load_modcols(K, l, v, idx, name, plus1):
    S = K.S
    t = K.sb(name, [128, 8], F32)
    r = Res()
    S.op("sp", lambda e: e.dma_start(out=t[:], in_=K.modv[l, v, idx * D:(idx + 1) * D].rearrange("(k p) -> p k", p=128),
                                     allow_slow_non_contiguous=True), writes=[r], dma=True)
    if plus1:
        S.op("dve", lambda e: e.tensor_scalar_add(out=t[:], in0=t[:], scalar1=1.0), reads=[r], writes=[r])
    return t, r


def load_bcast(K, src_row, n, name, dt=F32, eng="sp"):
    S = K.S
    t = K.sb(name, [128, n], dt)
    r = Res()
    S.op(eng, lambda e: e.dma_start(out=t[:], in_=src_row.partition_broadcast(128)), writes=[r], dma=True)
    return t, r


def phase_ffn(K, l):
    nc, S = K.nc, K.S
    moe = (l % 2 == 1)
    last = (l == DEPTH - 1)
    E = NE if moe else 1
    if moe:
        W1, W3, W2 = K.moe_w1[0], K.moe_w3[0], K.moe_w2[0]
    else:
        W1, W3, W2 = K.ffn_w1, K.ffn_w3, K.ffn_w2

    ident = K.sb("identf", [128, 128], F32)
    r_ident = Res()
    S.op("sp", lambda e: e.dma_start(out=ident[:], in_=K.ident), writes=[r_ident], dma=True)

    mods = {}
    for v in range(NB + 1):
        if v == NB and last:
            continue
        sh, r_sh = load_modcols(K, l, v, 3, "shf%d" % v, False)
        sc, r_sc = load_modcols(K, l, v, 4, "scf%d" % v, True)
        gf, r_gf = load_bcast(K, K.modv[l, v, 5 * D:6 * D], D, "gf%d" % v)
        mods[v] = (sh, r_sh, sc, r_sc, gf, r_gf)
    if moe:
        wr_t = K.sb("wrt", [128, 8, NE], F32)
        r_wr = Res()
        S.op("sp", lambda e: e.dma_start(out=wr_t[:], in_=K.moe_router[0].rearrange("(k p) e -> p k e", p=128)),
             writes=[r_wr], dma=True)
        br_t, r_br = load_bcast(K, K.moe_router_b[0], NE, "brt")

    TB = 8
    hT = K.sb("hT", [128, 8, TB * 128], BF16)
    r_hT = [Res() for _ in range(TB)]
    uT = K.sb("uT", [128, NF, TB * 128], BF16)
    r_uT = [[Res() for _ in range(2)] for _ in range(NF)]
    acc = K.sb("acc", [128, TB, D], F32)
    r_acc = [[Res(), Res()] for _ in range(TB)]
    G = K.sb("G", [128, TB, NE], F32)
    r_G = [Res() for _ in range(TB)]
    w2b = K.sb("w2b", [128, NF, D], BF16)
    r_w2 = [Res() for _ in range(NF)]
    xring = Ring(K, "xt", [128, D], F32, 2)
    nring = Ring(K, "xn", [128, D], F32, 2)
    jring = Ring(K, "jk", [128, D], BF16, 1)
    sring = Ring(K, "st", [128, 4], F32, 3)
    hfring = Ring(K, "hf", [128, 8, 128], F32, 2)
    w13ring = Ring(K, "w13", [128, 2, 8, 128], BF16, 3)
    sgring = Ring(K, "sg", [128, 512], F32, 2)
    tring = Ring(K, "tp", [128, 4, 128], F32, 1, psum=True)
    lgring = Ring(K, "lg", [128, NE], F32, 1, psum=True)
    gvring = Ring(K, "gv", [128, 2, 512], F32, 2, psum=True)
    oring = Ring(K, "op", [128, 512], F32, 2, psum=True)
    smring = Ring(K, "sm", [128, 4 * NE + 8], F32, 2)
    yring = Ring(K, "yt", [128, D], F32, 2)

    blocks = []
    for b in range(NB):
        blocks.append([(b, b, t) for t in range(0, 8)])
        blocks.append([(b, b, t) for t in range(8, 16)])
    if not last:
        blocks.append([(b, NB, t) for b in range(NB) for t in range(LT, NT)])

    def router(ti, lg, lr):
        sm, mr = smring.next()
        L0, E1, L2, E2, M = sm[:, 0:8], sm[:, 8:16], sm[:, 16:24], sm[:, 24:32], sm[:, 32:40]
        seq = [
            lambda e: e.tensor_tensor(out=L0, in0=lg[:], in1=br_t[:], op=ALU.add),
            lambda e: e.reduce_max(out=M[:, 0:1], in_=L0, axis=AX.X),
            lambda e: e.tensor_scalar(out=E1, in0=L0, scalar1=M[:, 0:1], scalar2=None, op0=ALU.is_equal),
            lambda e: e.scalar_tensor_tensor(out=L2, in0=E1, scalar=-1e30, in1=L0, op0=ALU.mult, op1=ALU.add),
            lambda e: e.reduce_max(out=M[:, 1:2], in_=L2, axis=AX.X),
            lambda e: e.tensor_scalar(out=E2, in0=L2, scalar1=M[:, 1:2], scalar2=None, op0=ALU.is_equal),
            lambda e: e.tensor_tensor(out=M[:, 2:3], in0=M[:, 1:2], in1=M[:, 0:1], op=ALU.subtract),
        ]
        for i, fn in enumerate(seq):
            S.op("dve", (lambda fn: (lambda e: fn(e)))(fn), reads=[mr, lr, r_br] if i == 0 else [mr], writes=[mr])
        S.op("act", lambda e, M=M: e.activation(out=M[:, 3:4], in_=M[:, 2:3], func=AF.Exp), reads=[mr], writes=[mr])
        seq2 = [
            lambda e: e.tensor_scalar_add(out=M[:, 4:5], in0=M[:, 3:4], scalar1=1.0),
            lambda e: e.reciprocal(out=M[:, 5:6], in_=M[:, 4:5]),
            lambda e: e.tensor_tensor(out=M[:, 6:7], in0=M[:, 3:4], in1=M[:, 5:6], op=ALU.mult),
            lambda e: e.tensor_scalar(out=E1, in0=E1, scalar1=M[:, 5:6], scalar2=None, op0=ALU.mult),
        ]
        for fn in seq2:
            S.op("dve", (lambda fn: (lambda e: fn(e)))(fn), reads=[mr], writes=[mr])
        S.op("dve", lambda e, E1=E1, E2=E2, M=M, ti=ti: e.scalar_tensor_tensor(
            out=G[:, ti, :], in0=E2, scalar=M[:, 6:7], in1=E1, op0=ALU.mult, op1=ALU.add),
            reads=[mr], writes=[r_G[ti]])


    w2_loaded = [False]

    def ffn_block(tiles):
        ntl = len(tiles)
        for ti in range(ntl):
            norm_tile(ti, *tiles[ti])
        experts(ntl)
        for ti in range(ntl):
            resid_tile(ti, *tiles[ti])

    def norm_tile(ti, b, v, t):
        sh, r_sh, sc, r_sc, gf, r_gf = mods[v]
        if True:
            xt, xr = xring.next()
            S.op("sp", lambda e, xt=xt, t=t: e.dma_start(out=xt[:], in_=xsrc(K, 1, b, t)), writes=[xr], dma=True)
            jk, jr = jring.next()
            stt, sr = sring.next()
            S.op("act", lambda e, jk=jk, xt=xt, stt=stt: e.activation(out=jk[:], in_=xt[:], func=AF.Square,
                                                                     scale=float(D ** -0.5), accum_out=stt[:, 0:1]),
                 reads=[xr], writes=[jr, sr])
            S.op("act", lambda e, stt=stt: e.activation(out=stt[:, 1:2], in_=stt[:, 0:1], func=AF.Sqrt, bias=EPS, scale=1.0),
                 reads=[sr], writes=[sr])
            S.op("dve", lambda e, stt=stt: e.reciprocal(out=stt[:, 2:3], in_=stt[:, 1:2]), reads=[sr], writes=[sr])
            xn, nr = nring.next()
            S.op("dve", lambda e, xn=xn, xt=xt, stt=stt: e.tensor_scalar(out=xn[:], in0=xt[:], scalar1=stt[:, 2:3],
                                                                       scalar2=None, op0=ALU.mult),
                 reads=[xr, sr], writes=[nr])
            hf, hr = hfring.next()
            for hh in range(2):
                tp, tr = tring.next()
                for kk in range(4):
                    k = hh * 4 + kk
                    S.op("pe", lambda e, tp=tp, xn=xn, k=k, kk=kk: e.transpose(out=tp[:, kk, :], in_=xn[:, k * 128:(k + 1) * 128],
                                                                             identity=ident[:]),
                         reads=[nr, r_ident], writes=[tr])
                for kk in range(4):
                    k = hh * 4 + kk
                    if moe:
                        S.op("dve", lambda e, hf=hf, tp=tp, k=k, kk=kk: e.tensor_scalar(
                            out=hf[:, k, :], in0=tp[:, kk, :], scalar1=sc[:, k:k + 1], scalar2=sh[:, k:k + 1],
                            op0=ALU.mult, op1=ALU.add), reads=[tr, r_sc, r_sh], writes=[hr])
                    else:
                        S.op("dve", lambda e, tp=tp, k=k, kk=kk, ti=ti: e.tensor_scalar(
                            out=hT[:, k, ti * 128:(ti + 1) * 128], in0=tp[:, kk, :], scalar1=sc[:, k:k + 1],
                            scalar2=sh[:, k:k + 1], op0=ALU.mult, op1=ALU.add),
                            reads=[tr, r_sc, r_sh], writes=[r_hT[ti]])
            if moe:
                S.op("pool", lambda e, hf=hf, ti=ti: e.tensor_copy(out=hT[:, :, ti * 128:(ti + 1) * 128], in_=hf[:]),
                     reads=[hr], writes=[r_hT[ti]])
                lg, lr = lgring.next()
                for k in range(8):
                    S.op("pe", lambda e, lg=lg, hf=hf, k=k: e.matmul(lg[:], lhsT=hf[:, k, :], rhs=wr_t[:, k, :],
                                                                     start=(k == 0), stop=(k == 7)),
                         reads=[hr, r_wr], writes=[lr])
                router(ti, lg, lr)

    def experts(ntl):
        NTOK = ntl * 128
        halves = [(h0, min(512, NTOK - h0)) for h0 in range(0, NTOK, 512)]
        for ex in range(E):
            w1e = W1[ex] if moe else W1[0]
            w3e = W3[ex] if moe else W3[0]
            w2e = W2[ex] if moe else W2[0]
            for f in range(NF):
                wt, wr = w13ring.next()
                S.op("pool", lambda e, wt=wt, w1e=w1e, f=f: e.dma_start(
                    out=wt[:, 0, :, :], in_=w1e[:, f * 128:(f + 1) * 128].rearrange("(k p) c -> p k c", p=128)),
                    writes=[wr], dma=True)
                r2 = Res()
                S.op("pool", lambda e, wt=wt, w3e=w3e, f=f: e.dma_start(
                    out=wt[:, 1, :, :], in_=w3e[:, f * 128:(f + 1) * 128].rearrange("(k p) c -> p k c", p=128)),
                    writes=[r2], dma=True)
                if moe or not w2_loaded[0]:
                    S.op("pool", lambda e, w2e=w2e, f=f: e.dma_start(out=w2b[:, f, :], in_=w2e[f * 128:(f + 1) * 128, :]),
                         writes=[r_w2[f]], dma=True)
                for hi, (h0, hn) in enumerate(halves):
                    gv, gr = gvring.next()
                    tiles_in = list(range(h0 // 128, (h0 + hn) // 128))
                    for j in range(2):
                        for k in range(8):
                            S.op("pe", lambda e, gv=gv, wt=wt, j=j, k=k, h0=h0, hn=hn: e.matmul(
                                gv[:, j, 0:hn], lhsT=wt[:, j, k, :], rhs=hT[:, k, h0:h0 + hn], start=(k == 0), stop=(k == 7)),
                                reads=[wr, r2] + [r_hT[i] for i in tiles_in], writes=[gr])
                    sg, sr2 = sgring.next()
                    S.op("act", lambda e, sg=sg, gv=gv, hn=hn: e.activation(out=sg[:, 0:hn], in_=gv[:, 0, 0:hn], func=AF.Silu),
                         reads=[gr], writes=[sr2])
                    S.op("dve", lambda e, sg=sg, gv=gv, f=f, h0=h0, hn=hn: e.tensor_tensor(
                        out=uT[:, f, h0:h0 + hn], in0=gv[:, 1, 0:hn], in1=sg[:, 0:hn], op=ALU.mult),
                        reads=[gr, sr2], writes=[r_uT[f][hi]])
            for ti in range(ntl):
                for cb in range(2):
                    ot, orr = oring.next()
                    for f in range(NF):
                        S.op("pe", lambda e, ot=ot, f=f, ti=ti, cb=cb: e.matmul(
                            ot[:], lhsT=uT[:, f, ti * 128:(ti + 1) * 128], rhs=w2b[:, f, cb * 512:(cb + 1) * 512],
                            start=(f == 0), stop=(f == NF - 1)),
                            reads=[r_uT[f][ti // 4], r_w2[f]], writes=[orr])
                    asl = acc[:, ti, cb * 512:(cb + 1) * 512]
                    if not moe:
                        S.op("dve", lambda e, asl=asl, ot=ot: e.tensor_copy(out=asl, in_=ot[:]),
                             reads=[orr], writes=[r_acc[ti][cb]])
                    elif ex == 0:
                        S.op("dve", lambda e, asl=asl, ot=ot, ti=ti, ex=ex: e.tensor_scalar(
                            out=asl, in0=ot[:], scalar1=G[:, ti, ex:ex + 1], scalar2=None, op0=ALU.mult),
                            reads=[orr, r_G[ti]], writes=[r_acc[ti][cb]])
                    else:
                        S.op("dve", lambda e, asl=asl, ot=ot, ti=ti, ex=ex: e.scalar_tensor_tensor(
                            out=asl, in0=ot[:], scalar=G[:, ti, ex:ex + 1], in1=asl, op0=ALU.mult, op1=ALU.add),
                            reads=[orr, r_G[ti], r_acc[ti][cb]], writes=[r_acc[ti][cb]])
        w2_loaded[0] = True

    def resid_tile(ti, b, v, t):
        sh, r_sh, sc, r_sc, gf, r_gf = mods[v]
        if True:
            xt, xr = xring.next()
            S.op("sp", lambda e, xt=xt, t=t: e.dma_start(out=xt[:], in_=xsrc(K, 1, b, t)), writes=[xr], dma=True)
            yt, yr = yring.next()
            S.op("pool", lambda e, yt=yt, ti=ti: e.tensor_tensor(out=yt[:], in0=acc[:, ti, :], in1=gf[:], op=ALU.mult),
                 reads=[r_acc[ti][0], r_acc[ti][1], r_gf], writes=[yr])
            S.op("dve", lambda e, yt=yt, xt=xt: e.tensor_tensor(out=yt[:], in0=yt[:], in1=xt[:], op=ALU.add),
                 reads=[yr, xr], writes=[yr])
            if last:
                dst = K.y[b, t * 128:(t + 1) * 128, :]
            else:
                dst = K.xcur[b, t * 128:(t + 1) * 128, :]
            S.op("sp", lambda e, yt=yt, dst=dst: e.dma_start(out=dst, in_=yt[:]), reads=[yr], writes=[Res()], dma=True)

    for blk in blocks:
        ffn_block(blk)


def phase_dbgcopy(K):
    S = K.S
    ring = Ring(K, "dc", [128, D], F32, 2)
    for b in range(NB):
        for t in range(NT):
            tt, tr = ring.next()
            S.op("sp", lambda e, tt=tt, b=b, t=t: e.dma_start(out=tt[:], in_=K.xcur[b, t * 128:(t + 1) * 128, :]),
                 writes=[tr], dma=True)
            S.op("sp", lambda e, tt=tt, b=b, t=t: e.dma_start(out=K.dbg["xcur"][b, t * 128:(t + 1) * 128, :], in_=tt[:]),
                 reads=[tr], writes=[Res()], dma=True)


def phase_initx(K):
    S = K.S
    ring = Ring(K, "ix", [128, D], F32, 2)
    for b in range(NB):
        for t in range(NT):
            tt, tr = ring.next()
            S.op("sp", lambda e, tt=tt, b=b, t=t: e.dma_start(out=tt[:], in_=xsrc(K, 0, b, t)), writes=[tr], dma=True)
            S.op("sp", lambda e, tt=tt, b=b, t=t: e.dma_start(out=K.xcur[b, t * 128:(t + 1) * 128, :], in_=tt[:]),
                 reads=[tr], writes=[Res()], dma=True)


def phase_attn(K, l):
    nc, S = K.nc, K.S
    last = (l == DEPTH - 1)
    op = S.op
    ZW = 2308

    identb = K.sb("identb", [128, 128], BF16)
    r_id = Res()
    op("pool", lambda e: e.dma_start(out=identb[:], in_=K.ident), writes=[r_id], dma=True)
    Wt = K.sb("Wt", [128, 8 * INW], BF16)
    w_in = Wt[:, :].rearrange("p (k c) -> p k c", k=8)
    woA = Wt[0:64, 0:8 * D].rearrange("p (h c) -> p h c", h=8)
    woB = Wt[0:64, 8 * D:12 * D].rearrange("p (h c) -> p h c", h=4)
    woC = Wt[:, 12 * D:14 * D].rearrange("p (h c) -> p h c", h=2)
    r_win = [Res() for _ in range(8)]
    gcol = K.sb("gcol", [128, 16], F32)
    r_gc = Res()
    op("sp", lambda e: e.dma_start(out=gcol[0:64, 0:8], in_=K.g_out[l, 0:512].rearrange("(h d) -> d h", d=64),
                                   allow_slow_non_contiguous=True), writes=[r_gc], dma=True)
    op("sp", lambda e: e.dma_start(out=gcol[0:64, 8:12], in_=K.g_out[l, 512:768].rearrange("(h d) -> d h", d=64),
                                   allow_slow_non_contiguous=True), writes=[r_gc], dma=True)
    op("sp", lambda e: e.dma_start(out=gcol[:, 12:14], in_=K.g_out[l, 768:1024].rearrange("(k p) -> p k", p=128),
                                   allow_slow_non_contiguous=True), writes=[r_gc], dma=True)
    stg = Ring(K, "wstg", [128, D], F32, 2)

    def load_win():
        for k in range(8):
            op("pool", lambda e, k=k: e.dma_start(out=w_in[:, k, :], in_=K.w_in[l, k * 128:(k + 1) * 128, :]),
               writes=[r_win[k]], dma=True)

    def load_wout():
        pieces = [(woA, h, 64, h * 64, h) for h in range(8)] + [(woB, h, 64, 512 + h * 64, 8 + h) for h in range(4)] + \
                 [(woC, k, 128, 768 + k * 128, 12 + k) for k in range(2)]
        for (dst, idx, np_, row0, gc) in pieces:
            st_, sr_ = stg.next()
            op("sp", lambda e, st_=st_, np_=np_, row0=row0: e.dma_start(out=st_[0:np_, :], in_=K.w_out[l, row0:row0 + np_, :]),
               writes=[sr_], dma=True)
            op("dve", lambda e, st_=st_, dst=dst, idx=idx, np_=np_, gc=gc: e.tensor_scalar(
                out=dst[0:np_, idx, :], in0=st_[0:np_, :], scalar1=gcol[0:np_, gc:gc + 1], scalar2=None, op0=ALU.mult),
                reads=[sr_, r_gc], writes=r_win)

    gn = K.sb("gn", [128, 4, 64], F32)
    gsw = K.sb("gsw", [128, 2, 64], F32)
    r_gn = Res()
    for i in range(4):
        op("sp", lambda e, i=i: e.dma_start(out=gn[:, i, :], in_=K.gains[l, i].partition_broadcast(128)), writes=[r_gn], dma=True)
    for i in range(2):
        for (d0, s0) in ((0, 16), (16, 0), (32, 48), (48, 32)):
            op("sp", lambda e, i=i, d0=d0, s0=s0: e.dma_start(out=gsw[:, i, d0:d0 + 16],
                                                              in_=K.gains[l, i, s0:s0 + 16].partition_broadcast(128)),
               writes=[r_gn], dma=True)
    tbl = K.sb("tbl", [128, 16, 4, 64], BF16)
    r_tbl = Res()
    for dr in range(15):
        for s_ in range(2):
            op("pool", lambda e, dr=dr, s_=s_: e.dma_start(out=tbl[s_ * 64:(s_ + 1) * 64, dr, :, :], in_=K.rpbT[l, dr]),
               writes=[r_tbl], dma=True)
    op("pool", lambda e: e.memset(tbl[:, 15, :, :], NEG), writes=[r_tbl])
    cwc = K.sb("cwc", [128, 2, 4], F32)
    r_cw = Res()
    for w in range(3):
        op("sp", lambda e, w=w: e.dma_start(out=cwc[:, :, w], in_=K.conv_w[l, w].rearrange("(k p) -> p k", p=128),
                                            allow_slow_non_contiguous=True), writes=[r_cw], dma=True)
    op("sp", lambda e: e.dma_start(out=cwc[:, :, 3], in_=K.conv_b[l].rearrange("(k p) -> p k", p=128),
                                   allow_slow_non_contiguous=True), writes=[r_cw], dma=True)
    ones_b = K.sb("ones_b", [128, 2], BF16)
    ones_f = K.sb("ones_f", [128, 64], F32)
    zt = K.sb("zt", [128, 2, 4], F32)
    r_one = Res()
    op("pool", lambda e: e.memset(ones_b[:], 1.0), writes=[r_one])
    op("pool", lambda e: e.memset(ones_f[:], 1.0), writes=[r_one])
    op("pool", lambda e: e.memset(zt[:], 0.0), writes=[r_one])
    r_cz = Res()
    for a in range(2):
        for (c0, n) in ((0, 1), (2049, 2), (2307, 1)):
            op("sp", lambda e, a=a, c0=c0, n=n: e.dma_start(
                out=K.czT[a, :, c0:c0 + n].rearrange("(k p) c -> p k c", p=128), in_=zt[:, :, 0:n], allow_slow_non_contiguous=True),
                reads=[r_one], writes=[r_cz], dma=True)
    mods = {}
    for v in range(NB + 1):
        sh, r_sh = load_modcols(K, l, v, 0, "sha%d" % v, False)
        sc, r_sc = load_modcols(K, l, v, 1, "sca%d" % v, True)
        mods[v] = (sh, r_sh, sc, r_sc)

    qTa = K.sb("qTa", [128, 4, NKEY], BF16)
    kTa = K.sb("kTa", [128, NKEY], BF16)
    Va = K.sb("Va", [128, NT, 2, 65], BF16)
    qTb = K.sb("qTb", [128, 2, NKEY], BF16)
    kTb = K.sb("kTb", [128, 2, NKEY], BF16)
    Vb = K.sb("Vb", [128, NT, 4, 65], BF16)
    r_q = [Res() for _ in range(NT)]
    op("pool", lambda e: e.memset(Va[:, :, :, 64:65], 1.0), writes=r_q)
    op("pool", lambda e: e.memset(Vb[:, :, :, 64:65], 1.0), writes=r_q)

    stp = [K.ps("stp%d" % i, [128, 2, 512], F32) for i in range(2)]
    bkm = [K.ps("bk%d" % i, [128, 512], F32) for i in range(2, 6)]
    bk = [stp[0][:, 0, :], stp[0][:, 1, :]] + [t_[:, :] for t_ in bkm] + [stp[1][:, 0, :], stp[1][:, 1, :]]
    r_bk = [Res() for _ in range(8)]
    tpA = bk[6][:, :].bitcast(BF16).rearrange("p (k c) -> p k c", k=8)
    tpQ = bk[7][:, :].bitcast(BF16).rearrange("p (k c) -> p k c", k=8)
    r_tpA, r_tpQ = r_bk[6], r_bk[7]
    ST = [(bk[0], r_bk[0]), (bk[1], r_bk[1])]
    ST4 = [(bk[0], r_bk[0]), (bk[1], r_bk[1]), (bk[6], r_bk[6]), (bk[7], r_bk[7])]
    OB = [(bk[2], r_bk[2]), (bk[3], r_bk[3])]
    OP_, r_OP = bk[4], r_bk[4]
    OPR = [(bk[4], r_bk[4]), (bk[6], r_bk[6]), (bk[7], r_bk[7])]
    BC, r_BC = bk[5], r_bk[5]
    r_SS = Res()

    ST = None
    mctr = [0]

    def zcol(t):
        return 1 + t * 128 if t < LT else 2051 + (t - LT) * 128

    def pre_stage(b):
        xring = Ring(K, "xa", [128, D], F32, 3)
        sring = Ring(K, "sa", [128, 4], F32, 4)
        nring = Ring(K, "na", [128, D], BF16, 2)
        hTs = [K.sb("hTa%d" % i, [128, 8, 512], BF16) for i in range(2)]
        r_hTs = [[Res() for _ in range(4)] for _ in range(2)]
        rpring = Ring(K, "rp", [128, 128], F32, 2)
        tabring = Ring(K, "tb", [128, 4, 64], F32, 3)
        q32ring = Ring(K, "q32", [128, 1152], F32, 3)
        q32res = [[Res() for _ in range(4)] for _ in range(3)]
        sqring = Ring(K, "sq32", [128, 1152], F32, 2)
        t1ring = Ring(K, "t1", [128, 1152], F32, 2)
        t2ring = Ring(K, "t2", [128, 640], F32, 2)
        smring = Ring(K, "sma", [128, 96], F32, 3)
        qbring = Ring(K, "qb", [128, 1152], BF16, 2)
        czring = Ring(K, "cz", [128, 512], F32, 6)
        tpQ2 = bk[5][:, :].bitcast(BF16).rearrange("p (k c) -> p k c", k=8)
        r_tpQ2 = r_bk[5]
        CG, r_CG = bk[4], r_bk[4]
        state = {}

        def stage_a1(t):
            xt, xr = xring.next()
            op("sp", lambda e: e.dma_start(out=xt[:], in_=xsrc(K, l, b, t)), writes=[xr], dma=True)
            xn, nr = nring.next()
            stt, sr = sring.next()
            op("act", lambda e: e.activation(out=xn[:], in_=xt[:], func=AF.Square, scale=float(D ** -0.5), accum_out=stt[:, 0:1]),
               reads=[xr], writes=[nr, sr])
            op("act", lambda e: e.activation(out=stt[:, 1:2], in_=stt[:, 0:1], func=AF.Sqrt, bias=EPS, scale=1.0), reads=[sr], writes=[sr])
            op("dve", lambda e: e.reciprocal(out=stt[:, 2:3], in_=stt[:, 1:2]), reads=[sr], writes=[sr])
            op("dve", lambda e: e.tensor_scalar(out=xn[:], in0=xt[:], scalar1=stt[:, 2:3], scalar2=None, op0=ALU.mult),
               reads=[xr, sr], writes=[nr])
            state[t] = dict(xn=xn, nr=nr)

        def stage_a2(t):
            grp, ti = (t // 4) % 2, t % 4
            hT, r_hT = hTs[grp], r_hTs[grp]
            v = b if t < LT else NB
            sh, r_sh, sc, r_sc = mods[v]
            xn, nr = state[t]["xn"], state[t]["nr"]
            for k in range(8):
                op("pe", lambda e, k=k: e.transpose(out=tpA[:, k, :], in_=xn[:, k * 128:(k + 1) * 128], identity=identb[:]),
                   reads=[nr, r_id], writes=[r_tpA])
            for k in range(8):
                if k % 2 == 0:
                    op("act", lambda e, k=k: e.activation(out=hT[:, k, ti * 128:(ti + 1) * 128], in_=tpA[:, k, :], func=AF.Identity,
                                                          bias=sh[:, k:k + 1], scale=sc[:, k:k + 1]),
                       reads=[r_tpA, r_sh, r_sc], writes=[r_hT[ti]])
                else:
                    op("dve", lambda e, k=k: e.tensor_scalar(out=hT[:, k, ti * 128:(ti + 1) * 128], in0=tpA[:, k, :],
                                                             scalar1=sc[:, k:k + 1], scalar2=sh[:, k:k + 1], op0=ALU.mult, op1=ALU.add),
                       reads=[r_tpA, r_sh, r_sc], writes=[r_hT[ti]])
            rp, rpr = rpring.next()
            op("sp", lambda e: e.dma_start(out=rp[:], in_=K.rope[t * 128:(t + 1) * 128, :]), writes=[rpr], dma=True)
            tab, tabr = tabring.next()
            for i in range(2):
                op("pool", lambda e, i=i: e.tensor_tensor(out=tab[:, 2 * i, :], in0=rp[:, 0:64], in1=gn[:, i, :], op=ALU.mult),
                   reads=[rpr, r_gn], writes=[tabr])
                op("pool", lambda e, i=i: e.tensor_tensor(out=tab[:, 2 * i + 1, :], in0=rp[:, 64:128], in1=gsw[:, i, :], op=ALU.mult),
                   reads=[rpr, r_gn], writes=[tabr])
            state[t].update(tab=tab, tabr=tabr, hT=hT, r_hT=r_hT, ti=ti)

        def stage_b(t):
            st_ = state[t]
            hT, r_hT, ti = st_["hT"], st_["r_hT"], st_["ti"]
            need_q = not (last and t >= LT)
            qi = q32ring.i % 3
            q32, _ = q32ring.next()
            qr = q32res[qi]

            def proj(c0, n, pb, prr):
                for k in range(8):
                    op("pe", lambda e, k=k: e.matmul(pb[:, 0:n], lhsT=hT[:, k, ti * 128:(ti + 1) * 128], rhs=w_in[:, k, c0:c0 + n],
                                                     start=(k == 0), stop=(k == 7)),
                       reads=[r_hT[ti], r_win[k]], writes=[prr])
            if need_q:
                proj(0, 512, bk[0], r_bk[0])
                proj(512, 256, bk[1], r_bk[1])
            proj(1536, 512, bk[2], r_bk[2])
            proj(2048, 256, bk[3], r_bk[3])
            if "nocp" in SKIP:
                st_.update(q32=q32, qr=qr, need_q=need_q)
                return
            if need_q:
                op("act", lambda e: e.activation(out=q32[:, 0:512], in_=bk[0][:, 0:512], func=AF.Copy), reads=[r_bk[0]], writes=[qr[0]])
                op("dve", lambda e: e.tensor_copy(out=q32[:, 512:768], in_=bk[1][:, 0:256]), reads=[r_bk[1]], writes=[qr[1]])
            op("dve", lambda e: e.tensor_copy(out=q32[:, 768:896], in_=bk[2][:, 0:128]), reads=[r_bk[2]], writes=[qr[2]])
            op("dve", lambda e: e.tensor_copy(out=q32[:, 896:1152], in_=bk[2][:, 256:512]), reads=[r_bk[2]], writes=[qr[3]])
            if "nov" not in SKIP:
                op("dve", lambda e: e.tensor_copy(out=Va[:, t, :, 0:64], in_=bk[2][:, 128:256].rearrange("p (g d) -> p g d", d=64)),
                   reads=[r_bk[2]], writes=[r_q[t]])
                op("dve" if "vdve" in SKIP else "act", (lambda e: e.tensor_copy(out=Vb[:, t, :, 0:64], in_=bk[3][:, 0:256].rearrange("p (g d) -> p g d", d=64))) if "vdve" in SKIP else
                   (lambda e: e.activation(out=Vb[:, t, :, 0:64], in_=bk[3][:, 0:256].rearrange("p (g d) -> p g d", d=64), func=AF.Copy)),
                   reads=[r_bk[3]], writes=[r_q[t]])
            st_.update(q32=q32, qr=qr, need_q=need_q)

        def stage_c(t):
            st_ = state[t]
            q32, qr, need_q, tab, tabr = st_["q32"], st_["qr"], st_["need_q"], st_["tab"], st_["tabr"]
            lo = 0 if need_q else 768
            hl = lo // 64
            qrs = qr if need_q else qr[2:4]
            sq, sqr = sqring.next()
            sm, smr = smring.next()
            op("act", lambda e: e.activation(out=sq[:, lo:1152], in_=q32[:, lo:1152], func=AF.Square), reads=qrs, writes=[sqr])
            op("dve", lambda e: e.tensor_reduce(out=sm[:, hl:18], in_=sq[:, lo:1152].rearrange("p (h d) -> p h d", d=64),
                                                axis=AX.X, op=ALU.add), reads=[sqr], writes=[smr])
            op("act", lambda e: e.activation(out=sm[:, 32 + hl:50], in_=sm[:, hl:18], func=AF.Sqrt, bias=EPS, scale=1.0 / 64),
               reads=[smr], writes=[smr])
            op("dve", lambda e: e.reciprocal(out=sm[:, 64 + hl:82], in_=sm[:, 32 + hl:50]), reads=[smr], writes=[smr])
            t1, t1r = t1ring.next()
            t2, t2r = t2ring.next()

            def v3(ap, c0, nh):
                return ap[:, c0:c0 + nh * 64].rearrange("p (h d) -> p h d", d=64)

            def bc(ap2, nh):
                return ap2.unsqueeze(1).to_broadcast([128, nh, 64])
            segs = []
            if need_q:
                segs += [(0, 8, tab[:, 0, :], [tabr], qr[0]), (512, 4, gn[:, 2, :], [r_gn], qr[1])]
            segs += [(768, 2, tab[:, 2, :], [tabr], qr[2]), (896, 4, gn[:, 3, :], [r_gn], qr[3])]
            for (c0, nh, tb_, tres, qres_) in segs:
                op("pool", lambda e, c0=c0, nh=nh, tb_=tb_: e.tensor_tensor(out=v3(t1, c0, nh), in0=v3(q32, c0, nh), in1=bc(tb_, nh), op=ALU.mult),
                   reads=[qres_] + tres, writes=[t1r])
            ropes = ([(0, 0, 8, 1, qr[0])] if need_q else []) + [(768, 512, 2, 3, qr[2])]
            for (c0, d0_, nh, ci, qres_) in ropes:
                s5 = q32[:, c0:c0 + nh * 64].rearrange("p (h a s d) -> p h a s d", a=2, s=2, d=16)
                t25 = t2[:, d0_:d0_ + nh * 64].rearrange("p (h a s d) -> p h a s d", a=2, s=2, d=16)
                sn5 = tab[:, ci, :].rearrange("p (a s d) -> p a s d", a=2, s=2, d=16)
                for s_ in range(2):
                    for a_ in range(2):
                        snb = sn5[:, a_, s_, :].unsqueeze(1).to_broadcast([128, nh, 16])
                        op("dve", lambda e, s_=s_, a_=a_, snb=snb, s5=s5, t25=t25: e.tensor_tensor(
                            out=t25[:, :, a_, s_, :], in0=s5[:, :, a_, 1 - s_, :], in1=snb, op=ALU.mult),
                            reads=[qres_, tabr], writes=[t2r])
                op("pool", lambda e, c0=c0, d0_=d0_, nh=nh: e.tensor_tensor(out=t1[:, c0:c0 + nh * 64], in0=t1[:, c0:c0 + nh * 64],
                                                                           in1=t2[:, d0_:d0_ + nh * 64], op=ALU.add),
                   reads=[t1r, t2r], writes=[t1r])
            qb_, qbr = qbring.next()
            if need_q:
                dv = qb_[:, 0:512].rearrange("p (j g d) -> p g j d", j=4, g=2)
                iv = t1[:, 0:512].rearrange("p (g j d) -> p g j d", j=4, g=2)
                rv = sm[:, 64:72].rearrange("p (g j) -> p g j", g=2).unsqueeze(3).to_broadcast([128, 2, 4, 64])
                op("dve", lambda e: e.tensor_tensor(out=dv, in0=iv, in1=rv, op=ALU.mult), reads=[t1r, smr], writes=[qbr])
                lo2, hl2 = 512, 8
            else:
                lo2, hl2 = 768, 12
            nh2 = 18 - hl2
            op("dve", lambda e: e.tensor_tensor(out=v3(qb_, lo2, nh2), in0=v3(t1, lo2, nh2),
                                                in1=sm[:, 64 + hl2:82].unsqueeze(2).to_broadcast([128, nh2, 64]), op=ALU.mult),
               reads=[t1r, smr], writes=[qbr])
            st_.update(qb=qb_, qbr=qbr)

        def stage_d(t):
            st_ = state.pop(t)
            qb_, qbr, need_q = st_["qb"], st_["qbr"], st_["need_q"]
            tsl = slice(t * 128, (t + 1) * 128)
            blocks = ([(j, j * 128) for j in range(4)] + [(4, 512), (5, 640)] if need_q else []) + [(6, 768), (7, 896)]
            for (slot, c0) in blocks:
                op("pe", lambda e, slot=slot, c0=c0: e.transpose(out=tpQ[:, slot, :], in_=qb_[:, c0:c0 + 128], identity=identb[:]),
                   reads=[qbr, r_id], writes=[r_tpQ])
            op("pe", lambda e: e.transpose(out=tpQ2[:, 0, :], in_=qb_[:, 1024:1152], identity=identb[:]), reads=[qbr, r_id], writes=[r_tpQ2])
            if need_q:
                op("dve", lambda e: e.tensor_copy(out=qTa[:, :, tsl], in_=tpQ[:, 0:4, :]), reads=[r_tpQ], writes=[r_q[t]])
                op("act", lambda e: e.activation(out=qTb[:, :, tsl], in_=tpQ[:, 4:6, :], func=AF.Copy), reads=[r_tpQ], writes=[r_q[t]])
            op("dve", lambda e: e.tensor_copy(out=kTa[:, tsl], in_=tpQ[:, 6, :]), reads=[r_tpQ], writes=[r_q[t]])
            op("act", lambda e: e.activation(out=kTb[:, 0, tsl], in_=tpQ[:, 7, :], func=AF.Copy), reads=[r_tpQ], writes=[r_q[t]])
            op("dve", lambda e: e.tensor_copy(out=kTb[:, 1, tsl], in_=tpQ2[:, 0, :]), reads=[r_tpQ2], writes=[r_q[t]])

        def cgroup(t0, ntl):
            grp = (t0 // 4) % 2
            hT, r_hT = hTs[grp], r_hTs[grp]
            ntok = ntl * 128
            z0 = zcol(t0)

            def mm(chunk):
                c0 = 768 + chunk * 128
                for k in range(8):
                    op("pe", lambda e, k=k: e.matmul(CG[:, 0:ntok], lhsT=w_in[:, k, c0:c0 + 128], rhs=hT[:, k, 0:ntok],
                                                     start=(k == 0), stop=(k == 7)),
                       reads=[r_win[k]] + r_hT[0:ntl], writes=[r_CG])

            def one(j):
                mm(2 + j)
                c1, c1r = czring.next()
                op("act", lambda e: e.activation(out=c1[:, 0:ntok], in_=CG[:, 0:ntok], func=AF.Copy), reads=[r_CG], writes=[c1r])
                mm(4 + j)
                c2, c2r = czring.next()
                op("dve", lambda e: e.tensor_tensor(out=c2[:, 0:ntok], in0=CG[:, 0:ntok], in1=c1[:, 0:ntok], op=ALU.mult),
                   reads=[r_CG, c1r], writes=[c2r])
                op("sp", lambda e: e.dma_start(out=K.czT[0, j * 128:(j + 1) * 128, z0:z0 + ntok], in_=c2[:, 0:ntok]),
                   reads=[c2r], writes=[r_cz], dma=True)
                mm(j)
                c3, c3r = czring.next()
                op("act", lambda e: e.activation(out=c3[:, 0:ntok], in_=CG[:, 0:ntok], func=AF.Copy), reads=[r_CG], writes=[c3r])
                op("sp", lambda e: e.dma_start(out=K.czT[1, j * 128:(j + 1) * 128, z0:z0 + ntok], in_=c3[:, 0:ntok]),
                   reads=[c3r], writes=[r_cz], dma=True)
            for j in range(2):
                one(j)

        stage_a1(0)
        stage_a2(0)
        for t in range(NT):
            if t + 1 < NT:
                stage_a1(t + 1)
            if t >= 1:
                stage_c(t - 1)
            if t + 1 < NT:
                stage_a2(t + 1)
            stage_b(t)
            if t >= 1:
                stage_d(t - 1)
            if t % 4 == 3 or t == NT - 1:
                cgroup(t - t % 4, t % 4 + 1)
        stage_c(NT - 1)
        stage_d(NT - 1)

    pt2ring = ptring = sbring = ptbring = rdring = bcring = ocring = sqbring = sqcring = OaT = ObT = ocT = r_Oa = r_Ob = r_Oc = zring = cpring = yring = accring = rsring = xring = garing = None

    def alloc_attn_bufs():
        nonlocal pt2ring, ptring, sbring, ptbring, rdring, bcring, ocring, sqbring, sqcring, OaT, ObT, ocT, r_Oa, r_Ob, r_Oc, zring, cpring, yring, accring, rsring, xring, garing
        ptring = Ring(K, "pt", [128, 512], BF16, 1)
        pt2ring = Ring(K, "pt2", [128, 2, 512], BF16, 4)
        sbring = Ring(K, "sbb", [128, 256], F32, 2)
        ptbring = Ring(K, "ptb", [128, 256], BF16, 16)
        rdring = Ring(K, "rd", [128, 512], F32, 3)
        bcring = Ring(K, "bcs", [64, 512], F32, 2)
        ocring = Ring(K, "ocp", [128, 512], F32, 3)
        sqbring = Ring(K, "sqb", [128, 512], BF16, 3)
        sqcring = Ring(K, "sqc", [128, 512], BF16, 2)
        for i_ in range(3):
            op("pool", lambda e, i_=i_: e.memset(sqbring.t[i_][:], 0.0), writes=[sqbring.r[i_]])
        OaT = K.sb("OaT", [64, 8, 512], BF16)
        ObT = K.sb("ObT", [64, 4, 512], BF16)
        ocT = K.sb("ocT", [128, 2, 512], BF16)
        r_Oa = [Res() for _ in range(8)]
        r_Ob = [Res() for _ in range(4)]
        r_Oc = [Res() for _ in range(2)]
        zring = Ring(K, "zz", [128, 516], F32, 2)
        cpring = Ring(K, "cp", [128, 512], F32, 2)
        yring = Ring(K, "ya", [128, 512], F32, 2)
        accring = Ring(K, "aca", [128, D], F32, 2)
        rsring = Ring(K, "rsa", [128, 16], F32, 2)
        xring = Ring(K, "xb", [128, D], F32, 2)
        garing = Ring(K, "ga", [128, D], F32, 1)

    dq = []

    def defer(k, fn):
        dq.append([k, fn])

    def tick():
        for it in dq:
            it[0] -= 1
        while dq and dq[0][0] <= 0:
            dq.pop(0)[1]()

    def flush_deferred():
        while dq:
            dq.pop(0)[1]()

    def normalise(ob, obr, n, dst_fn, pe_fn=None, d1=0, d2=0):
        oc_, ocr = ocring.next()
        op("dve", lambda e: e.tensor_copy(out=oc_[0:65, 0:n], in_=ob[0:65, 0:n]), reads=[obr], writes=[ocr])
        rd, rdr = rdring.next()
        op("dve", lambda e: e.reciprocal(out=rd[64:65, 0:n], in_=oc_[64:65, 0:n]), reads=[ocr], writes=[rdr])

        def p2():
            bcs, bcr = bcring.next()
            for h0 in range(0, n, 256):
                hn = min(256, n - h0)
                op("pe", lambda e, h0=h0, hn=hn: e.matmul(BC[0:64, 0:hn], lhsT=ones_f[64:65, 0:64], rhs=rd[64:65, h0:h0 + hn],
                                                          start=True, stop=True), reads=[rdr, r_one], writes=[r_BC])
                op("dve", lambda e, h0=h0, hn=hn: e.tensor_copy(out=bcs[:, h0:h0 + hn], in_=BC[0:64, 0:hn]), reads=[r_BC], writes=[bcr])
            ctx_ = dst_fn(oc_, bcs, ocr, bcr)
            if pe_fn is not None:
                if d2 > 0:
                    defer(d2, lambda: pe_fn(ctx_))
                else:
                    pe_fn(ctx_)
        if d1 > 0:
            defer(d1, p2)
        else:
            p2()

    def ss_sq(srcT, r_src, np_, n):
        sqb, sqr = (sqbring if np_ == 64 else sqcring).next()
        op("dve", lambda e: e.tensor_tensor(out=sqb[0:np_, 0:n], in0=srcT, in1=srcT, op=ALU.mult), reads=[r_src], writes=[sqr])
        return sqb, sqr

    def ss_mm(sqb, sqr, n, col0, stride):
        for ti in range(n // 128):
            c = 256 + 2 * (col0 + ti * stride)
            op("pe", lambda e, ti=ti, c=c: e.matmul(BC[:, c:c + 2], lhsT=sqb[:, ti * 128:(ti + 1) * 128], rhs=ones_b[:, 0:2],
                                                    start=True, stop=True), reads=[sqr, r_one], writes=[r_SS])

    def ss_cols(srcT, r_src, np_, n, col0, stride):
        if "noss" in SKIP:
            return
        sqb, sqr = ss_sq(srcT, r_src, np_, n)
        ss_mm(sqb, sqr, n, col0, stride)

    def attn_block(b, q0, n, ctxq):
        ntl = n // 128
        t_first = q0 // 128
        qres = [r_q[t_first + i] for i in range(ntl)]
        kcs = list(range(LT, NT)) if ctxq else list(range(NT))
        items = [(j, kc) for j in range(4) for kc in kcs]

        def a_st(i):
            j, kc = items[i]
            sp_ = stp[i % 2]
            r0_, r1_ = (r_bk[0], r_bk[1]) if i % 2 == 0 else (r_bk[6], r_bk[7])
            for g, rr_ in ((0, r0_), (1, r1_)):
                op("pe", lambda e, g=g: e.matmul(sp_[:, g, 0:n], lhsT=kTa[64 * g:64 * g + 64, kc * 128:(kc + 1) * 128],
                                                 rhs=qTa[64 * g:64 * g + 64, j, q0:q0 + n], start=True, stop=True),
                   reads=[r_q[kc]] + qres, writes=[rr_])
            pt, ptr = pt2ring.next()
            op("act", lambda e: e.activation(out=pt[:, :, 0:n], in_=sp_[:, :, 0:n], func=AF.Exp, scale=0.125),
               reads=[r0_, r1_], writes=[ptr])
            return pt, ptr

        def a_pv(i, ptp):
            j, kc = items[i]
            pt, ptr = ptp
            for g in range(2):
                ob, obr = OB[g]
                if "nopv" not in SKIP:
                    op("pe", lambda e, g=g, ob=ob: e.matmul(ob[0:65, 0:n], lhsT=Va[:, kc, g, :], rhs=pt[:, g, 0:n],
                                                           start=(kc == kcs[0]), stop=(kc == kcs[-1])),
                       reads=[ptr, r_q[kc]], writes=[obr])
            if kc == kcs[-1] and "nonorm" not in SKIP:
                for g in range(2):
                    h = 4 * g + j
                    ob, obr = OB[g]

                    def fin(ob_, bcs, obr_, bcr, h=h):
                        op("dve", lambda e: e.tensor_tensor(out=OaT[:, h, 0:n], in0=ob_[0:64, 0:n], in1=bcs[:, 0:n], op=ALU.mult),
                           reads=[obr_, bcr], writes=[r_Oa[h]])
                        return ss_sq(OaT[:, h, 0:n], r_Oa[h], 64, n)

                    def pef(c_, h=h):
                        ss_mm(c_[0], c_[1], n, h, 8)
                    normalise(ob, obr, n, fin, pef, d1=2 + g, d2=2)

        if "noA" in SKIP:
            items = []
        pend = [a_st(i) for i in range(min(2, len(items)))]
        for i in range(len(items)):
            if i + 2 < len(items):
                pend.append(a_st(i + 2))
            a_pv(i, pend.pop(0))
            tick()
        flush_deferred()

        nrow = n // 64
        bitems = []
        for rr in range(nrow):
            if ctxq:
                chunks = [(LT, None, None), (LT + 1, None, None)]
            else:
                r = q0 // 64 + rr
                rs = min(max(r - 4, 0), 24)
                kt0 = rs // 2
                nch = 5 if rs % 2 else 4
                chunks = []
                for c in range(nch):
                    drs = []
                    for s_ in range(2):
                        kr = 2 * (kt0 + c) + s_
                        drs.append(kr - r + 7 if rs <= kr <= rs + 7 else 15)
                    chunks.append((kt0 + c, drs[0], drs[1]))
                chunks += [(LT, None, None), (LT + 1, None, None)]
            for ci, ch in enumerate(chunks):
                bitems.append((rr, ch, ci == 0, ci == len(chunks) - 1))

        def b_st(i):
            rr, (kt, d0, d1), first, lastc = bitems[i]
            qa = q0 + rr * 64
            for h in (0, 2, 1, 3):
                pr, hf = h // 2, h % 2
                stb, str_ = ST4[2 * (i % 2) + hf]
                op("pe", lambda e, h=h, pr=pr, hf=hf, stb=stb: e.matmul(stb[:, pr * 64:(pr + 1) * 64], lhsT=kTb[64 * hf:64 * hf + 64, pr, kt * 128:(kt + 1) * 128],
                                                                        rhs=qTb[64 * hf:64 * hf + 64, pr, qa:qa + 64], start=True, stop=True),
                   reads=[r_q[kt], r_q[qa // 128]], writes=[str_])
            pt, ptr = ptbring.next()
            ptv = pt[:, 0:256].rearrange("p (pr hf c) -> p hf pr c", pr=2, hf=2)
            for hf in range(2):
                stb, str_ = ST4[2 * (i % 2) + hf]
                sv = stb[:, 0:128].rearrange("p (pr c) -> p pr c", pr=2)
                if d0 is None:
                    op("act", lambda e, hf=hf, sv=sv: e.activation(out=ptv[:, hf], in_=sv, func=AF.Exp, scale=0.125), reads=[str_], writes=[ptr])
                else:
                    sbb, sbr = sbring.next()
                    bv_ = sbb[:, 0:128].rearrange("p (pr c) -> p pr c", pr=2)
                    for s_, dd in ((0, d0), (1, d1)):
                        ps_ = slice(64 * s_, 64 * s_ + 64)
                        tv = tbl[ps_, dd, :, :].rearrange("p (pr hf) c -> p hf pr c", hf=2)[:, hf]
                        op("dve", lambda e, ps_=ps_, tv=tv, sv=sv, bv_=bv_: e.scalar_tensor_tensor(
                            out=bv_[ps_], in0=sv[ps_], scalar=0.125, in1=tv, op0=ALU.mult, op1=ALU.add), reads=[str_, r_tbl], writes=[sbr])
                    op("act", lambda e, hf=hf, bv_=bv_: e.activation(out=ptv[:, hf], in_=bv_, func=AF.Exp), reads=[sbr], writes=[ptr])
            return pt, ptr

        def b_pv_row(rr, pts):
            ob, obr = OB[(rr // 2) % 2]
            cbase = (rr % 2) * 256
            nchk = len(pts)
            for h in range(4):
                for ci, (kt, pt, ptr) in enumerate(pts):
                    op("pe", lambda e, h=h, kt=kt, pt=pt, ci=ci: e.matmul(ob[0:65, cbase + h * 64:cbase + (h + 1) * 64], lhsT=Vb[:, kt, h, :],
                                                                        rhs=pt[:, h * 64:(h + 1) * 64], start=(ci == 0), stop=(ci == nchk - 1)),
                       reads=[ptr, r_q[kt]], writes=[obr])
            if rr % 2 == 1 and "nobnorm" not in SKIP:
                r0 = rr - 1

                def fin(ob_, bcs, obr_, bcr):
                    for h in range(4):
                        ov = ObT[:, h, r0 * 64:r0 * 64 + 128].rearrange("p (r c) -> p r c", r=2)
                        iv = ob_[0:64, :].rearrange("p (r h c) -> p r h c", r=2, h=4)[:, :, h, :]
                        bv = bcs[:, :].rearrange("p (r h c) -> p r h c", r=2, h=4)[:, :, h, :]
                        op("dve", lambda e, ov=ov, iv=iv, bv=bv: e.tensor_tensor(out=ov, in0=iv, in1=bv, op=ALU.mult),
                           reads=[obr_, bcr], writes=[r_Ob[h]])
                    return None
                normalise(ob, obr, 512, fin, None, d1=1)

        def b_st_row(rr):
            idxs = [i for i in range(len(bitems)) if bitems[i][0] == rr]
            out = []
            for i in idxs:
                pt, ptr = b_st(i)
                out.append((bitems[i][1][0], pt, ptr))
            return out

        if "noB" in SKIP:
            bitems = []
        nrows_b = nrow if bitems else 0
        curp = b_st_row(0) if nrows_b else None
        for rr in range(nrows_b):
            nxtp = b_st_row(rr + 1) if rr + 1 < nrows_b else None
            b_pv_row(rr, curp)
            tick()
            curp = nxtp
        flush_deferred()
        for h in range(4):
            if "noB" not in SKIP:
                ss_cols(ObT[:, h, 0:n], r_Ob[h], 64, n, 32 + h, 4)

        z0 = zcol(t_first)
        for j in range(2 if "noC" not in SKIP else 0):
            zz, zr = zring.next()
            op("sp", lambda e, j=j, zz=zz: e.dma_start(out=zz[:, 0:n + 2], in_=K.czT[0, j * 128:(j + 1) * 128, z0 - 1:z0 + n + 1]),
               reads=[r_cz], writes=[zr], dma=True)
            cp, cpr = cpring.next()
            op("sp", lambda e, j=j, cp=cp: e.dma_start(out=cp[:, 0:n], in_=K.czT[1, j * 128:(j + 1) * 128, z0:z0 + n]),
               reads=[r_cz], writes=[cpr], dma=True)
            yy, yr = yring.next()
            op("dve", lambda e, j=j, zz=zz, yy=yy: e.tensor_scalar(out=yy[:, 0:n], in0=zz[:, 0:n], scalar1=cwc[:, j, 0:1], scalar2=None, op0=ALU.mult),
               reads=[zr, r_cw], writes=[yr])
            for w in (1, 2):
                op("dve", lambda e, j=j, zz=zz, yy=yy, w=w: e.scalar_tensor_tensor(out=yy[:, 0:n], in0=zz[:, w:w + n], scalar=cwc[:, j, w:w + 1],
                                                                                   in1=yy[:, 0:n], op0=ALU.mult, op1=ALU.add),
                   reads=[zr, r_cw, yr], writes=[yr])
            op("dve", lambda e, j=j, cp=cp, yy=yy: e.scalar_tensor_tensor(out=ocT[:, j, 0:n], in0=yy[:, 0:n], scalar=cwc[:, j, 3:4], in1=cp[:, 0:n],
                                                                          op0=ALU.add, op1=ALU.mult), reads=[yr, cpr, r_cw], writes=[r_Oc[j]])
            ss_cols(ocT[:, j, 0:n], r_Oc[j], 128, n, 48 + j, 2)

        if "oT" in K.dbg:
            for h in range(8):
                op("pool", lambda e, h=h: e.dma_start(out=K.dbg["oT"][b, h * 64:(h + 1) * 64, q0:q0 + n], in_=OaT[:, h, 0:n]),
                   reads=[r_Oa[h]], writes=[Res()], dma=True)
            for h in range(4):
                op("pool", lambda e, h=h: e.dma_start(out=K.dbg["oT"][b, 512 + h * 64:512 + (h + 1) * 64, q0:q0 + n], in_=ObT[:, h, 0:n]),
                   reads=[r_Ob[h]], writes=[Res()], dma=True)
            for j in range(2):
                op("pool", lambda e, j=j: e.dma_start(out=K.dbg["oT"][b, 768 + j * 128:768 + (j + 1) * 128, q0:q0 + n], in_=ocT[:, j, 0:n]),
                   reads=[r_Oc[j]], writes=[Res()], dma=True)
        if "nomerge" in SKIP:
            return
        v = NB if ctxq else b
        gat, gar = garing.next()
        op("sp", lambda e: e.dma_start(out=gat[:], in_=K.modv[l, v, 2 * D:3 * D].partition_broadcast(128)), writes=[gar], dma=True)
        rs_, rsr = rsring.next()
        for gi_, (c0, nh, width) in enumerate(((0, 8, 512.0), (32, 4, 256.0), (48, 2, 256.0))):
            op("dve", lambda e, gi_=gi_, c0=c0, nh=nh: e.tensor_reduce(
                out=rs_[:, gi_ * 4:gi_ * 4 + ntl], in_=BC[:, 256 + 2 * c0:256 + 2 * (c0 + ntl * nh)].rearrange("p (t h two) -> p t h two", h=nh, two=2)[:, :, :, 0],
                axis=AX.X, op=ALU.add), reads=[r_SS], writes=[rsr])
            op("act", lambda e, gi_=gi_, width=width: e.activation(out=rs_[:, gi_ * 4:gi_ * 4 + ntl], in_=rs_[:, gi_ * 4:gi_ * 4 + ntl],
                                                                  func=AF.Sqrt, bias=EPS, scale=1.0 / width), reads=[rsr], writes=[rsr])
        op("dve", lambda e: e.reciprocal(out=rs_[:, 0:12], in_=rs_[:, 0:12]), reads=[rsr], writes=[rsr])
        for ti in range(ntl):
            t = t_first + ti
            tk = slice(ti * 128, (ti + 1) * 128)
            ac, acr = accring.next()
            xt, xr = xring.next()
            op("sp", lambda e, xt=xt, t=t: e.dma_start(out=xt[:], in_=xsrc(K, l, b, t)), writes=[xr], dma=True)
            for cb in range(2):
                cs = slice(cb * 512, (cb + 1) * 512)
                groups = (([(OaT[:, h, tk], woA[:, h, cs], r_Oa[h]) for h in range(8)], 0),
                          ([(ObT[:, h, tk], woB[:, h, cs], r_Ob[h]) for h in range(4)], 1),
                          ([(ocT[:, j, tk], woC[:, j, cs], r_Oc[j]) for j in range(2)], 2))
                for mm, gi_ in groups:
                    opb, opr = OPR[mctr[0] % 3]
                    mctr[0] += 1
                    for i, (lt, rh, rr_) in enumerate(mm):
                        op("pe", lambda e, lt=lt, rh=rh, i=i, nmm=len(mm), opb=opb: e.matmul(opb[:, :], lhsT=lt, rhs=rh, start=(i == 0), stop=(i == nmm - 1)),
                           reads=[rr_] + r_win, writes=[opr])
                    sc1 = rs_[:, gi_ * 4 + ti:gi_ * 4 + ti + 1]
                    if gi_ == 0:
                        op("dve", lambda e, ac=ac, cs=cs, sc1=sc1, opb=opb: e.tensor_scalar(out=ac[:, cs], in0=opb[:, :], scalar1=sc1, scalar2=None, op0=ALU.mult),
                           reads=[opr, rsr], writes=[acr])
                    else:
                        op("dve", lambda e, ac=ac, cs=cs, sc1=sc1, opb=opb: e.scalar_tensor_tensor(out=ac[:, cs], in0=opb[:, :], scalar=sc1, in1=ac[:, cs],
                                                                                          op0=ALU.mult, op1=ALU.add),
                           reads=[opr, rsr, acr], writes=[acr])
            op("pool", lambda e, ac=ac: e.tensor_tensor(out=ac[:], in0=ac[:], in1=gat[:], op=ALU.mult), reads=[acr, gar], writes=[acr])
            op("dve", lambda e, ac=ac, xt=xt: e.tensor_tensor(out=ac[:], in0=ac[:], in1=xt[:], op=ALU.add), reads=[acr, xr], writes=[acr])
            op("sp", lambda e, ac=ac, t=t: e.dma_start(out=K.xcur[b, t * 128:(t + 1) * 128, :], in_=ac[:]), reads=[acr], writes=[Res()], dma=True)

    for b in range(NB):
        load_win()
        with ExitStack() as st2:
            K.stk.append(st2)
            pre_stage(b)
            K.flush()
            K.stk.pop()
        load_wout()
        if "noattn" in SKIP:
            continue
        with ExitStack() as st2:
            K.stk.append(st2)
            alloc_attn_bufs()
            for qb in range(4):
                attn_block(b, qb * 512, 512, False)
            if not last:
                attn_block(b, SEQ, NCTX, True)
            K.flush()
            K.stk.pop()


def _rope_table():
    t = np.arange(SEQ)
    row = (t // GW).astype(np.float32)
    col = (t % GW).astype(np.float32)
    half = HD // 2
    inv = (np.float32(10000.0) ** (-np.arange(0, half, 2, dtype=np.float32) / np.float32(half))).astype(np.float32)
    ar = row[:, None] * inv
    ac = col[:, None] * inv
    cr, sr, cc, sc = np.cos(ar), np.sin(ar), np.cos(ac), np.sin(ac)
    tab = np.zeros((NKEY, 128), np.float32)
    tab[:SEQ, 0:64] = np.concatenate([cr, cr, cc, cc], 1)
    tab[:SEQ, 64:128] = np.concatenate([-sr, sr, -sc, sc], 1)
    tab[SEQ:, 0:64] = 1.0
    return tab


def _rpb_layout(rpb):
    cc = np.arange(GW)
    cs = np.clip(cc - 8, 0, GW - 16)
    dc_idx = np.clip(cc[None, :] - cc[:, None] + 15, 0, 30)
    in_win = (cc[None, :] >= cs[:, None]) & (cc[None, :] < cs[:, None] + 16)
    g = rpb[:, :, :, dc_idx]
    g = np.where(in_win[None, None, None], g, np.float32(NEG))
    return np.ascontiguousarray(g.transpose(0, 2, 4, 1, 3)).astype(np.float32)


def make_in_maps(inp, cores=range(8)):
    f = lambda a: np.ascontiguousarray(np.asarray(a, dtype=np.float32))
    shared = {
        "w_mod": f(inp["w_mod"]), "b_mod": f(inp["b_mod"]), "w_in": f(inp["w_in"]),
        "gains": f(np.stack([inp["gq_a"], inp["gk_a"], inp["gq_b"], inp["gk_b"]], 1)),
        "rpbT": _rpb_layout(np.asarray(inp["rpb"], np.float32)),
        "conv_w": f(inp["conv_w"]), "conv_b": f(inp["conv_b"]), "g_out": f(inp["g_out"]), "w_out": f(inp["w_out"]),
        "ffn_w1": f(inp["ffn_w1"]), "ffn_w3": f(inp["ffn_w3"]), "ffn_w2": f(inp["ffn_w2"]),
        "moe_router": f(inp["moe_router"]), "moe_router_b": f(inp["moe_router_b"]),
        "moe_w1": f(inp["moe_w1"]), "moe_w3": f(inp["moe_w3"]), "moe_w2": f(inp["moe_w2"]),
        "ident": np.eye(128, dtype=np.float32), "rope": _rope_table(),
    }
    maps = []
    for i in cores:
        m = dict(shared)
        m["x2"] = f(inp["x"][NB * i:NB * i + NB])
        m["ctx2"] = f(inp["ctx"][NB * i:NB * i + NB])
        m["cvec"] = f(np.concatenate([inp["c"][NB * i:NB * i + NB], np.asarray(inp["c_ctx"])[None]], 0))
        maps.append(m)
    return maps


_NC_CACHE = {}


def kernel(**inputs):
    if "nc" not in _NC_CACHE:
        _NC_CACHE["nc"] = build()
    nc = _NC_CACHE["nc"]
    maps = make_in_maps(inputs)
    res = run_bass_kernel_spmd(nc, maps, core_ids=list(range(8)))
    return np.concatenate([np.asarray(r["y"], dtype=np.float32) for r in res.results], axis=0)
```
